# Optimizing a Trainium2 kernel written in Bass

```python
import math
import jax, jax.numpy as jnp
from jax import lax
import numpy as np

D_MODEL = 1024
BATCH = 16
SEQ = 4096
DEPTH = 1

ATTN_HEADS = D_MODEL // 256
ATTN_HEAD_DIM = 64
ATTN_V_DIM = 2 * ATTN_HEAD_DIM
ATTN_QK_WIDTH = ATTN_HEADS * 2 * ATTN_HEAD_DIM
ATTN_V_WIDTH = ATTN_HEADS * ATTN_V_DIM
Q_BLOCK = 128
NEG_INF = -1e30
REL_BUCKETS = 32
REL_MAX_DISTANCE = 128
SSM_GROUP_CH = 16
SSM_WIDTH = D_MODEL // 2
SSM_GROUPS = SSM_WIDTH // SSM_GROUP_CH
SSM_STATE = 64
SSM_DT_MIN = 1e-3
SSM_DT_MAX = 1e-1
SSM_EIG_MAX_RE = -1e-4
N_BRANCHES = 2
SPLIT_Q = ATTN_QK_WIDTH
SPLIT_K = SPLIT_Q + ATTN_QK_WIDTH
SPLIT_V = SPLIT_K + ATTN_V_WIDTH
SPLIT_U = SPLIT_V + SSM_WIDTH
IN_WIDTH = SPLIT_U + N_BRANCHES * D_MODEL
MOE_GROUPS = 4
MOE_EXPERTS_PER_GROUP = 8
MOE_EXPERTS = MOE_GROUPS * MOE_EXPERTS_PER_GROUP
MOE_TOP_K = 2
MOE_D_FF = D_MODEL // 2
MOE_BLOCK = 256
NORM_EPS = 1e-6
SUBLN_EPS = 1e-5

kernel_name = 'hybrid_diffattn_s5_hiermoe_block'


def rms_norm(x, g, eps=NORM_EPS):
    xf = x.astype(jnp.float32)
    y = xf * lax.rsqrt(jnp.mean(xf * xf, axis=-1, keepdims=True) + eps)
    return (y * g.astype(jnp.float32)).astype(x.dtype)


def modulate(h, shift, scale):
    return h * (1 + scale[:, None, :]) + shift[:, None, :]


def rel_bucket(dist):
    max_exact = REL_BUCKETS // 2
    n = jnp.maximum(dist, 0)
    log_ratio = jnp.log(jnp.maximum(n, 1).astype(jnp.float32) / max_exact) / math.log(REL_MAX_DISTANCE / max_exact)
    large = max_exact + (log_ratio * (REL_BUCKETS - max_exact)).astype(jnp.int32)
    large = jnp.minimum(large, REL_BUCKETS - 1)
    return jnp.where(n < max_exact, n, large)


def diff_attention(q, k, v, rel_bias, lq1, lk1, lq2, lk2, subln_g, lambda_init):
    B_, S_ = q.shape[0], q.shape[1]
    f32 = jnp.float32
    lam = (jnp.exp(jnp.sum(lq1.astype(f32) * lk1.astype(f32)))
           - jnp.exp(jnp.sum(lq2.astype(f32) * lk2.astype(f32))) + lambda_init)
    qT = jnp.transpose(q, (0, 2, 3, 1, 4)) * (ATTN_HEAD_DIM ** -0.5)
    kT = jnp.transpose(k, (0, 2, 3, 1, 4))
    vT = jnp.transpose(v, (0, 2, 1, 3))
    outs = []
    for blk in range(S_ // Q_BLOCK):
        q0 = blk * Q_BLOCK
        q1 = q0 + Q_BLOCK
        dist = jnp.arange(q0, q1)[:, None] - jnp.arange(q1)[None, :]
        bias = jnp.transpose(rel_bias[rel_bucket(dist)], (2, 0, 1)).astype(f32)
        s = jnp.einsum('bhmqd,bhmkd->bhmqk', qT[:, :, :, q0:q1], kT[:, :, :, :q1]).astype(f32)
        s = jnp.where(dist >= 0, s + bias[None, :, None], NEG_INF)
        p = jax.nn.softmax(s, axis=-1)
        w = p[:, :, 0] - lam * p[:, :, 1]
        outs.append(jnp.einsum('bhqk,bhkd->bhqd', w.astype(vT.dtype), vT[:, :, :q1]))
    o = jnp.concatenate(outs, axis=2)
    o = rms_norm(o, subln_g, SUBLN_EPS) * (1 - lambda_init)
    return jnp.transpose(o, (0, 2, 1, 3)).reshape(B_, S_, ATTN_V_WIDTH)


def _complex_linear_combine(e1, e2):
    a1r, a1i, b1r, b1i = e1
    a2r, a2i, b2r, b2i = e2
    return (a2r * a1r - a2i * a1i,
            a2r * a1i + a2i * a1r,
            a2r * b1r - a2i * b1i + b2r,
            a2r * b1i + a2i * b1r + b2i)


def s5_branch(u, lam_re, lam_im, log_step, b_re, b_im, c_re, c_im, d_skip, w_glu):
    B_, S_ = u.shape[0], u.shape[1]
    f32 = jnp.float32
    ug = u.astype(f32).reshape(B_, S_, SSM_GROUPS, SSM_GROUP_CH)
    lr = jnp.minimum(lam_re.astype(f32), SSM_EIG_MAX_RE)
    li = lam_im.astype(f32)
    step = jnp.exp(log_step.astype(f32))[:, None]
    mag = jnp.exp(lr * step)
    ang = li * step
    a_re = mag * jnp.cos(ang)
    a_im = mag * jnp.sin(ang)
    den = lr * lr + li * li
    num_re = a_re - 1.0
    coef_re = (num_re * lr + a_im * li) / den
    coef_im = (a_im * lr - num_re * li) / den
    br = b_re.astype(f32)
    bi = b_im.astype(f32)
    bb_re = coef_re[..., None] * br - coef_im[..., None] * bi
    bb_im = coef_re[..., None] * bi + coef_im[..., None] * br
    bu_re = jnp.einsum('bsgh,gph->bsgp', ug, bb_re)
    bu_im = jnp.einsum('bsgh,gph->bsgp', ug, bb_im)
    a_re_s = jnp.broadcast_to(a_re, (S_,) + a_re.shape)
    a_im_s = jnp.broadcast_to(a_im, (S_,) + a_im.shape)

    def scan_one(bre, bim):
        _, _, xr, xi = lax.associative_scan(_complex_linear_combine, (a_re_s, a_im_s, bre, bim), axis=0)
        return xr, xi

    xr, xi = jax.vmap(scan_one)(bu_re, bu_im)
    y = (jnp.einsum('bsgp,ghp->bsgh', xr, c_re.astype(f32))
         - jnp.einsum('bsgp,ghp->bsgh', xi, c_im.astype(f32))
         + d_skip.astype(f32) * ug)
    y = jax.nn.gelu(y.reshape(B_, S_, SSM_WIDTH)).astype(u.dtype)
    gl = y @ w_glu
    return gl[..., :SSM_WIDTH] * jax.nn.sigmoid(gl[..., SSM_WIDTH:])


def hier_moe(h, w_rg, b_rg, w_re, b_re, w_e_in, w_e_out):
    B_, S_, D = h.shape
    T = B_ * S_
    xt = h.reshape(T, D)
    lg = (xt @ w_rg + b_rg).astype(jnp.float32)
    pg = jax.nn.softmax(lg, axis=-1)
    grp = jnp.argmax(lg, axis=-1)
    p_grp = jnp.take_along_axis(pg, grp[:, None], axis=-1)
    le_all = (jnp.einsum('td,gde->tge', xt, w_re) + b_re).astype(jnp.float32)
    le = jnp.take_along_axis(le_all, grp[:, None, None], axis=1)[:, 0]
    top_v, top_i = lax.top_k(le, MOE_TOP_K)
    comb = p_grp * jax.nn.softmax(top_v, axis=-1)
    eid = grp[:, None] * MOE_EXPERTS_PER_GROUP + top_i
    n_assign = T * MOE_TOP_K
    n_pad = (-(-n_assign // MOE_BLOCK) + MOE_EXPERTS) * MOE_BLOCK
    e_flat = eid.reshape(n_assign).astype(jnp.int32)
    w_flat = comb.reshape(n_assign)
    tok_flat = jnp.repeat(jnp.arange(T, dtype=jnp.int32), MOE_TOP_K)
    order = jnp.argsort(e_flat)
    e_sorted = e_flat[order]
    counts = jax.ops.segment_sum(jnp.ones_like(e_flat), e_flat, num_segments=MOE_EXPERTS)
    starts = jnp.cumsum(counts) - counts
    padded = ((counts + MOE_BLOCK - 1) // MOE_BLOCK) * MOE_BLOCK
    pends = jnp.cumsum(padded)
    pstarts = pends - padded
    dest = pstarts[e_sorted] + (jnp.arange(n_assign, dtype=jnp.int32) - starts[e_sorted])
    row_tok = jnp.full((n_pad,), T, jnp.int32).at[dest].set(tok_flat[order])
    row_w = jnp.zeros((n_pad,), jnp.float32).at[dest].set(w_flat[order])
    n_blocks = n_pad // MOE_BLOCK
    block_start = jnp.arange(n_blocks, dtype=jnp.int32) * MOE_BLOCK
    block_e = jnp.minimum(jnp.searchsorted(pends, block_start, side='right'), MOE_EXPERTS - 1)
    x_pad = jnp.concatenate([xt, jnp.zeros((1, D), xt.dtype)], axis=0)
    xs = x_pad[row_tok].reshape(n_blocks, MOE_BLOCK, D)

    def expert_block(args):
        xb, e = args
        hid = xb @ w_e_in[e]
        a, g = jnp.split(hid, 2, axis=-1)
        return (jax.nn.silu(a) * g) @ w_e_out[e]

    ys = lax.map(expert_block, (xs, block_e)).reshape(n_pad, D)
    out = jnp.zeros((T + 1, D), h.dtype).at[row_tok].add(ys * row_w[:, None].astype(ys.dtype))
    return out[:T].reshape(B_, S_, D)


def setup_inputs(seed: int = 0) -> dict:
    key = jax.random.key(seed)
    ks = jax.random.split(key, 32)
    f32 = jnp.float32

    def nrm(k, shape, scale):
        return jax.random.normal(k, shape, f32) * scale

    G, P, H = SSM_GROUPS, SSM_STATE, SSM_GROUP_CH
    n = jnp.arange(SSM_STATE, dtype=f32)
    return {
        'x': nrm(ks[0], (BATCH, SEQ, D_MODEL), 1.0),
        'c': nrm(ks[1], (BATCH, D_MODEL), 1.0),
        'w_ada': nrm(ks[2], (DEPTH, D_MODEL, 6 * D_MODEL), 0.5 * D_MODEL ** -0.5),
        'b_ada': nrm(ks[3], (DEPTH, 6 * D_MODEL), 0.01),
        'norm1_g': 1.0 + nrm(ks[4], (DEPTH, D_MODEL), 0.01),
        'w_in': nrm(ks[5], (DEPTH, D_MODEL, IN_WIDTH), D_MODEL ** -0.5),
        'rel_bias': nrm(ks[6], (REL_BUCKETS, ATTN_HEADS), 0.5),
        'lambda_q1': nrm(ks[7], (DEPTH, ATTN_HEAD_DIM), 0.1),
        'lambda_k1': nrm(ks[8], (DEPTH, ATTN_HEAD_DIM), 0.1),
        'lambda_q2': nrm(ks[9], (DEPTH, ATTN_HEAD_DIM), 0.1),
        'lambda_k2': nrm(ks[10], (DEPTH, ATTN_HEAD_DIM), 0.1),
        'subln_g': 1.0 + nrm(ks[11], (DEPTH, ATTN_V_DIM), 0.01),
        'ssm_lambda_re': -0.5 + nrm(ks[12], (DEPTH, G, P), 0.01),
        'ssm_lambda_im': math.pi * n + nrm(ks[13], (DEPTH, G, P), 0.01),
        'ssm_log_step': jax.random.uniform(ks[14], (DEPTH, G), f32, math.log(SSM_DT_MIN), math.log(SSM_DT_MAX)),
        'ssm_b_re': nrm(ks[15], (DEPTH, G, P, H), (2 * H) ** -0.5),
        'ssm_b_im': nrm(ks[16], (DEPTH, G, P, H), (2 * H) ** -0.5),
        'ssm_c_re': nrm(ks[17], (DEPTH, G, H, P), P ** -0.5),
        'ssm_c_im': nrm(ks[18], (DEPTH, G, H, P), P ** -0.5),
        'ssm_d': nrm(ks[19], (DEPTH, G, H), 1.0),
        'w_glu': nrm(ks[20], (DEPTH, SSM_WIDTH, 2 * SSM_WIDTH), SSM_WIDTH ** -0.5),
        'w_proj_attn': nrm(ks[21], (DEPTH, ATTN_V_WIDTH, D_MODEL), ATTN_V_WIDTH ** -0.5),
        'w_proj_ssm': nrm(ks[22], (DEPTH, SSM_WIDTH, D_MODEL), SSM_WIDTH ** -0.5),
        'w_out': nrm(ks[23], (DEPTH, D_MODEL, D_MODEL), D_MODEL ** -0.5),
        'norm2_g': 1.0 + nrm(ks[24], (DEPTH, D_MODEL), 0.01),
        'w_router_group': nrm(ks[25], (DEPTH, D_MODEL, MOE_GROUPS), D_MODEL ** -0.5),
        'b_router_group': nrm(ks[26], (DEPTH, MOE_GROUPS), 0.01),
        'w_router_expert': nrm(ks[27], (DEPTH, MOE_GROUPS, D_MODEL, MOE_EXPERTS_PER_GROUP), D_MODEL ** -0.5),
        'b_router_expert': nrm(ks[28], (DEPTH, MOE_GROUPS, MOE_EXPERTS_PER_GROUP), 0.01),
        'w_expert_in': nrm(ks[29], (DEPTH, MOE_EXPERTS, D_MODEL, 2 * MOE_D_FF), D_MODEL ** -0.5),
        'w_expert_out': nrm(ks[30], (DEPTH, MOE_EXPERTS, MOE_D_FF, D_MODEL), MOE_D_FF ** -0.5),
        'final_g': 1.0 + nrm(ks[31], (D_MODEL,), 0.01),
    }


def reference(x, c, w_ada, b_ada, norm1_g, w_in, rel_bias, lambda_q1, lambda_k1, lambda_q2, lambda_k2,
              subln_g, ssm_lambda_re, ssm_lambda_im, ssm_log_step, ssm_b_re, ssm_b_im, ssm_c_re, ssm_c_im,
              ssm_d, w_glu, w_proj_attn, w_proj_ssm, w_out, norm2_g, w_router_group, b_router_group,
              w_router_expert, b_router_expert, w_expert_in, w_expert_out, final_g):
    B_, S_ = x.shape[0], x.shape[1]
    c_act = jax.nn.silu(c)
    for layer in range(DEPTH):
        lambda_init = 0.8 - 0.6 * math.exp(-0.3 * layer)
        mod = c_act @ w_ada[layer] + b_ada[layer]
        shift1, scale1, gate1, shift2, scale2, gate2 = jnp.split(mod, 6, axis=-1)
        h = modulate(rms_norm(x, norm1_g[layer]), shift1, scale1)
        proj = h @ w_in[layer]
        q, k, v, u, gates = jnp.split(proj, [SPLIT_Q, SPLIT_K, SPLIT_V, SPLIT_U], axis=-1)
        y_attn = diff_attention(
            q.reshape(B_, S_, ATTN_HEADS, 2, ATTN_HEAD_DIM),
            k.reshape(B_, S_, ATTN_HEADS, 2, ATTN_HEAD_DIM),
            v.reshape(B_, S_, ATTN_HEADS, ATTN_V_DIM),
            rel_bias, lambda_q1[layer], lambda_k1[layer], lambda_q2[layer], lambda_k2[layer],
            subln_g[layer], lambda_init)
        y_ssm = s5_branch(u, ssm_lambda_re[layer], ssm_lambda_im[layer], ssm_log_step[layer],
                          ssm_b_re[layer], ssm_b_im[layer], ssm_c_re[layer], ssm_c_im[layer],
                          ssm_d[layer], w_glu[layer])
        g_attn, g_ssm = jnp.split(jax.nn.sigmoid(gates), 2, axis=-1)
        merged = g_attn * (y_attn @ w_proj_attn[layer]) + g_ssm * (y_ssm @ w_proj_ssm[layer])
        x = x + gate1[:, None, :] * (merged @ w_out[layer])
        h = modulate(rms_norm(x, norm2_g[layer]), shift2, scale2)
        x = x + gate2[:, None, :] * hier_moe(h, w_router_group[layer], b_router_group[layer],
                                             w_router_expert[layer], b_router_expert[layer],
                                             w_expert_in[layer], w_expert_out[layer])
    return rms_norm(x, final_g)
```

```python
import math
import functools
import contextlib
import numpy as np
import concourse.bass as bass
import concourse.mybir as mybir
from concourse.bass_utils import run_bass_kernel_spmd

F32 = mybir.dt.float32
BF16 = mybir.dt.bfloat16
AF = mybir.ActivationFunctionType
ALU = mybir.AluOpType
AX = mybir.AxisListType

D = 1024
NCORES = 8
NEG = -1e30
LAMBDA_INIT = 0.8 - 0.6 * math.exp(-0.3 * 0)


class KB:
    def __init__(self, nc, es):
        self.nc = nc
        self.es = es
        self.eng = dict(pe=nc.tensor, act=nc.scalar, dve=nc.vector, pool=nc.gpsimd, sp=nc.sync)
        self.sem = {e: es.enter_context(nc.semaphore("s_" + e)) for e in ("pe", "act", "dve", "pool")}
        self.tick = {e: 0 for e in self.sem}
        self.seen = {e: {} for e in self.eng}
        self.res = {}
        self.dsem = {}
        self.dfree = []
        self.dfree_sw = []
        self.nd = 0
        self.nins = 0

    def _wait(self, e, ev, raw, isdma=False):
        sem, val, src = ev
        if (not isdma) and src == e and (e == "pe" or not raw):
            return
        if self.seen[e].get(sem, 0) >= val:
            return
        self.eng[e].wait_ge(sem, val)
        self.seen[e][sem] = val

    def _deps(self, e, reads, writes, isdma=False):
        for k in reads:
            r = self.res.get(k)
            if r and r[0]:
                self._wait(e, r[0], True, isdma)
        for k in writes:
            r = self.res.get(k)
            if r:
                if r[0]:
                    self._wait(e, r[0], False, isdma)
                for ev in r[1].values():
                    self._wait(e, ev, False, isdma)

    def _reg(self, ev, reads, writes):
        for k in reads:
            r = self.res.setdefault(k, [None, {}])
            r[1][ev[0]] = ev
        for k in writes:
            self.res[k] = [ev, {}]

    def op(self, e, fn, reads=(), writes=()):
        self._deps(e, reads, writes)
        ins = fn(self.eng[e])
        self.tick[e] += 1
        ins.then_inc(self.sem[e], 1)
        self._reg((self.sem[e], self.tick[e], e), reads, writes)
        self.nins += 1

    def dma(self, q, out, in_, key, reads=(), writes=(), **kw):
        self._deps(q, reads, writes, isdma=True)
        ins = self.eng[q].dma_start(out=out, in_=in_, **kw)
        self._finish_dma(ins, key, q == "pool", reads, writes)

    def _finish_dma(self, ins, key, sw, reads, writes):
        k2 = (key, sw)
        d = self.dsem.get(k2)
        if d is None:
            fl = self.dfree_sw if sw else self.dfree
            if fl:
                d = fl.pop()
            else:
                self.nd += 1
                d = [self.es.enter_context(self.nc.semaphore("d%d" % self.nd)), 0, sw]
            self.dsem[k2] = d
        d[1] += 16
        ins.then_inc(d[0], 16)
        self._reg((d[0], d[1], "dma"), reads, writes)
        self.nins += 1

    def idma(self, out, out_off, in_, in_off, key, reads=(), writes=(), **kw):
        self._deps("pool", reads, writes, isdma=True)
        ins = self.eng["pool"].indirect_dma_start(out=out, out_offset=out_off, in_=in_, in_offset=in_off, **kw)
        self._finish_dma(ins, key, True, reads, writes)

    def barrier(self):
        evs = [(self.sem[e], self.tick[e], e) for e in self.sem if self.tick[e] > 0]
        evs += [(d[0], d[1], "dma") for d in self.dsem.values() if d[1] > 0]
        for e in self.eng:
            for ev in evs:
                self._wait(e, ev, True, isdma=True)
        self.res = {}
        for d in self.dsem.values():
            (self.dfree_sw if d[2] else self.dfree).append(d)
        self.dsem = {}


class Rec:
    def __init__(self):
        self.L = []

    def op(self, e, fn, reads=(), writes=()):
        self.L.append((e, fn, list(reads), list(writes)))


def emit_interleaved(kb, a, b):
    ia = ib = 0
    while ia < len(a) or ib < len(b):
        if ia < len(a):
            kb.op(*a[ia])
            ia += 1
        if ib < len(b):
            kb.op(*b[ib])
            ib += 1


def build(S, NS, debug=None):
    nc = bass.Bass("TRN2", target_bir_lowering=False)
    NT = S // 128
    NG = S // 512
    dbg = debug or ()

    def din(name, shape):
        return nc.dram_tensor(name, shape, F32, kind="ExternalInput").ap()

    x = din("x", [NS, S, D])
    c = din("c", [NS, D])
    w_ada = din("w_ada", [D, 6 * D])
    b_ada = din("b_ada", [1, 6 * D])
    norm1_g = din("norm1_g", [1, D])
    w_in = din("w_in", [D, 4096])
    rel_bias = din("rel_bias", [32, 4])
    lam_q1 = din("lambda_q1", [1, 64])
    lam_k1 = din("lambda_k1", [1, 64])
    lam_q2 = din("lambda_q2", [1, 64])
    lam_k2 = din("lambda_k2", [1, 64])
    subln_g = din("subln_g", [1, 128])
    ssm_lre = din("ssm_lambda_re", [32, 64])
    ssm_lim = din("ssm_lambda_im", [32, 64])
    ssm_ls = din("ssm_log_step", [32, 1])
    ssm_bre = din("ssm_b_re", [32, 64, 16])
    ssm_bim = din("ssm_b_im", [32, 64, 16])
    ssm_cre = din("ssm_c_re", [32, 16, 64])
    ssm_cim = din("ssm_c_im", [32, 16, 64])
    ssm_d = din("ssm_d", [512, 1])
    w_glu = din("w_glu", [512, 1024])
    w_pa = din("w_proj_attn", [512, 1024])
    w_ps = din("w_proj_ssm", [512, 1024])
    w_out = din("w_out", [1024, 1024])
    norm2_g = din("norm2_g", [1, D])
    w_rg = din("w_router_group", [1024, 4])
    b_rg = din("b_router_group", [1, 4])
    w_re = din("w_router_expert", [4, 1024, 8])
    b_re = din("b_router_expert", [1, 32])
    w_ein = din("w_expert_in", [32, 1024, 1024])
    w_eout = din("w_expert_out", [32, 512, 1024])
    final_g = din("final_g", [1, D])
    relmask = din("relmask", [33, 2 * 128 * 128])
    y = nc.dram_tensor("y", [NS, S, D], F32, kind="ExternalOutput").ap()

    def scratch(name, shape, dt):
        kind = "ExternalOutput" if name in dbg else "Internal"
        return nc.dram_tensor(name, shape, dt, kind=kind).ap()

    qT_s = scratch("qT_s", [NS, 4, 128, S], BF16)
    kT_s = scratch("kT_s", [NS, 4, 128, S], BF16)
    v_s = scratch("v_s", [NS, S, 512], BF16)
    uT_s = scratch("uT_s", [NS, 4, 128, S], F32)
    sg_s = scratch("sg_s", [NS, 16, 128, S], BF16)
    mod_s = scratch("mod_s", [128, 48 * NS], F32)
    tab_s = scratch("tab_s", [4, 2 * 128 * 128], F32)
    gate_s = scratch("gate_s", [128, NS * 4 * D], F32)
    xmid_s = scratch("xmid_s", [NS, S, D], F32)
    h2T_s = scratch("h2T_s", [NS, 8, 128, S], BF16)
    comb_s = scratch("comb_s", [NS, S, 32], F32)
    TT_ = NS * S
    BLK = 512
    NB = 2 * TT_ // BLK + 32
    rt_s = scratch("rt_s", [TT_, 8], F32)
    h2tok_s = scratch("h2tok_s", [TT_, D], F32)
    rowtab_s = scratch("rowtab_s", [NB * BLK, 4], F32)
    ys_s = scratch("ys_s", [2 * TT_ + 128, D], F32)
    yaT_s = scratch("yaT_s", [NS, 4, 128, S], BF16)
    ysT_s = scratch("ysT_s", [NS, 4, 128, S], BF16)

    with contextlib.ExitStack() as es:
        kb = KB(nc, es)

        def SB(name, shape, dt, stack=es):
            return stack.enter_context(nc.sbuf_tensor(name, shape, dt))

        ps = [es.enter_context(nc.psum_tensor("ps%d" % i, [128, 512], F32)) for i in range(8)]

        ident = SB("ident", [128, 128], F32)
        ones = SB("ones", [128, 128], F32)
        modT = SB("modT", [128, 48, NS], F32)
        Tri = SB("Tri", [128, 128], F32)
        cnt_bc = SB("cnt_bc", [128, 64], F32)
        widx_in = SB("widx_in", [128, NB, 8], mybir.dt.int32)
        widx_out = SB("widx_out", [128, NB, 4], mybir.dt.int32)

        with contextlib.ExitStack() as p0:
            io = SB("io", [128, 128], F32, p0)
            cT = SB("cT", [128, 8, NS], F32, p0)
            sc = SB("sc", [128, 8, NS], F32, p0)
            sc_bc = SB("sc_bc", [128, NS, 8, 128], F32, p0)
            b_adaT = SB("b_adaT", [128, 48], F32, p0)
            bada_bc = SB("bada_bc", [128, 4, D], F32, p0)
            wst = [SB("wst%d" % i, [128, 8, 512], F32, p0) for i in range(3)]
            gate_bc = SB("gate_bc", [128, NS, 4, D], F32, p0)

            kb.op("pool", lambda e: e.iota(io[:], pattern=[[1, 128]], base=0, channel_multiplier=-1,
                                           allow_small_or_imprecise_dtypes=True), writes=["io"])
            kb.op("dve", lambda e: e.tensor_single_scalar(out=ident[:], in_=io[:], scalar=0.0, op=ALU.is_equal),
                  reads=["io"], writes=["ident"])
            kb.op("dve", lambda e: e.memset(ones[:], 1.0), writes=["ones"])
            kb.op("dve", lambda e: e.tensor_single_scalar(out=Tri[:], in_=io[:], scalar=0.0, op=ALU.is_ge),
                  reads=["io"], writes=["Tri"])
            for b in range(NS):
                kb.dma("sp", cT[:, :, b], c[b].rearrange("(k p) -> p k", p=128), "cT", writes=["cT"],
                       allow_slow_non_contiguous=True)
            kb.dma("sp", b_adaT[:], b_ada.rearrange("o (j p) -> p (o j)", p=128), "b_adaT", writes=["b_adaT"],
                   allow_slow_non_contiguous=True)
            for gi_, c0_ in enumerate((2048, 5120, 3072, 4096)):
                kb.dma("sp", bada_bc[:, gi_, :], b_ada[:, c0_:c0_ + 1024].partition_broadcast(128), "bada_bc",
                       writes=["bada_bc"])
            kb.op("act", lambda e: e.activation(out=sc[:], in_=cT[:], func=AF.Silu), reads=["cT"], writes=["sc"])
            for b in range(NS):
                for k in range(8):
                    kb.op("dve", lambda e, b=b, k=k: e.tensor_scalar_mul(out=sc_bc[:, b, k, :], in0=ones[:],
                                                                       scalar1=sc[:, k, b:b + 1]),
                          reads=["sc", "ones"], writes=["sc_bc"])
            def p0_load(jb):
                kb.dma("sp" if jb % 2 == 0 else "pool", wst[jb % 3][:],
                       w_ada[:, jb * 512:(jb + 1) * 512].rearrange("(k p) n -> p k n", p=128), ("wst", jb % 3),
                       writes=[("wst", jb % 3)])

            p0_load(0)
            p0_load(1)
            for jb in range(12):
                wt = wst[jb % 3]
                wk = ("wst", jb % 3)
                if jb + 2 < 12:
                    p0_load(jb + 2)
                pb = ps[jb % 2]
                pk = ("ps", jb % 2)
                for m in range(4):
                    for k in range(8):
                        kb.op("pe", lambda e, m=m, k=k: e.matmul(pb[:, m * NS:(m + 1) * NS],
                                                                 lhsT=wt[:, k, m * 128:(m + 1) * 128],
                                                                 rhs=sc[:, k, :], start=(k == 0), stop=(k == 7)),
                              reads=[wk, "sc"], writes=[pk])
                for b in range(NS):
                    kb.op("dve", lambda e, b=b: e.tensor_tensor(
                        out=modT[:, jb * 4:(jb + 1) * 4, b],
                        in0=pb[:, 0:4 * NS].rearrange("p (m b) -> p m b", b=NS)[:, :, b],
                        in1=b_adaT[:, jb * 4:(jb + 1) * 4], op=ALU.add),
                          reads=[pk, "b_adaT"], writes=["modT"])
                if jb in (4, 5, 10, 11, 6, 7, 8, 9):
                    gi = {4: 0, 5: 0, 10: 1, 11: 1, 6: 2, 7: 2, 8: 3, 9: 3}[jb]
                    half = jb % 2
                    for b in range(NS):
                        pg = ps[2 + b % 2]
                        pgk = ("ps", 2 + b % 2)
                        for k in range(8):
                            kb.op("pe", lambda e, k=k, b=b: e.matmul(pg[:], lhsT=sc_bc[:, b, k, :], rhs=wt[:, k, :],
                                                                     start=(k == 0), stop=(k == 7)),
                                  reads=[wk, "sc_bc"], writes=[pgk])
                        kb.op("dve", lambda e, b=b: e.tensor_tensor(
                            out=gate_bc[:, b, gi, half * 512:(half + 1) * 512], in0=pg[:],
                            in1=bada_bc[:, gi, half * 512:(half + 1) * 512], op=ALU.add),
                              reads=[pgk, "bada_bc"], writes=["gate_bc"])
            for lo in (8, 32):
                kb.op("dve", lambda e, lo=lo: e.tensor_scalar_add(out=modT[:, lo:lo + 8, :], in0=modT[:, lo:lo + 8, :],
                                                                  scalar1=1.0),
                      reads=["modT"], writes=["modT"])
            for b in range(NS):
                kb.op("dve", lambda e: e.tensor_scalar_add(out=gate_bc[:, b, 3, :], in0=gate_bc[:, b, 3, :], scalar1=1.0),
                      reads=["gate_bc"], writes=["gate_bc"])
            kb.dma("sp", gate_s, gate_bc[:].rearrange("p b g d -> p (b g d)"), "gate_st", reads=["gate_bc"])
            if "mod_s" in dbg:
                kb.dma("sp", mod_s, modT[:].rearrange("p j b -> p (j b)"), "modT_st", reads=["modT"])
            kb.barrier()

        with contextlib.ExitStack() as p1:
            winb = SB("winb", [128, 8, 4096], BF16, p1)
            g1bc = SB("g1bc", [128, D], F32, p1)
            kb.dma("sp", g1bc[:], norm1_g.partition_broadcast(128), "g1bc", writes=["g1bc"])
            xt = [SB("xt%d" % i, [128, D], F32, p1) for i in range(2)]
            junk = SB("junk", [128, D], F32, p1)
            xn = [SB("xn%d" % i, [128, D], F32, p1) for i in range(2)]
            ss = SB("ss", [128, 8], F32, p1)
            hT = [SB("hT%d" % i, [128, 8, 512], BF16, p1) for i in range(2)]
            stq = [SB("stq%d" % i, [128, 24, 512], BF16, p1) for i in range(2)]
            stu = [SB("stu%d" % i, [128, 4, 512], F32, p1) for i in range(2)]
            stv = [SB("stv%d" % i, [128, 4, 512], BF16, p1) for i in range(2)]
            for k in range(8):
                kb.dma("pool", winb[:, k, :], w_in[k * 128:(k + 1) * 128, :], ("winb", k), writes=[("winb", k)])
            tcnt = 0
            pcnt = 0
            ecnt = 0
            for b in range(NS):
                for g in range(NG):
                    gs = (b * NG + g) % 2
                    hk = ("hT", gs)
                    for t in range(4):
                        tok0 = g * 512 + t * 128
                        sl = tcnt % 2
                        tcnt += 1
                        kb.dma("sp", xt[sl][:], x[b, tok0:tok0 + 128, :], ("xt", sl), writes=[("xt", sl)])
                        kb.op("act", lambda e: e.activation(out=junk[:], in_=xt[sl][:], func=AF.Square,
                                                            accum_out=ss[:, sl:sl + 1]),
                              reads=[("xt", sl)], writes=["junk", ("ss", sl)])
                        kb.op("dve", lambda e: e.tensor_scalar(out=ss[:, 2 + sl:3 + sl], in0=ss[:, sl:sl + 1],
                                                               scalar1=1.0 / D, scalar2=1e-6, op0=ALU.mult,
                                                               op1=ALU.add),
                              reads=[("ss", sl)], writes=[("ss2", sl)])
                        kb.op("act", lambda e: e.activation(out=ss[:, 6 + sl:7 + sl], in_=ss[:, 2 + sl:3 + sl],
                                                            func=AF.Sqrt),
                              reads=[("ss2", sl)], writes=[("ss3", sl)])
                        kb.op("dve", lambda e: e.reciprocal(out=ss[:, 4 + sl:5 + sl], in_=ss[:, 6 + sl:7 + sl]),
                              reads=[("ss3", sl)], writes=[("rstd", sl)])
                        kb.op("dve", lambda e: e.scalar_tensor_tensor(out=xn[sl][:], in0=xt[sl][:],
                                                                      scalar=ss[:, 4 + sl:5 + sl], in1=g1bc[:],
                                                                      op0=ALU.mult, op1=ALU.mult),
                              reads=[("xt", sl), ("rstd", sl), "g1bc"], writes=[("xn", sl)])
                        for k in range(8):
                            pb = ps[sl * 2 + k // 4]
                            pk = ("ps", sl * 2 + k // 4)
                            kb.op("pe", lambda e, k=k, pb=pb: e.transpose(pb[:, (k % 4) * 128:(k % 4 + 1) * 128],
                                                                          xn[sl][:, k * 128:(k + 1) * 128], ident[:]),
                                  reads=[("xn", sl)], writes=[pk])
                        for k in range(8):
                            pb = ps[sl * 2 + k // 4]
                            pk = ("ps", sl * 2 + k // 4)
                            kb.op("act", lambda e, k=k, pb=pb: e.activation(
                                out=hT[gs][:, k, t * 128:(t + 1) * 128],
                                in_=pb[:, (k % 4) * 128:(k % 4 + 1) * 128], func=AF.Identity,
                                scale=modT[:, 8 + k, b:b + 1], bias=modT[:, k, b:b + 1]),
                                  reads=[pk], writes=[hk])
                    for t in range(4):
                        pi = 4 + pcnt % 4
                        pcnt += 1
                        for k in range(8):
                            kb.op("pe", lambda e, k=k: e.matmul(ps[pi][:], lhsT=hT[gs][:, k, t * 128:(t + 1) * 128],
                                                                rhs=winb[:, k, 1024:1536], start=(k == 0),
                                                                stop=(k == 7)),
                                  reads=[hk, ("winb", k)], writes=[("ps", pi)])
                        kb.op("dve", lambda e: e.tensor_copy(out=stv[gs][:, t, :], in_=ps[pi][:]),
                              reads=[("ps", pi)], writes=[("stv", gs, t)])
                    for m in list(range(0, 8)) + list(range(12, 32)):
                        pi = 4 + pcnt % 4
                        pcnt += 1
                        for k in range(8):
                            kb.op("pe", lambda e, k=k: e.matmul(ps[pi][:], lhsT=winb[:, k, m * 128:(m + 1) * 128],
                                                                rhs=hT[gs][:, k, :], start=(k == 0), stop=(k == 7)),
                                  reads=[hk, ("winb", k)], writes=[("ps", pi)])
                        if m < 8:
                            eng = "dve" if (ecnt % 2 == 0) else "act"
                            ecnt += 1
                            if eng == "dve":
                                kb.op("dve", lambda e: e.tensor_copy(out=stq[gs][:, m, :], in_=ps[pi][:]),
                                      reads=[("ps", pi)], writes=[("stq", gs, m)])
                            else:
                                kb.op("act", lambda e: e.copy(out=stq[gs][:, m, :], in_=ps[pi][:]),
                                      reads=[("ps", pi)], writes=[("stq", gs, m)])
                        elif m < 16:
                            kb.op("dve", lambda e: e.tensor_copy(out=stu[gs][:, m - 12, :], in_=ps[pi][:]),
                                  reads=[("ps", pi)], writes=[("stu", gs, m - 12)])
                        else:
                            kb.op("act", lambda e: e.activation(out=stq[gs][:, m - 8, :], in_=ps[pi][:],
                                                                func=AF.Sigmoid),
                                  reads=[("ps", pi)], writes=[("stq", gs, m - 8)])
                    c0 = g * 512
                    kb.dma("pool", qT_s[b, :, :, c0:c0 + 512].rearrange("m p t -> p m t"), stq[gs][:, 0:4, :],
                           ("stq", gs), reads=[("stq", gs, m) for m in range(0, 4)])
                    kb.dma("pool", kT_s[b, :, :, c0:c0 + 512].rearrange("m p t -> p m t"), stq[gs][:, 4:8, :],
                           ("stq", gs), reads=[("stq", gs, m) for m in range(4, 8)])
                    kb.dma("pool", sg_s[b, :, :, c0:c0 + 512].rearrange("m p t -> p m t"), stq[gs][:, 8:24, :],
                           ("stq", gs), reads=[("stq", gs, m) for m in range(8, 24)])
                    kb.dma("pool", uT_s[b, :, :, c0:c0 + 512].rearrange("m p t -> p m t"), stu[gs][:],
                           ("stu", gs), reads=[("stu", gs, m) for m in range(4)])
                    kb.dma("pool", v_s[b, c0:c0 + 512, :].rearrange("(t p) n -> p t n", p=128), stv[gs][:],
                           ("stv", gs), reads=[("stv", gs, t) for t in range(4)])
            kb.barrier()

        with contextlib.ExitStack() as p2:
            rbx = SB("rbx", [33, 4], F32, p2)
            rb32 = SB("rb32", [32, 4], F32, p2)
            b31_32 = SB("b31_32", [32, 4], F32, p2)
            b31_bc = SB("b31_bc", [128, 4], F32, p2)
            rmk = [SB("rmk%d" % i, [33, 4096], F32, p2) for i in range(2)]
            tabst = [SB("tabst%d" % i, [4, 4096], F32, p2) for i in range(2)]
            T8 = SB("T8", [128, 8, 128], F32, p2)
            E8 = SB("E8", [128, 8, 128], F32, p2)
            lamt = SB("lamt", [128, 4, 64], F32, p2)
            lams = SB("lams", [128, 8], F32, p2)
            gsub = SB("gsub", [128, 128], F32, p2)
            kT = SB("kT", [128, 4, S], BF16, p2)
            qT = SB("qT", [128, 4, S], BF16, p2)
            vsb = SB("vsb", [128, NT, 4, 130], BF16, p2)
            pT = [SB("pT%d" % i, [128, 512], BF16, p2) for i in range(3)]
            osb = [SB("osb%d" % i, [128, 512], F32, p2) for i in range(2)]
            sm = [SB("sm%d" % i, [128, 16], F32, p2) for i in range(2)]
            ojunk = SB("ojunk", [128, 128], F32, p2)
            yst = [SB("yst%d" % i, [128, 4, 512], BF16, p2) for i in range(2)]

            kb.op("dve", lambda e: e.memset(rbx[:], -8e30), writes=["rbx"])
            kb.dma("sp", rb32[:], rel_bias, "rb32", writes=["rb32"])
            kb.dma("sp", b31_32[:], rel_bias[31:32, :].partition_broadcast(32), "b31_32", writes=["b31_32"])
            kb.dma("sp", b31_bc[:], rel_bias[31:32, :].partition_broadcast(128), "b31_bc", writes=["b31_bc"])
            kb.op("dve", lambda e: e.tensor_tensor(out=rb32[:], in0=rb32[:], in1=b31_32[:], op=ALU.subtract),
                  reads=["rb32", "b31_32"], writes=["rb32"])
            kb.op("dve", lambda e: e.tensor_scalar_mul(out=rbx[0:32, :], in0=rb32[:], scalar1=8.0),
                  reads=["rb32", "rbx"], writes=["rbx"])
            for pc in range(8):
                rk = ("rmk", pc % 2)
                kb.dma("sp", rmk[pc % 2][:], relmask[:, pc * 4096:(pc + 1) * 4096], rk, writes=[rk])
                for n in range(8):
                    pi = n % 4
                    kb.op("pe", lambda e: e.matmul(ps[pi][0:4, :], lhsT=rbx[:, :], rhs=rmk[pc % 2][:, n * 512:(n + 1) * 512],
                                                   start=True, stop=True),
                          reads=["rbx", rk], writes=[("ps", pi)])
                    kb.op("dve", lambda e: e.tensor_copy(out=tabst[pc % 2][:, n * 512:(n + 1) * 512], in_=ps[pi][0:4, :]),
                          reads=[("ps", pi)], writes=[("tabst", pc % 2, n)])
                kb.dma("sp", tab_s[:, pc * 4096:(pc + 1) * 4096], tabst[pc % 2][:], ("tabst", pc % 2),
                       reads=[("tabst", pc % 2, n) for n in range(8)])
            for i, la in enumerate((lam_q1, lam_k1, lam_q2, lam_k2)):
                kb.dma("sp", lamt[:, i, :], la.partition_broadcast(128), "lamt", writes=["lamt"])
            kb.dma("sp", gsub[:], subln_g.partition_broadcast(128), "gsub", writes=["gsub"])
            kb.op("dve", lambda e: e.tensor_scalar_mul(out=gsub[:], in0=gsub[:], scalar1=1.0 - LAMBDA_INIT),
                  reads=["gsub"], writes=["gsub"])
            for i in range(2):
                kb.op("dve", lambda e: e.tensor_tensor(out=lamt[:, 2 * i, :], in0=lamt[:, 2 * i, :],
                                                       in1=lamt[:, 2 * i + 1, :], op=ALU.mult),
                      reads=["lamt"], writes=["lamt"])
                kb.op("dve", lambda e: e.reduce_sum(out=lams[:, i:i + 1], in_=lamt[:, 2 * i, :], axis=AX.X),
                      reads=["lamt"], writes=["lams"])
            kb.op("act", lambda e: e.activation(out=lams[:, 2:4], in_=lams[:, 0:2], func=AF.Exp),
                  reads=["lams"], writes=["lams"])
            kb.op("dve", lambda e: e.tensor_tensor(out=lams[:, 4:5], in0=lams[:, 3:4], in1=lams[:, 2:3], op=ALU.subtract),
                  reads=["lams"], writes=["lams"])
            kb.op("dve", lambda e: e.tensor_scalar_add(out=lams[:, 5:6], in0=lams[:, 4:5], scalar1=-LAMBDA_INIT),
                  reads=["lams"], writes=["lams"])
            kb.barrier()
            kb.dma("sp", T8[:], tab_s.rearrange("h (b k q) -> k (h b) q", b=2, k=128), "T8", writes=["T8"])
            kb.op("act", lambda e: e.activation(out=E8[:], in_=T8[:], func=AF.Exp, scale=0.125), reads=["T8"],
                  writes=["E8"])
            kb.op("dve", lambda e: e.memset(vsb[:, :, :, 128:130], 1.0), writes=["vones"])

            scnt = 0
            ptc = 0
            pend = []
            LOOK = 2

            def flush(keep):
                while len(pend) > keep:
                    pend.pop(0)()

            def emit_qk_exp(i, h, m, j0, nb, si, pt, ptk):
                for jj in range(nb):
                    j = j0 + jj
                    near = (j >= i - 1)
                    kb.op("pe", lambda e: e.matmul(
                        ps[si][:, jj * 128:(jj + 1) * 128],
                        lhsT=kT[m * 64:(m + 1) * 64, h, j * 128:(j + 1) * 128],
                        rhs=qT[m * 64:(m + 1) * 64, h, i * 128:(i + 1) * 128],
                        start=True, stop=True),
                          reads=[("kT", h), ("qT", h)], writes=[("ps", si)])
                kb.op("act", lambda e: e.activation(out=pt[:, 0:nb * 128], in_=ps[si][:, 0:nb * 128],
                                                    func=AF.Exp, scale=0.125, bias=b31_bc[:, h:h + 1]),
                      reads=[("ps", si), "b31_bc"], writes=[ptk])
                for jj in range(nb):
                    j = j0 + jj
                    if j >= i - 1:
                        kb.op("dve", lambda e: e.tensor_tensor(out=pt[:, jj * 128:(jj + 1) * 128],
                                                               in0=pt[:, jj * 128:(jj + 1) * 128],
                                                               in1=E8[:, h * 2 + (i - j), :], op=ALU.mult),
                              reads=[ptk, "E8"], writes=[ptk])

            def emit_pv(i, h, m, j0, nb, pt, ptk):
                pob = ps[4 + h % 2]
                pok = ("ps", 4 + h % 2)
                for jj in range(nb):
                    j = j0 + jj
                    kb.op("pe", lambda e: e.matmul(
                        pob[:, m * 256:m * 256 + 129], lhsT=pt[:, jj * 128:(jj + 1) * 128],
                        rhs=vsb[:, j, h, 0:129], start=(j == 0), stop=(j == i)),
                          reads=[ptk, ("vsb", h), "vones"], writes=[pok])

            def emit_fin(i, h):
                ob = osb[i % 2]
                ok = ("osb", i % 2)
                smt = sm[i % 2]
                smk = ("sm", i % 2)
                pob = ps[4 + h % 2]
                pok = ("ps", 4 + h % 2)
                kb.op("dve", lambda e: e.reciprocal(out=smt[:, 0:1], in_=pob[:, 128:129]),
                      reads=[pok], writes=[smk])
                kb.op("dve", lambda e: e.reciprocal(out=smt[:, 1:2], in_=pob[:, 384:385]),
                      reads=[pok], writes=[smk])
                kb.op("dve", lambda e: e.tensor_tensor(out=smt[:, 2:3], in0=smt[:, 1:2], in1=lams[:, 5:6],
                                                       op=ALU.mult),
                      reads=[smk, "lams"], writes=[smk])
                kb.op("dve", lambda e: e.tensor_scalar_mul(out=ob[:, h * 128:(h + 1) * 128], in0=pob[:, 0:128],
                                                           scalar1=smt[:, 0:1]),
                      reads=[pok, smk], writes=[ok])
                kb.op("dve", lambda e: e.scalar_tensor_tensor(out=ob[:, h * 128:(h + 1) * 128],
                                                              in0=pob[:, 256:384], scalar=smt[:, 2:3],
                                                              in1=ob[:, h * 128:(h + 1) * 128],
                                                              op0=ALU.mult, op1=ALU.add),
                      reads=[pok, smk, ok], writes=[ok])
                kb.op("act", lambda e: e.activation(out=ojunk[:], in_=ob[:, h * 128:(h + 1) * 128],
                                                    func=AF.Square, accum_out=smt[:, 4 + h:5 + h]),
                      reads=[ok], writes=["ojunk", (smk, "ss")])

            def emit_post(b, i):
                ob = osb[i % 2]
                ok = ("osb", i % 2)
                smt = sm[i % 2]
                smk = ("sm", i % 2)
                kb.op("dve", lambda e: e.tensor_scalar(out=smt[:, 8:12], in0=smt[:, 4:8], scalar1=1.0 / 128,
                                                       scalar2=1e-5, op0=ALU.mult, op1=ALU.add),
                      reads=[(smk, "ss")], writes=[(smk, "v")])
                kb.op("act", lambda e: e.activation(out=smt[:, 12:16], in_=smt[:, 8:12], func=AF.Ln),
                      reads=[(smk, "v")], writes=[(smk, "ln")])
                kb.op("act", lambda e: e.activation(out=smt[:, 8:12], in_=smt[:, 12:16], func=AF.Exp, scale=-0.5),
                      reads=[(smk, "ln")], writes=[(smk, "v")])
                for h in range(4):
                    kb.op("dve", lambda e: e.scalar_tensor_tensor(out=ob[:, h * 128:(h + 1) * 128],
                                                                  in0=ob[:, h * 128:(h + 1) * 128],
                                                                  scalar=smt[:, 8 + h:9 + h], in1=gsub[:],
                                                                  op0=ALU.mult, op1=ALU.mult),
                          reads=[ok, (smk, "v"), "gsub"], writes=[ok])
                ysl = (i // 4) % 2
                for h in range(4):
                    kb.op("pe", lambda e: e.transpose(ps[6][:, h * 128:(h + 1) * 128], ob[:, h * 128:(h + 1) * 128],
                                                      ident[:]),
                          reads=[ok], writes=[("ps", 6)])
                kb.op("act", lambda e: e.copy(out=yst[ysl][:, :, (i % 4) * 128:(i % 4 + 1) * 128],
                                              in_=ps[6][:].rearrange("p (h q) -> p h q", h=4)),
                      reads=[("ps", 6)], writes=[("yst", ysl, i % 4)])
                if i % 4 == 3:
                    c0 = (i // 4) * 512
                    kb.dma("pool", yaT_s[b, :, :, c0:c0 + 512].rearrange("m p t -> p m t"), yst[ysl][:],
                           ("yst", ysl), reads=[("yst", ysl, t) for t in range(4)])

            for b in range(NS):
                flush(0)
                for h in range(4):
                    kb.dma("sp", kT[:, h, :], kT_s[b, h], ("kT", h), writes=[("kT", h)])
                    kb.dma("sp", qT[:, h, :], qT_s[b, h], ("qT", h), writes=[("qT", h)])
                for h in range(4):
                    kb.dma("sp", vsb[:, :, h, 0:128],
                           v_s[b, :, h * 128:(h + 1) * 128].rearrange("(t p) d -> p t d", p=128), ("vsb", h),
                           writes=[("vsb", h)])
                for i in range(NT):
                    for h in range(4):
                        for m in range(2):
                            j0 = 0
                            while j0 <= i:
                                nb = min(4, i + 1 - j0)
                                si = scnt % 4
                                scnt += 1
                                pt = pT[ptc % 3]
                                ptk = ("pT", ptc % 3)
                                ptc += 1
                                emit_qk_exp(i, h, m, j0, nb, si, pt, ptk)
                                pend.append(functools.partial(emit_pv, i, h, m, j0, nb, pt, ptk))
                                flush(LOOK)
                                j0 += nb
                        pend.append(functools.partial(emit_fin, i, h))
                    pend.append(functools.partial(emit_post, b, i))
            flush(0)
            kb.barrier()

        with contextlib.ExitStack() as p3:
            TWO_PI = 2.0 * math.pi
            prm = SB("prm", [128, 24, 16], F32, p3)
            Bpad = SB("Bpad", [128, 2, 16, 128], F32, p3)
            Cpad = SB("Cpad", [128, 3, 16, 128], F32, p3)
            dcol = SB("dcol", [128, 4], F32, p3)
            Dg = SB("Dg", [128, 4, 128], F32, p3)
            wglub = SB("wglub", [128, 4, 1024], BF16, p3)
            carry = SB("carry", [128, 16, 2], F32, p3)
            cs_tab = SB("cs_tab", [128, 16, 512], F32, p3)
            sn_tab = SB("sn_tab", [128, 16, 512], F32, p3)
            ctmp = SB("ctmp", [128, 4], F32, p3)
            p3s = contextlib.ExitStack()
            bnat = SB("bnat", [128, 2, 16, 16], F32, p3s)
            bbT = SB("bbT", [128, 2, 16, 16], F32, p3s)
            bbX = SB("bbX", [128, 128], F32, p3s)
            CT = SB("CT", [128, 2, 16, 16], F32, p3s)
            negpi = SB("negpi", [128, 1], F32, p3s)
            tpos = SB("tpos", [128, 512], F32, p3s)
            tq_ = SB("tq_", [128, 512], F32, p3s)
            tn_ = SB("tn_", [128, 512], F32, p3s)
            tc_ = SB("tc_", [128, 512], F32, p3s)
            tph = SB("tph", [128, 512], F32, p3s)

            P = lambda i: prm[:, i, :]
            LRE, LIM, STEP, MAG, ANG, ARE, AIM, DEN, NRE, CRE, CIM, FT, TA, TB, TC, F512, S512, C512 = range(18)
            kb.dma("sp", P(LRE), ssm_lre.rearrange("(gt gi) p -> gi p gt", gi=2)[0], "prm0", writes=["prm"],
                   allow_slow_non_contiguous=True) if False else None
            for gi in range(2):
                kb.dma("sp", prm[gi * 64:(gi + 1) * 64, LRE, :], ssm_lre.rearrange("(gt gi) p -> gi p gt", gi=2)[gi],
                       "prm", writes=["prm"], allow_slow_non_contiguous=True)
                kb.dma("sp", prm[gi * 64:(gi + 1) * 64, LIM, :], ssm_lim.rearrange("(gt gi) p -> gi p gt", gi=2)[gi],
                       "prm", writes=["prm"], allow_slow_non_contiguous=True)
                kb.dma("sp", prm[gi * 64:(gi + 1) * 64, STEP, :],
                       ssm_ls.rearrange("(gt gi) o -> gi (o gt)", gi=2)[gi:gi + 1, :].partition_broadcast(64),
                       "prm", writes=["prm"], allow_slow_non_contiguous=True)
                for ri, src in enumerate((ssm_bre, ssm_bim)):
                    kb.dma("sp", bnat[gi * 64:(gi + 1) * 64, ri, :, :],
                           src.rearrange("(gt gi) p h -> gi p gt h", gi=2)[gi], "bnat", writes=["bnat"])
                for ri, src in enumerate((ssm_cre, ssm_cim)):
                    for gt in range(16):
                        kb.dma("sp", CT[gi * 64:(gi + 1) * 64, ri, gt, :],
                               src[2 * gt + gi].rearrange("h p -> p h"), "CT", writes=["CT"],
                               allow_slow_non_contiguous=True)
            kb.dma("sp", dcol[:], ssm_d.rearrange("(cc p) o -> p (cc o)", p=128), "dcol", writes=["dcol"],
                   allow_slow_non_contiguous=True)
            for k in range(4):
                kb.dma("pool", wglub[:, k, :], w_glu[k * 128:(k + 1) * 128, :], ("wglub", k), writes=[("wglub", k)])
            kb.op("pool", lambda e: e.iota(tpos[:], pattern=[[1, 512]], base=0, channel_multiplier=0,
                                           allow_small_or_imprecise_dtypes=True), writes=["tpos"])
            kb.op("dve", lambda e: e.memset(negpi[:], -math.pi), writes=["negpi"])

            def V(fn, rd=("prm",), wr=("prm",)):
                kb.op("dve", fn, reads=list(rd), writes=list(wr))
            V(lambda e: e.tensor_scalar_min(out=P(LRE), in0=P(LRE), scalar1=-1e-4))
            kb.op("act", lambda e: e.activation(out=P(STEP), in_=P(STEP), func=AF.Exp), reads=["prm"], writes=["prm"])
            V(lambda e: e.tensor_tensor(out=P(TA), in0=P(LRE), in1=P(STEP), op=ALU.mult))
            kb.op("act", lambda e: e.activation(out=P(MAG), in_=P(TA), func=AF.Exp), reads=["prm"], writes=["prm"])
            V(lambda e: e.tensor_tensor(out=P(ANG), in0=P(LIM), in1=P(STEP), op=ALU.mult))
            MAGIC = 12582912.0

            def sincos(out_sin, out_cos, turns, tq, tn, tc, rk, wk):
                kb.op("pool", lambda e: e.tensor_scalar_add(out=tq, in0=turns, scalar1=MAGIC), reads=rk, writes=wk)
                kb.op("dve", lambda e: e.scalar_tensor_tensor(out=tn, in0=tq, scalar=MAGIC, in1=turns,
                                                              op0=ALU.subtract, op1=ALU.subtract), reads=rk + wk, writes=wk)
                kb.op("act", lambda e: e.activation(out=out_sin, in_=tn, func=AF.Sin, scale=-TWO_PI), reads=wk, writes=wk)
                kb.op("pool", lambda e: e.tensor_scalar_add(out=tc, in0=turns, scalar1=0.25), reads=rk + wk, writes=wk)
                kb.op("pool", lambda e: e.tensor_scalar_add(out=tq, in0=tc, scalar1=MAGIC), reads=wk, writes=wk)
                kb.op("dve", lambda e: e.scalar_tensor_tensor(out=tn, in0=tq, scalar=MAGIC, in1=tc,
                                                              op0=ALU.subtract, op1=ALU.subtract), reads=wk, writes=wk)
                kb.op("act", lambda e: e.activation(out=out_cos, in_=tn, func=AF.Sin, scale=-TWO_PI), reads=wk, writes=wk)

            V(lambda e: e.tensor_scalar_mul(out=P(FT), in0=P(ANG), scalar1=1.0 / TWO_PI))
            sincos(P(AIM), P(ARE), P(FT), P(TA), P(TB), P(TC), ["prm"], ["prm"])
            V(lambda e: e.tensor_scalar_mul(out=P(F512), in0=P(FT), scalar1=512.0))
            sincos(P(S512), P(C512), P(F512), P(TA), P(TB), P(TC), ["prm"], ["prm"])
            V(lambda e: e.tensor_tensor(out=P(AIM), in0=P(AIM), in1=P(MAG), op=ALU.mult))
            V(lambda e: e.tensor_tensor(out=P(ARE), in0=P(ARE), in1=P(MAG), op=ALU.mult))
            V(lambda e: e.tensor_tensor(out=P(DEN), in0=P(LRE), in1=P(LRE), op=ALU.mult))
            V(lambda e: e.tensor_tensor(out=P(TA), in0=P(LIM), in1=P(LIM), op=ALU.mult))
            V(lambda e: e.tensor_tensor(out=P(DEN), in0=P(DEN), in1=P(TA), op=ALU.add))
            V(lambda e: e.reciprocal(out=P(DEN), in_=P(DEN)))
            V(lambda e: e.tensor_scalar_add(out=P(NRE), in0=P(ARE), scalar1=-1.0))
            V(lambda e: e.tensor_tensor(out=P(TA), in0=P(NRE), in1=P(LRE), op=ALU.mult))
            V(lambda e: e.tensor_tensor(out=P(TB), in0=P(AIM), in1=P(LIM), op=ALU.mult))
            V(lambda e: e.tensor_tensor(out=P(TA), in0=P(TA), in1=P(TB), op=ALU.add))
            V(lambda e: e.tensor_tensor(out=P(CRE), in0=P(TA), in1=P(DEN), op=ALU.mult))
            V(lambda e: e.tensor_tensor(out=P(TA), in0=P(AIM), in1=P(LRE), op=ALU.mult))
            V(lambda e: e.tensor_tensor(out=P(TB), in0=P(NRE), in1=P(LIM), op=ALU.mult))
            V(lambda e: e.tensor_tensor(out=P(TA), in0=P(TA), in1=P(TB), op=ALU.subtract))
            V(lambda e: e.tensor_tensor(out=P(CIM), in0=P(TA), in1=P(DEN), op=ALU.mult))
            for gt in range(16):
                cre = prm[:, CRE, gt:gt + 1]
                cim = prm[:, CIM, gt:gt + 1]
                V(lambda e: e.tensor_scalar_mul(out=bbT[:, 0, gt, :], in0=bnat[:, 1, gt, :], scalar1=cim),
                  rd=("prm", "bnat"), wr=("bbT",))
                V(lambda e: e.scalar_tensor_tensor(out=bbT[:, 0, gt, :], in0=bnat[:, 0, gt, :], scalar=cre,
                                                   in1=bbT[:, 0, gt, :], op0=ALU.mult, op1=ALU.subtract),
                  rd=("prm", "bnat", "bbT"), wr=("bbT",))
                V(lambda e: e.tensor_scalar_mul(out=bbT[:, 1, gt, :], in0=bnat[:, 0, gt, :], scalar1=cim),
                  rd=("prm", "bnat"), wr=("bbT",))
                V(lambda e: e.scalar_tensor_tensor(out=bbT[:, 1, gt, :], in0=bnat[:, 1, gt, :], scalar=cre,
                                                   in1=bbT[:, 1, gt, :], op0=ALU.mult, op1=ALU.add),
                  rd=("prm", "bnat", "bbT"), wr=("bbT",))
            kb.op("pool", lambda e: e.memset(Cpad[:], 0.0), writes=["Cpad"])
            for gt in range(16):
                for ri in range(2):
                    V(lambda e: e.memset(bbX[:], 0.0), rd=(), wr=("bbX",))
                    for gi in range(2):
                        c0 = (gt % 4) * 32 + gi * 16
                        V(lambda e: e.tensor_copy(out=bbX[gi * 64:(gi + 1) * 64, c0:c0 + 16],
                                                  in_=bbT[gi * 64:(gi + 1) * 64, ri, gt, :]),
                          rd=("bbT", "bbX"), wr=("bbX",))
                    pi = (gt * 2 + ri) % 4
                    kb.op("pe", lambda e: e.transpose(ps[pi][:, 0:128], bbX[:], ident[:]), reads=["bbX"],
                          writes=[("ps", pi)])
                    kb.op("act", lambda e: e.copy(out=Bpad[:, ri, gt, :], in_=ps[pi][:, 0:128]),
                          reads=[("ps", pi)], writes=["Bpad"])
                for gi in range(2):
                    c0 = (gt % 4) * 32 + gi * 16
                    rows = slice(gi * 64, (gi + 1) * 64)
                    kb.op("pool", lambda e: e.tensor_copy(out=Cpad[rows, 0, gt, c0:c0 + 16], in_=CT[rows, 0, gt, :]),
                          reads=["CT", "Cpad"], writes=["Cpad"])
                    kb.op("pool", lambda e: e.tensor_scalar_mul(out=Cpad[rows, 1, gt, c0:c0 + 16],
                                                                in0=CT[rows, 0, gt, :], scalar1=-1.0),
                          reads=["CT", "Cpad"], writes=["Cpad"])
                    kb.op("pool", lambda e: e.tensor_scalar_mul(out=Cpad[rows, 2, gt, c0:c0 + 16],
                                                                in0=CT[rows, 1, gt, :], scalar1=-1.0),
                          reads=["CT", "Cpad"], writes=["Cpad"])
            for cc in range(4):
                V(lambda e: e.tensor_scalar_mul(out=Dg[:, cc, :], in0=ident[:], scalar1=dcol[:, cc:cc + 1]),
                  rd=("dcol",), wr=("Dg",))

            for gt in range(16):
                kb.op("dve", lambda e: e.tensor_scalar_mul(out=tph[:], in0=tpos[:], scalar1=prm[:, FT, gt:gt + 1]),
                      reads=["tpos", "prm", "tabw"], writes=["tph"])
                sincos(sn_tab[:, gt, :], cs_tab[:, gt, :], tph[:], tq_[:], tn_[:], tc_[:], ["tph"], ["tabw"])

            kb.barrier()
            p3s.close()
            uTt = [SB("uTt%d" % i, [128, 4, 512], F32, p3) for i in range(2)]
            W = {}
            for nm in ("t1", "t2", "t3", "t4", "bre", "bim", "wre", "wim", "p1", "p2", "p3", "p4"):
                W[nm] = [SB("w_%s%d" % (nm, i), [128, 512], F32, p3) for i in range(2)]
            ysb = [SB("ysb0", [128, 512], F32, p3)] * 2
            gt1 = [SB("gt1_0", [128, 512], F32, p3)] * 2
            gth = [SB("gth0", [128, 512], F32, p3)] * 2
            gyT = [SB("gyT%d" % i, [128, 4, 512], BF16, p3) for i in range(2)]
            sgt = [SB("sgt0", [128, 512], F32, p3)] * 2
            ysT = [SB("ysT0", [128, 4, 512], BF16, p3)] * 2
            TT = lambda e, o, a, b_, op: e.tensor_tensor(out=o, in0=a, in1=b_, op=op)

            def ssm_s1(us, cc, gt, s2):
                uk = ("uTt", us)
                pa, pb_ = ps[s2 * 2], ps[s2 * 2 + 1]
                pak, pbk = ("ps", s2 * 2), ("ps", s2 * 2 + 1)
                kb.op("pe", lambda e: e.matmul(pa[:], lhsT=Bpad[:, 0, gt, :], rhs=uTt[us][:, cc, :],
                                               start=True, stop=True), reads=["Bpad", uk], writes=[pak])
                kb.op("pe", lambda e: e.matmul(pb_[:], lhsT=Bpad[:, 1, gt, :], rhs=uTt[us][:, cc, :],
                                               start=True, stop=True), reads=["Bpad", uk], writes=[pbk])

            def ssm_s23(gt, s2, kb):
                w = {k_: v_[s2] for k_, v_ in W.items()}
                K_ = lambda nm: (nm, s2)
                cs_, sn_ = cs_tab[:, gt, :], sn_tab[:, gt, :]
                pa, pb_ = ps[s2 * 2], ps[s2 * 2 + 1]
                pak, pbk = ("ps", s2 * 2), ("ps", s2 * 2 + 1)
                kb.op("dve", lambda e: TT(e, w["t1"][:], pa[:], cs_, ALU.mult), reads=[pak, "tabw"], writes=[K_("t1")])
                kb.op("dve", lambda e: TT(e, w["t2"][:], pb_[:], sn_, ALU.mult), reads=[pbk, "tabw"], writes=[K_("t2")])
                kb.op("dve", lambda e: TT(e, w["t3"][:], pb_[:], cs_, ALU.mult), reads=[pbk, "tabw"], writes=[K_("t3")])
                kb.op("dve", lambda e: TT(e, w["t4"][:], pa[:], sn_, ALU.mult), reads=[pak, "tabw"], writes=[K_("t4")])
                kb.op("dve", lambda e: TT(e, w["bre"][:], w["t1"][:], w["t2"][:], ALU.add),
                      reads=[K_("t1"), K_("t2")], writes=[K_("bre")])
                kb.op("dve", lambda e: TT(e, w["bim"][:], w["t3"][:], w["t4"][:], ALU.subtract),
                      reads=[K_("t3"), K_("t4")], writes=[K_("bim")])

            def ssm_back(gt, s2, yb, first, kb):
                w = {k_: v_[s2] for k_, v_ in W.items()}
                K_ = lambda nm: (nm, s2)
                cs_, sn_ = cs_tab[:, gt, :], sn_tab[:, gt, :]
                for ri, (bn, wn) in enumerate((("bre", "wre"), ("bim", "wim"))):
                    kb.op("dve", lambda e, ri=ri, bn=bn, wn=wn: e.tensor_tensor_scan(
                        out=w[wn][:], data0=prm[:, MAG, gt:gt + 1].to_broadcast([128, 512]),
                        data1=w[bn][:], initial=carry[:, gt, ri:ri + 1], op0=ALU.mult, op1=ALU.add),
                          reads=[K_(bn), "prm", ("carry", gt)], writes=[K_(wn)])
                wl_re, wl_im = w["wre"][:, 511:512], w["wim"][:, 511:512]
                c5, s5 = prm[:, C512, gt:gt + 1], prm[:, S512, gt:gt + 1]
                kb.op("dve", lambda e: e.tensor_tensor(out=ctmp[:, 0:1], in0=wl_im, in1=s5, op=ALU.mult),
                      reads=[K_("wim"), "prm", "ctmp"], writes=["ctmp"])
                kb.op("dve", lambda e: e.tensor_tensor(out=ctmp[:, 1:2], in0=wl_im, in1=c5, op=ALU.mult),
                      reads=[K_("wim"), "prm", "ctmp"], writes=["ctmp"])
                kb.op("dve", lambda e: e.scalar_tensor_tensor(out=carry[:, gt, 0:1], in0=wl_re, scalar=c5,
                                                              in1=ctmp[:, 0:1], op0=ALU.mult, op1=ALU.subtract),
                      reads=[K_("wre"), "prm", "ctmp", ("carry", gt)], writes=[("carry", gt)])
                kb.op("dve", lambda e: e.scalar_tensor_tensor(out=carry[:, gt, 1:2], in0=wl_re, scalar=s5,
                                                              in1=ctmp[:, 1:2], op0=ALU.mult, op1=ALU.add),
                      reads=[K_("wre"), "prm", "ctmp", ("carry", gt)], writes=[("carry", gt)])
                kb.op("dve", lambda e: TT(e, w["p1"][:], cs_, w["wre"][:], ALU.mult),
                      reads=["tabw", K_("wre")], writes=[K_("p1")])
                kb.op("pool", lambda e: TT(e, w["p2"][:], sn_, w["wim"][:], ALU.mult),
                      reads=["tabw", K_("wim")], writes=[K_("p2")])
                kb.op("pool", lambda e: TT(e, w["p3"][:], sn_, w["wre"][:], ALU.mult),
                      reads=["tabw", K_("wre")], writes=[K_("p3")])
                kb.op("pool", lambda e: TT(e, w["p4"][:], cs_, w["wim"][:], ALU.mult),
                      reads=["tabw", K_("wim")], writes=[K_("p4")])
                for qi, (pn, ci) in enumerate((("p1", 0), ("p2", 1), ("p3", 2), ("p4", 2))):
                    kb.op("pe", lambda e, qi=qi, pn=pn, ci=ci: e.matmul(ps[yb][:], lhsT=Cpad[:, ci, gt, :], rhs=w[pn][:],
                                                                        start=(first and qi == 0), stop=False),
                          reads=["Cpad", K_(pn)], writes=[("ps", yb)])

            def ssm_ccend(us, cc, yb):
                uk = ("uTt", us)
                kb.op("pe", lambda e: e.matmul(ps[yb][:], lhsT=Dg[:, cc, :], rhs=uTt[us][:, cc, :],
                                               start=False, stop=True),
                      reads=["Dg", uk], writes=[("ps", yb)])
                kb.op("act", lambda e: e.activation(out=gyT[us][:, cc, :], in_=ps[yb][:], func=AF.Gelu_apprx_tanh),
                      reads=[("ps", yb)], writes=[("gyT", us, cc)])

            def ssm_glu(b, n, us):
                for m in range(4):
                    for half in range(2):
                        pi = 6 + half
                        mm = m + 4 * half
                        for k in range(4):
                            kb.op("pe", lambda e: e.matmul(ps[pi][:], lhsT=wglub[:, k, mm * 128:(mm + 1) * 128],
                                                           rhs=gyT[us][:, k, :], start=(k == 0), stop=(k == 3)),
                                  reads=[("wglub", k), ("gyT", us, k)], writes=[("ps", pi)])
                    sg_ = sgt[m % 2]
                    kb.op("act", lambda e: e.activation(out=sg_[:], in_=ps[7][:], func=AF.Sigmoid),
                          reads=[("ps", 7)], writes=[("sgt", 0)])
                    kb.op("dve", lambda e: e.tensor_tensor(out=ysT[us][:, m, :], in0=ps[6][:], in1=sg_[:],
                                                           op=ALU.mult),
                          reads=[("ps", 6), ("sgt", 0)], writes=[("ysT", 0, m)])
                kb.dma("act", ysT_s[b, :, :, n * 512:(n + 1) * 512].rearrange("m p t -> p m t"), ysT[us][:],
                       ("ysT", 0), reads=[("ysT", 0, m) for m in range(4)])

            items = [(b, n, cc, g4) for b in range(NS) for n in range(NG) for cc in range(4) for g4 in range(4)]
            NI = len(items)

            def do_s1(idx):
                b, n, cc, g4 = items[idx]
                us = (b * NG + n) % 2
                if cc == 0 and g4 == 0:
                    uk = ("uTt", us)
                    kb.dma("sp", uTt[us][:], uT_s[b, :, :, n * 512:(n + 1) * 512].rearrange("m p t -> p m t"), uk,
                           writes=[uk])
                ssm_s1(us, cc, cc * 4 + g4, idx % 2)

            def do_s23(idx, kb_):
                b, n, cc, g4 = items[idx]
                ssm_s23(cc * 4 + g4, idx % 2, kb_)

            do_s1(0)
            if NI > 1:
                do_s1(1)
            do_s23(0, kb)
            for idx in range(NI):
                b, n, cc, g4 = items[idx]
                us = (b * NG + n) % 2
                if idx + 2 < NI:
                    do_s1(idx + 2)
                rfront = Rec()
                if idx + 1 < NI:
                    do_s23(idx + 1, rfront)
                if n == 0 and cc == 0 and g4 == 0:
                    kb.op("dve", lambda e: e.memset(carry[:], 0.0),
                          reads=["carry"] + [("carry", g_) for g_ in range(16)],
                          writes=["carry"] + [("carry", g_) for g_ in range(16)])
                yb = 4 + ((b * NG + n) * 4 + cc) % 2
                rback = Rec()
                ssm_back(cc * 4 + g4, idx % 2, yb, g4 == 0, rback)
                emit_interleaved(kb, rback.L, rfront.L)
                if g4 == 3:
                    ssm_ccend(us, cc, yb)
                    if cc == 3:
                        ssm_glu(b, n, us)
            kb.barrier()

        with contextlib.ExitStack() as p4:
            wpab = SB("wpab", [128, 4, 1024], BF16, p4)
            wpsb = SB("wpsb", [128, 4, 1024], BF16, p4)
            woutb = SB("woutb", [128, 8, 1024], BF16, p4)
            wr = SB("wr", [128, 8, 36], F32, p4)
            br_bc = SB("br_bc", [128, 36], F32, p4)
            g2bc = SB("g2bc", [128, D], F32, p4)
            gate1 = SB("gate1", [128, NS, D], F32, p4)
            yaT_t = [SB("yaT_t%d" % i, [128, 4, 512], BF16, p4) for i in range(2)]
            ysT_t = [SB("ysT_t%d" % i, [128, 4, 512], BF16, p4) for i in range(2)]
            sg_t = [SB("sg_t%d" % i, [128, 16, 512], BF16, p4) for i in range(2)]
            mga = [SB("mga%d" % i, [128, 512], F32, p4) for i in range(2)]
            mgb = [SB("mgb%d" % i, [128, 512], F32, p4) for i in range(2)]
            mrgT = SB("mrgT", [128, 8, 512], BF16, p4)
            x4 = [SB("x4_%d" % i, [128, D], F32, p4) for i in range(3)]
            xm = [SB("xm%d" % i, [128, D], F32, p4) for i in range(2)]
            xn2 = [SB("xn2_%d" % i, [128, D], F32, p4) for i in range(2)]
            junk4 = SB("junk4", [128, D], F32, p4)
            st4 = [SB("st4_%d" % i, [128, 8], F32, p4) for i in range(2)]
            h2f = [SB("h2f%d" % i, [128, 8, 128], F32, p4) for i in range(2)]
            rt = [SB("rt%d" % i, [128, 160], F32, p4) for i in range(2)]
            mod2 = SB("mod2", [128, NS, 2, D], F32, p4)
            h2t = [SB("h2t%d" % i, [128, D], F32, p4) for i in range(2)]
            Sprev = SB("Sprev", [128, 64], F32, p4)
            iota_e = SB("iota_e", [128, 64], F32, p4)
            iota_p = SB("iota_p", [128, 1], F32, p4)
            tmp64 = [SB("tmp64_%d" % i, [128, 64], F32, p4) for i in range(2)]
            rtt = [SB("rtt%d" % i, [128, 8], F32, p4) for i in range(2)]
            for b in range(NS):
                kb.dma("sp", mod2[:, b, :, :], gate_s[:, (b * 4 + 2) * D:(b * 4 + 4) * D].rearrange("p (g d) -> p g d", g=2),
                       "mod2", writes=["mod2"])
            kb.op("dve", lambda e: e.memset(Sprev[:], 0.0), writes=["Sprev"])
            kb.op("pool", lambda e: e.iota(iota_e[:], pattern=[[0, 2], [1, 32]], base=0, channel_multiplier=0,
                                           allow_small_or_imprecise_dtypes=True), writes=["iota_e"])
            kb.op("pool", lambda e: e.iota(iota_p[:], pattern=[[0, 1]], base=0, channel_multiplier=1,
                                           allow_small_or_imprecise_dtypes=True), writes=["iota_p"])

            for k in range(4):
                kb.dma("pool", wpab[:, k, :], w_pa[k * 128:(k + 1) * 128, :], ("wpab", k), writes=[("wpab", k)])
                kb.dma("pool", wpsb[:, k, :], w_ps[k * 128:(k + 1) * 128, :], ("wpsb", k), writes=[("wpsb", k)])
            for k in range(8):
                kb.dma("pool", woutb[:, k, :], w_out[k * 128:(k + 1) * 128, :], ("woutb", k), writes=[("woutb", k)])
            kb.dma("sp", wr[:, :, 0:4], w_rg.rearrange("(k p) n -> p k n", p=128), "wr", writes=["wr"],
                   allow_slow_non_contiguous=True)
            for g in range(4):
                kb.dma("sp", wr[:, :, 4 + g * 8:12 + g * 8], w_re[g].rearrange("(k p) n -> p k n", p=128), "wr",
                       writes=["wr"], allow_slow_non_contiguous=True)
            kb.dma("sp", br_bc[:, 0:4], b_rg.partition_broadcast(128), "br_bc", writes=["br_bc"])
            kb.dma("sp", br_bc[:, 4:36], b_re.partition_broadcast(128), "br_bc", writes=["br_bc"])
            kb.dma("sp", g2bc[:], norm2_g.partition_broadcast(128), "g2bc", writes=["g2bc"])
            for b in range(NS):
                kb.dma("sp", gate1[:, b, :], gate_s[:, (b * 4) * D:(b * 4 + 1) * D], "gate1", writes=["gate1"])

            mcnt = [0]

            def p4_loadg(b, n):
                gs = (b * NG + n) % 2
                c0 = n * 512
                kb.dma("sp", yaT_t[gs][:], yaT_s[b, :, :, c0:c0 + 512].rearrange("m p t -> p m t"), ("yaT_t", gs),
                       writes=[("yaT_t", gs)])
                kb.dma("sp", ysT_t[gs][:], ysT_s[b, :, :, c0:c0 + 512].rearrange("m p t -> p m t"), ("ysT_t", gs),
                       writes=[("ysT_t", gs)])
                kb.dma("sp", sg_t[gs][:], sg_s[b, :, :, c0:c0 + 512].rearrange("m p t -> p m t"), ("sg_t", gs),
                       writes=[("sg_t", gs)])

            def p4_M(b, n):
                gs = (b * NG + n) % 2
                c0 = n * 512
                for m in range(8):
                    ia, ib = 4 + mcnt[0] % 3, 4 + (mcnt[0] + 1) % 3
                    mcnt[0] += 2
                    pA, pB = ps[ia], ps[ib]
                    pAk, pBk = ("ps", ia), ("ps", ib)
                    for k in range(4):
                        kb.op("pe", lambda e: e.matmul(pA[:], lhsT=wpab[:, k, m * 128:(m + 1) * 128],
                                                       rhs=yaT_t[gs][:, k, :], start=(k == 0), stop=(k == 3)),
                              reads=[("wpab", k), ("yaT_t", gs)], writes=[pAk])
                    for k in range(4):
                        kb.op("pe", lambda e: e.matmul(pB[:], lhsT=wpsb[:, k, m * 128:(m + 1) * 128],
                                                       rhs=ysT_t[gs][:, k, :], start=(k == 0), stop=(k == 3)),
                              reads=[("wpsb", k), ("ysT_t", gs)], writes=[pBk])
                    kb.op("dve", lambda e: e.tensor_tensor(out=mga[m % 2][:], in0=pA[:], in1=sg_t[gs][:, m, :],
                                                           op=ALU.mult),
                          reads=[pAk, ("sg_t", gs)], writes=[("mga", m % 2)])
                    kb.op("dve", lambda e: e.tensor_tensor(out=mgb[m % 2][:], in0=pB[:], in1=sg_t[gs][:, 8 + m, :],
                                                           op=ALU.mult),
                          reads=[pBk, ("sg_t", gs)], writes=[("mgb", m % 2)])
                    kb.op("dve", lambda e: e.tensor_tensor(out=mrgT[:, m, :], in0=mga[m % 2][:], in1=mgb[m % 2][:],
                                                            op=ALU.add),
                          reads=[("mga", m % 2), ("mgb", m % 2)], writes=[("mrgT", m)])

            def p4_loadx(idx):
                b, n, t = tiles4[idx]
                tok0 = n * 512 + t * 128
                kb.dma("sp", x4[idx % 3][:], x[b, tok0:tok0 + 128, :], ("x4", idx % 3), writes=[("x4", idx % 3)])

            def p4_A(b, n, t, sl, xsl):
                gs = (b * NG + n) % 2
                c0 = n * 512
                tok0 = c0 + t * 128
                st = st4[sl]
                for half in range(2):
                    pi = half
                    for k in range(8):
                        kb.op("pe", lambda e: e.matmul(ps[pi][:], lhsT=mrgT[:, k, t * 128:(t + 1) * 128],
                                                       rhs=woutb[:, k, half * 512:(half + 1) * 512],
                                                       start=(k == 0), stop=(k == 7)),
                              reads=[("mrgT", k), ("woutb", k)], writes=[("ps", pi)])
                    hs = slice(half * 512, (half + 1) * 512)
                    kb.op("dve", lambda e: e.tensor_tensor(out=xm[sl][:, hs], in0=ps[pi][:], in1=gate1[:, b, hs],
                                                           op=ALU.mult),
                          reads=[("ps", pi), "gate1"], writes=[("xm", sl, half)])
                    kb.op("dve", lambda e: e.tensor_tensor(out=xm[sl][:, hs], in0=xm[sl][:, hs],
                                                            in1=x4[xsl][:, hs], op=ALU.add),
                          reads=[("xm", sl, half), ("x4", xsl)], writes=[("xm", sl, half)])
                xmk = [("xm", sl, 0), ("xm", sl, 1)]
                kb.dma("sp", xmid_s[b, tok0:tok0 + 128, :], xm[sl][:], ("xm", sl), reads=xmk)
                kb.op("act", lambda e: e.activation(out=junk4[:], in_=xm[sl][:], func=AF.Square,
                                                    accum_out=st[:, 0:1]),
                      reads=xmk, writes=["junk4", ("st4", sl)])
                kb.op("dve", lambda e: e.tensor_scalar(out=st[:, 1:2], in0=st[:, 0:1], scalar1=1.0 / D,
                                                       scalar2=1e-6, op0=ALU.mult, op1=ALU.add),
                      reads=[("st4", sl)], writes=[("st4", sl)])
                kb.op("act", lambda e: e.activation(out=st[:, 2:3], in_=st[:, 1:2], func=AF.Ln),
                      reads=[("st4", sl)], writes=[("st4", sl)])
                kb.op("act", lambda e: e.activation(out=st[:, 3:4], in_=st[:, 2:3], func=AF.Exp, scale=-0.5),
                      reads=[("st4", sl)], writes=[("st4", sl)])
                kb.op("dve", lambda e: e.scalar_tensor_tensor(out=xn2[sl][:], in0=xm[sl][:], scalar=st[:, 3:4],
                                                              in1=g2bc[:], op0=ALU.mult, op1=ALU.mult),
                      reads=xmk + [("st4", sl), "g2bc"], writes=[("xn2", sl)])
                for k in range(8):
                    pi = 2 + k // 4
                    kb.op("pe", lambda e: e.transpose(ps[pi][:, (k % 4) * 128:(k % 4 + 1) * 128],
                                                      xn2[sl][:, k * 128:(k + 1) * 128], ident[:]),
                          reads=[("xn2", sl)], writes=[("ps", pi)])
                for k in range(8):
                    pi = 2 + k // 4
                    kb.op("act", lambda e: e.activation(out=h2f[sl][:, k, :],
                                                        in_=ps[pi][:, (k % 4) * 128:(k % 4 + 1) * 128],
                                                        func=AF.Identity, scale=modT[:, 32 + k, b:b + 1],
                                                        bias=modT[:, 24 + k, b:b + 1]),
                          reads=[("ps", pi)], writes=[("h2f", sl)])
                kb.op("dve", lambda e: e.tensor_tensor(out=h2t[sl][:], in0=xn2[sl][:], in1=mod2[:, b, 1, :],
                                                       op=ALU.mult),
                      reads=[("xn2", sl), "mod2"], writes=[("h2t", sl)])
                kb.op("dve", lambda e: e.tensor_tensor(out=h2t[sl][:], in0=h2t[sl][:], in1=mod2[:, b, 0, :],
                                                        op=ALU.add),
                      reads=[("h2t", sl), "mod2"], writes=[("h2t", sl)])
                kb.dma("sp", h2tok_s[b * S + tok0:b * S + tok0 + 128, :], h2t[sl][:], ("h2t", sl),
                       reads=[("h2t", sl)])

            def p4_B(b, n, t, sl, kb):
                gs = (b * NG + n) % 2
                c0 = n * 512
                tok0 = c0 + t * 128
                st = st4[sl]
                R = rt[sl]
                rk = ("rt", sl)
                bkB = 7 if sl == 0 else 3
                pr = ps[bkB][:, 0:128]
                for k in range(8):
                    kb.op("pe", lambda e, k=k: e.matmul(pr[:, 0:36], lhsT=h2f[sl][:, k, :], rhs=wr[:, k, :],
                                                        start=(k == 0), stop=(k == 7)),
                          reads=[("h2f", sl), "wr"], writes=[("ps", bkB)])

                def RV(fn, extra=()):
                    kb.op("dve", fn, reads=[rk] + list(extra), writes=[rk])
                L = R[:, 0:36]
                RV(lambda e: e.tensor_tensor(out=L, in0=pr[:, 0:36], in1=br_bc[:], op=ALU.add),
                   extra=[("ps", bkB), "br_bc"])
                RV(lambda e: e.reduce_max(out=R[:, 36:37], in_=R[:, 0:4], axis=AX.X))
                RV(lambda e: e.tensor_scalar(out=R[:, 40:44], in0=R[:, 0:4], scalar1=R[:, 36:37], scalar2=None,
                                             op0=ALU.is_equal))
                RV(lambda e: e.tensor_scalar_mul(out=R[:, 37:38], in0=R[:, 36:37], scalar1=-1.0))
                kb.op("act", lambda e: e.activation(out=R[:, 44:48], in_=R[:, 0:4], func=AF.Exp,
                                                    bias=R[:, 37:38], accum_out=R[:, 38:39]),
                      reads=[rk], writes=[rk])
                RV(lambda e: e.reciprocal(out=R[:, 39:40], in_=R[:, 38:39]))
                RV(lambda e: e.tensor_scalar(out=R[:, 48:52], in0=R[:, 40:44], scalar1=-1.0, scalar2=1e30,
                                             op0=ALU.add, op1=ALU.mult))
                for g in range(4):
                    RV(lambda e, g=g: e.tensor_scalar(out=R[:, 64 + g * 8:72 + g * 8], in0=R[:, 4 + g * 8:12 + g * 8],
                                                      scalar1=R[:, 48 + g:49 + g], scalar2=None, op0=ALU.add))
                LM = R[:, 64:96]
                RV(lambda e: e.reduce_max(out=R[:, 52:53], in_=LM, axis=AX.X))
                RV(lambda e: e.tensor_scalar(out=R[:, 96:128], in0=LM, scalar1=R[:, 52:53], scalar2=None,
                                             op0=ALU.is_equal))
                RV(lambda e: e.scalar_tensor_tensor(out=R[:, 128:160], in0=R[:, 96:128], scalar=-1e30, in1=LM,
                                                    op0=ALU.mult, op1=ALU.add))
                RV(lambda e: e.reduce_max(out=R[:, 53:54], in_=R[:, 128:160], axis=AX.X))
                RV(lambda e: e.tensor_scalar(out=R[:, 64:96], in0=R[:, 128:160], scalar1=R[:, 53:54],
                                             scalar2=None, op0=ALU.is_equal))
                RV(lambda e: e.tensor_tensor(out=R[:, 54:55], in0=R[:, 53:54], in1=R[:, 52:53], op=ALU.subtract))
                kb.op("act", lambda e: e.activation(out=R[:, 55:56], in_=R[:, 54:55], func=AF.Exp),
                      reads=[rk], writes=[rk])
                RV(lambda e: e.tensor_scalar_add(out=R[:, 56:57], in0=R[:, 55:56], scalar1=1.0))
                RV(lambda e: e.reciprocal(out=R[:, 57:58], in_=R[:, 56:57]))
                RV(lambda e: e.tensor_tensor(out=R[:, 58:59], in0=R[:, 57:58], in1=R[:, 55:56], op=ALU.mult))
                RV(lambda e: e.tensor_tensor(out=R[:, 59:60], in0=R[:, 57:58], in1=R[:, 39:40], op=ALU.mult))
                RV(lambda e: e.tensor_tensor(out=R[:, 60:61], in0=R[:, 58:59], in1=R[:, 39:40], op=ALU.mult))
                OH = R[:, 64:128]
                pc = ps[bkB][:, 64:128]
                if hasattr(kb, "L"):
                    kb.split = len(kb.L)
                kb.op("pe", lambda e: e.matmul(pc, lhsT=Tri[:], rhs=OH, start=True, stop=False),
                      reads=[rk, "Tri"], writes=[("ps", bkB)])
                kb.op("pe", lambda e: e.matmul(pc, lhsT=ones[:], rhs=Sprev[:], start=False, stop=True),
                      reads=["Sprev", "ones"], writes=[("ps", bkB)])
                T64 = tmp64[sl]
                RTT = rtt[sl]
                tk, rtk = ("tmp64", sl), ("rtt", sl)
                kb.op("dve", lambda e: e.tensor_tensor(out=T64[:], in0=pc, in1=OH, op=ALU.mult),
                      reads=[("ps", bkB), rk], writes=[tk])
                kb.op("dve", lambda e: e.reduce_sum(out=RTT[:, 2:4], in_=T64[:].rearrange("p (s e) -> p s e", s=2),
                                                    axis=AX.X), reads=[tk], writes=[rtk])
                kb.op("dve", lambda e: e.tensor_scalar_add(out=RTT[:, 2:4], in0=RTT[:, 2:4], scalar1=-1.0),
                      reads=[rtk], writes=[rtk])
                kb.op("dve", lambda e: e.tensor_tensor(out=T64[:], in0=iota_e[:], in1=OH, op=ALU.mult),
                      reads=["iota_e", rk, tk], writes=[tk])
                kb.op("dve", lambda e: e.reduce_sum(out=RTT[:, 0:2], in_=T64[:].rearrange("p (s e) -> p s e", s=2),
                                                    axis=AX.X), reads=[tk, rtk], writes=[rtk])
                kb.op("dve", lambda e: e.tensor_copy(out=RTT[:, 4:5], in_=R[:, 60:61]), reads=[rk, rtk], writes=[rtk])
                kb.op("dve", lambda e: e.tensor_copy(out=RTT[:, 5:6], in_=R[:, 59:60]), reads=[rk, rtk], writes=[rtk])
                kb.op("dve", lambda e: e.tensor_scalar_add(out=RTT[:, 6:7], in0=iota_p[:],
                                                           scalar1=float(b * S + tok0)),
                      reads=["iota_p", rtk], writes=[rtk])
                kb.op("dve", lambda e: e.memset(RTT[:, 7:8], 0.0), reads=[rtk], writes=[rtk])
                kb.op("dve", lambda e: e.tensor_tensor(out=Sprev[:], in0=Sprev[:], in1=OH, op=ALU.add),
                      reads=["Sprev", rk], writes=["Sprev"])
                return functools.partial(kb_real.dma, "sp", rt_s[b * S + tok0:b * S + tok0 + 128, :], RTT[:], rtk,
                                         reads=[rtk])

            kb_real = kb
            tiles4 = [(b, n, t) for b in range(NS) for n in range(NG) for t in range(4)]
            groups4 = [(b, n) for b in range(NS) for n in range(NG)]
            NT4 = len(tiles4)
            p4_loadg(*groups4[0])
            p4_loadx(0)
            if NT4 > 1:
                p4_loadx(1)
            late = []
            for ti in range(0, NT4, 2):
                pair = [ti] + ([ti + 1] if ti + 1 < NT4 else [])
                for tj in pair:
                    b, n, t = tiles4[tj]
                    if tj + 2 < NT4:
                        p4_loadx(tj + 2)
                    if t == 1 and tj // 4 + 1 < len(groups4):
                        p4_loadg(*groups4[tj // 4 + 1])
                    if t == 0:
                        p4_M(b, n)
                    p4_A(b, n, t, tj % 2, tj % 3)
                for f in late:
                    f()
                late = []
                recs = []
                for tj in pair:
                    b, n, t = tiles4[tj]
                    r_ = Rec()
                    late.append(p4_B(b, n, t, tj % 2, r_))
                    recs.append(r_)
                if len(recs) == 2:
                    emit_interleaved(kb, recs[0].L[:recs[0].split], recs[1].L[:recs[1].split])
                    emit_interleaved(kb, recs[0].L[recs[0].split:], [])
                    emit_interleaved(kb, recs[1].L[recs[1].split:], [])
                else:
                    emit_interleaved(kb, recs[0].L, [])
            for f in late:
                f()
            kb.op("pe", lambda e: e.matmul(ps[1][:, 64:128], lhsT=ones[:], rhs=Sprev[:], start=True, stop=True),
                  reads=["Sprev", "ones"], writes=[("ps", 1)])
            kb.op("dve", lambda e: e.tensor_copy(out=cnt_bc[:], in_=ps[1][:, 64:128]), reads=[("ps", 1)],
                  writes=["cnt_bc"])
            kb.barrier()

        with contextlib.ExitStack() as p4b:
            I32 = mybir.dt.int32
            MAGIC_ = 12582912.0
            NRI = NB * BLK // 128
            rinit = SB("rinit", [128, NRI, 4], F32, p4b)
            bt = SB("bt", [128, 8, 32], F32, p4b)
            base = SB("base", [128, 64], F32, p4b)
            ebt = SB("ebt", [128, NB], F32, p4b)
            eb2 = SB("eb2", [128, 2, NB], F32, p4b)
            kp = SB("kp", [128, 12], F32, p4b)
            iota32 = SB("iota32", [128, 32], F32, p4b)
            rti = [SB("rti%d" % i, [128, 8], F32, p4b) for i in range(2)]
            ohs = [SB("ohs%d" % i, [128, 64], F32, p4b) for i in range(2)]
            dstf = [SB("dstf%d" % i, [128, 2], F32, p4b) for i in range(2)]
            dsti = [SB("dsti%d" % i, [128, 2], I32, p4b) for i in range(2)]
            pay = [SB("pay%d" % i, [128, 2, 4], F32, p4b) for i in range(2)]
            kb.op("pool", lambda e: e.memset(rinit[:], 0.0), writes=["rinit"])
            trash = SB("trash", [128, NRI], F32, p4b)
            kb.op("pool", lambda e: e.iota(trash[:], pattern=[[0, NRI]], base=2 * TT_, channel_multiplier=1,
                                           allow_small_or_imprecise_dtypes=True), writes=["trash"])
            kb.op("pool", lambda e: e.tensor_copy(out=rinit[:, :, 2], in_=trash[:]), reads=["rinit", "trash"],
                  writes=["rinit"])
            kb.dma("sp", rowtab_s.rearrange("(r p) c -> p r c", p=128), rinit[:], "rinit", reads=["rinit"],
                   writes=["rowtab_init"])
            kb.op("pool", lambda e: e.iota(iota32[:], pattern=[[1, 32]], base=0, channel_multiplier=0,
                                           allow_small_or_imprecise_dtypes=True), writes=["iota32"])
            kb.op("pool", lambda e: e.iota(kp[:, 0:8], pattern=[[128, 8]], base=0, channel_multiplier=1,
                                           allow_small_or_imprecise_dtypes=True), writes=["kp"])
            kb.op("pool", lambda e: e.iota(kp[:, 8:12], pattern=[[128, 4]], base=0, channel_multiplier=1,
                                           allow_small_or_imprecise_dtypes=True), reads=["kp"], writes=["kp"])
            B_ = lambda i: bt[:, i, :]

            def BV(fn, extra=()):
                kb.op("dve", fn, reads=["bt", "cnt_bc"] + list(extra), writes=["bt"])
            BV(lambda e: e.tensor_tensor(out=B_(0), in0=cnt_bc[:, 0:32], in1=cnt_bc[:, 32:64], op=ALU.add))
            BV(lambda e: e.tensor_scalar(out=B_(1), in0=B_(0), scalar1=BLK / 2 - 0.5, scalar2=1.0 / BLK, op0=ALU.add,
                                         op1=ALU.mult))
            BV(lambda e: e.tensor_scalar_add(out=B_(5), in0=B_(1), scalar1=MAGIC_))
            BV(lambda e: e.tensor_scalar_add(out=B_(2), in0=B_(5), scalar1=-MAGIC_))
            BV(lambda e: e.tensor_tensor_scan(out=B_(3), data0=ones[:, 0:32], data1=B_(2), initial=0.0, op0=ALU.mult,
                                              op1=ALU.add), extra=["ones"])
            BV(lambda e: e.tensor_tensor(out=B_(4), in0=B_(3), in1=B_(2), op=ALU.subtract))
            kb.op("dve", lambda e: e.tensor_scalar_mul(out=base[:, 0:32], in0=B_(4), scalar1=float(BLK)),
                  reads=["bt"], writes=["base"])
            kb.op("dve", lambda e: e.tensor_tensor(out=base[:, 32:64], in0=base[:, 0:32], in1=cnt_bc[:, 0:32], op=ALU.add),
                  reads=["base", "cnt_bc"], writes=["base"])
            for blk in range(NB):
                BV(lambda e: e.tensor_single_scalar(out=B_(5), in_=B_(3), scalar=float(blk), op=ALU.is_le))
                kb.op("dve", lambda e: e.reduce_sum(out=ebt[:, blk:blk + 1], in_=B_(5), axis=AX.X), reads=["bt", "ebt"],
                      writes=["ebt"])
            kb.op("dve", lambda e: e.tensor_scalar_mul(out=eb2[:, 0, :], in0=ebt[:], scalar1=1024.0), reads=["ebt"],
                  writes=["eb2"])
            kb.op("dve", lambda e: e.tensor_scalar_mul(out=eb2[:, 1, :], in0=ebt[:], scalar1=512.0), reads=["ebt", "eb2"],
                  writes=["eb2"])
            for blk in range(NB):
                kb.op("dve", lambda e: e.tensor_scalar(out=widx_in[:, blk, :], in0=kp[:, 0:8],
                                                       scalar1=eb2[:, 0, blk:blk + 1], scalar2=None, op0=ALU.add),
                      reads=["kp", "eb2", "widx"], writes=["widx"])
                kb.op("dve", lambda e: e.tensor_scalar(out=widx_out[:, blk, :], in0=kp[:, 8:12],
                                                       scalar1=eb2[:, 1, blk:blk + 1], scalar2=None, op0=ALU.add),
                      reads=["kp", "eb2", "widx"], writes=["widx"])
            for ti in range(TT_ // 128):
                sl = ti % 2
                RI = rti[sl]
                rik, ohk, dk, pk_ = ("rti", sl), ("ohs", sl), ("dst", sl), ("pay", sl)
                kb.dma("sp", RI[:], rt_s[ti * 128:(ti + 1) * 128, :], rik, writes=[rik])
                for sidx in range(2):
                    hsl = slice(sidx * 32, (sidx + 1) * 32)
                    kb.op("dve", lambda e: e.tensor_scalar(out=ohs[sl][:, hsl], in0=iota32[:],
                                                           scalar1=RI[:, sidx:sidx + 1], scalar2=None, op0=ALU.is_equal),
                          reads=["iota32", rik, ohk], writes=[ohk])
                kb.op("dve", lambda e: e.tensor_tensor(out=ohs[sl][:], in0=ohs[sl][:], in1=base[:], op=ALU.mult),
                      reads=[ohk, "base"], writes=[ohk])
                kb.op("dve", lambda e: e.reduce_sum(out=dstf[sl][:], in_=ohs[sl][:].rearrange("p (s e) -> p s e", s=2),
                                                    axis=AX.X), reads=[ohk, dk], writes=[dk])
                kb.op("dve", lambda e: e.tensor_tensor(out=dstf[sl][:], in0=dstf[sl][:], in1=RI[:, 2:4], op=ALU.add),
                      reads=[dk, rik], writes=[dk])
                kb.op("dve", lambda e: e.tensor_copy(out=dsti[sl][:], in_=dstf[sl][:]), reads=[dk, ("dsti", sl)],
                      writes=[("dsti", sl)])
                kb.op("dve", lambda e: e.memset(pay[sl][:], 0.0), reads=[pk_], writes=[pk_])
                for sidx in range(2):
                    kb.op("dve", lambda e: e.tensor_copy(out=pay[sl][:, sidx, 0:1], in_=RI[:, 6:7]), reads=[rik, pk_],
                          writes=[pk_])
                    kb.op("dve", lambda e: e.tensor_copy(out=pay[sl][:, sidx, 1:2], in_=RI[:, 4 + sidx:5 + sidx]),
                          reads=[rik, pk_], writes=[pk_])
                    kb.op("dve", lambda e: e.tensor_scalar_add(out=pay[sl][:, sidx, 2:3], in0=RI[:, 6:7],
                                                               scalar1=float(sidx * TT_)),
                          reads=[rik, pk_], writes=[pk_])
                for sidx in range(2):
                    kb.idma(rowtab_s[:, :], bass.IndirectOffsetOnAxis(ap=dsti[sl][:, sidx:sidx + 1], axis=0),
                            pay[sl][:, sidx, :], None, pk_, reads=[pk_, ("dsti", sl), "rowtab_init"])
            kb.barrier()

        with contextlib.ExitStack() as p5:
            I32 = mybir.dt.int32
            w_ein_f = w_ein.rearrange("e k n -> (e k) n")
            w_eout_f = w_eout.rearrange("e k n -> (e k) n")
            wei = [SB("wei%d" % i, [128, 8, 1024], BF16, p5) for i in range(3)]
            weo = [SB("weo%d" % i, [128, 4, 1024], BF16, p5) for i in range(3)]
            rtab = SB("rtab", [128, 3, 4, 4], F32, p5)
            rti32 = SB("rti32", [128, 3, 4, 2], I32, p5)
            xg = [SB("xg%d" % i, [128, D], F32, p5) for i in range(8)]
            xT = [SB("xT%d" % i, [128, 8, 512], BF16, p5) for i in range(2)]
            sa = [SB("sa%d" % i, [128, 512], F32, p5) for i in range(2)]
            hid = [SB("hid%d" % i, [128, 4, 512], BF16, p5) for i in range(2)]
            yb_ = [SB("yb%d" % i, [128, D], F32, p5) for i in range(8)]
            ocnt_ = [0]

            reg_in = nc.gpsimd.to_reg(32 * 1024 - 1)
            reg_out = nc.gpsimd.to_reg(32 * 512 - 1)

            def LOADW(blk):
                ws = blk % 3
                for k in range(8):
                    kb.idma(wei[ws][:, k, :], None, w_ein_f[:, :],
                            bass.IndirectOffsetOnAxis(ap=widx_in[:, blk, k:k + 1], axis=0), ("wei", ws, k),
                            reads=["widx"], writes=[("wei", ws, k)], bounds_check=reg_in, oob_is_err=False)
                for k in range(4):
                    kb.idma(weo[ws][:, k, :], None, w_eout_f[:, :],
                            bass.IndirectOffsetOnAxis(ap=widx_out[:, blk, k:k + 1], axis=0), ("weo", ws, k),
                            reads=["widx"], writes=[("weo", ws, k)], bounds_check=reg_out, oob_is_err=False)

            def LOADX(blk):
                rs = blk % 3
                for sub in range(4):
                    r0 = blk * BLK + sub * 128
                    rk_ = ("rtab", rs, sub)
                    gi_ = (blk % 2) * 4 + sub
                    kb.dma("sp", rtab[:, rs, sub, :], rowtab_s[r0:r0 + 128, :], rk_, writes=[rk_])
                    kb.op("dve", lambda e: e.tensor_copy(out=rti32[:, rs, sub, 0:1], in_=rtab[:, rs, sub, 0:1]),
                          reads=[rk_, ("rti32", rs, sub)], writes=[("rti32", rs, sub)])
                    kb.op("dve", lambda e: e.tensor_copy(out=rti32[:, rs, sub, 1:2], in_=rtab[:, rs, sub, 2:3]),
                          reads=[rk_, ("rti32", rs, sub)], writes=[("rti32", rs, sub)])
                    kb.idma(xg[gi_][:, :], None, h2tok_s[:, :],
                            bass.IndirectOffsetOnAxis(ap=rti32[:, rs, sub, 0:1], axis=0), ("xg", gi_),
                            reads=[("rti32", rs, sub)], writes=[("xg", gi_)])

            def COMP(blk):
                ws = blk % 3
                rs = blk % 3
                xs = blk % 2
                for sub in range(4):
                    gi_ = (blk % 2) * 4 + sub
                    for k in range(8):
                        pi = 6 + k // 4
                        kb.op("pe", lambda e: e.transpose(ps[pi][:, (k % 4) * 128:(k % 4 + 1) * 128],
                                                          xg[gi_][:, k * 128:(k + 1) * 128], ident[:]),
                              reads=[("xg", gi_)], writes=[("ps", pi)])
                    kb.op("act", lambda e: e.copy(out=xT[xs][:, 0:4, sub * 128:(sub + 1) * 128],
                                                  in_=ps[6][:].rearrange("p (k t) -> p k t", k=4)),
                          reads=[("ps", 6)], writes=[("xT", xs, sub, 0)])
                    kb.op("dve", lambda e: e.tensor_copy(out=xT[xs][:, 4:8, sub * 128:(sub + 1) * 128],
                                                         in_=ps[7][:].rearrange("p (k t) -> p k t", k=4)),
                          reads=[("ps", 7)], writes=[("xT", xs, sub, 1)])
                xkeys = [("xT", xs, sub, hh) for sub in range(4) for hh in range(2)]
                for j in range(4):
                    pa_i, pg_i = (j % 2) * 2, (j % 2) * 2 + 1
                    for k in range(8):
                        kb.op("pe", lambda e: e.matmul(ps[pa_i][:], lhsT=wei[ws][:, k, j * 128:(j + 1) * 128],
                                                       rhs=xT[xs][:, k, :], start=(k == 0), stop=(k == 7)),
                              reads=[("wei", ws, k)] + xkeys, writes=[("ps", pa_i)])
                    for k in range(8):
                        kb.op("pe", lambda e: e.matmul(ps[pg_i][:],
                                                       lhsT=wei[ws][:, k, 512 + j * 128:512 + (j + 1) * 128],
                                                       rhs=xT[xs][:, k, :], start=(k == 0), stop=(k == 7)),
                              reads=[("wei", ws, k)] + xkeys, writes=[("ps", pg_i)])
                    kb.op("act", lambda e: e.activation(out=sa[j % 2][:], in_=ps[pa_i][:], func=AF.Silu),
                          reads=[("ps", pa_i)], writes=[("sa", j % 2)])
                    kb.op("dve", lambda e: e.tensor_tensor(out=hid[xs][:, j, :], in0=ps[pg_i][:],
                                                           in1=sa[j % 2][:], op=ALU.mult),
                          reads=[("ps", pg_i), ("sa", j % 2)], writes=[("hid", xs, j)])
                for sub in range(4):
                    yi = (blk % 2) * 4 + sub
                    for half in range(2):
                        po = 4 + ocnt_[0] % 2
                        ocnt_[0] += 1
                        for j in range(4):
                            kb.op("pe", lambda e: e.matmul(ps[po][:], lhsT=hid[xs][:, j, sub * 128:(sub + 1) * 128],
                                                           rhs=weo[ws][:, j, half * 512:(half + 1) * 512],
                                                           start=(j == 0), stop=(j == 3)),
                                  reads=[("hid", xs, j), ("weo", ws, j)], writes=[("ps", po)])
                        kb.op("dve" if half == 0 else "act",
                              (lambda e: e.tensor_scalar_mul(out=yb_[yi][:, 0:512], in0=ps[po][:],
                                                             scalar1=rtab[:, rs, sub, 1:2])) if half == 0 else
                              (lambda e: e.activation(out=yb_[yi][:, 512:1024], in_=ps[po][:], func=AF.Copy,
                                                      scale=rtab[:, rs, sub, 1:2])),
                              reads=[("ps", po), ("rtab", rs, sub)], writes=[("yb", yi, half)])

            def SCAT(blk):
                rs = blk % 3
                for sub in range(4):
                    yi = (blk % 2) * 4 + sub
                    kb.idma(ys_s[:, :], bass.IndirectOffsetOnAxis(ap=rti32[:, rs, sub, 1:2], axis=0),
                            yb_[yi][:, :], None, ("yb", yi),
                            reads=[("yb", yi, 0), ("yb", yi, 1), ("rti32", rs, sub)])

            LOADW(0)
            LOADX(0)
            if NB > 1:
                LOADW(1)
            for blk in range(NB):
                if blk + 1 < NB:
                    LOADX(blk + 1)
                if blk + 2 < NB:
                    LOADW(blk + 2)
                COMP(blk)
                if blk >= 1:
                    SCAT(blk - 1)
            SCAT(NB - 1)
            kb.barrier()

        with contextlib.ExitStack() as p6:
            gate2 = SB("gate2", [128, NS, D], F32, p6)
            gfbc = SB("gfbc", [128, D], F32, p6)
            x5 = [SB("x5_%d" % i, [128, D], F32, p6) for i in range(3)]
            y0 = [SB("y0_%d" % i, [128, D], F32, p6) for i in range(3)]
            y1 = [SB("y1_%d" % i, [128, D], F32, p6) for i in range(3)]
            xo = [SB("xo%d" % i, [128, D], F32, p6) for i in range(3)]
            junk5 = SB("junk5", [128, D], F32, p6)
            st5 = [SB("st5_%d" % i, [128, 8], F32, p6) for i in range(3)]
            kb.dma("sp", gfbc[:], final_g.partition_broadcast(128), "gfbc", writes=["gfbc"])
            for b in range(NS):
                kb.dma("sp", gate2[:, b, :], gate_s[:, (b * 4 + 1) * D:(b * 4 + 2) * D], "gate2", writes=["gate2"])
            tiles6 = [(b, ti) for b in range(NS) for ti in range(NT)]

            def p6_load(idx):
                b, ti = tiles6[idx]
                sl = idx % 3
                tok0 = ti * 128
                tg = b * S + tok0
                kb.dma("sp", x5[sl][:], xmid_s[b, tok0:tok0 + 128, :], ("x5", sl), writes=[("x5", sl)])
                kb.dma("sp", y0[sl][:], ys_s[tg:tg + 128, :], ("y0", sl), writes=[("y0", sl)])
                kb.dma("sp", y1[sl][:], ys_s[TT_ + tg:TT_ + tg + 128, :], ("y1", sl), writes=[("y1", sl)])

            p6_load(0)
            if len(tiles6) > 1:
                p6_load(1)
            for idx in range(len(tiles6)):
                if True:
                    b, ti = tiles6[idx]
                    sl = idx % 3
                    tok0 = ti * 128
                    st = st5[sl]
                    if idx + 2 < len(tiles6):
                        p6_load(idx + 2)
                    kb.op("dve", lambda e: e.tensor_tensor(out=y0[sl][:], in0=y0[sl][:], in1=y1[sl][:], op=ALU.add),
                          reads=[("y0", sl), ("y1", sl)], writes=[("y0", sl)])
                    kb.op("dve", lambda e: e.tensor_tensor(out=xo[sl][:], in0=y0[sl][:], in1=gate2[:, b, :], op=ALU.mult),
                          reads=[("y0", sl), "gate2"], writes=[("xo", sl)])
                    kb.op("dve", lambda e: e.tensor_tensor(out=xo[sl][:], in0=xo[sl][:], in1=x5[sl][:], op=ALU.add),
                          reads=[("xo", sl), ("x5", sl)], writes=[("xo", sl)])
                    kb.op("act", lambda e: e.activation(out=junk5[:], in_=xo[sl][:], func=AF.Square,
                                                        accum_out=st[:, 0:1]),
                          reads=[("xo", sl)], writes=["junk5", ("st5", sl)])
                    kb.op("dve", lambda e: e.tensor_scalar(out=st[:, 1:2], in0=st[:, 0:1], scalar1=1.0 / D,
                                                           scalar2=1e-6, op0=ALU.mult, op1=ALU.add),
                          reads=[("st5", sl)], writes=[("st5", sl)])
                    kb.op("act", lambda e: e.activation(out=st[:, 2:3], in_=st[:, 1:2], func=AF.Ln),
                          reads=[("st5", sl)], writes=[("st5", sl)])
                    kb.op("act", lambda e: e.activation(out=st[:, 3:4], in_=st[:, 2:3], func=AF.Exp, scale=-0.5),
                          reads=[("st5", sl)], writes=[("st5", sl)])
                    kb.op("dve", lambda e: e.scalar_tensor_tensor(out=xo[sl][:], in0=xo[sl][:], scalar=st[:, 3:4],
                                                                  in1=gfbc[:], op0=ALU.mult, op1=ALU.mult),
                          reads=[("xo", sl), ("st5", sl), "gfbc"], writes=[("xo", sl)])
                    kb.dma("sp", y[b, tok0:tok0 + 128, :], xo[sl][:], ("xo", sl), reads=[("xo", sl)])
            kb.barrier()

        kb.barrier()
    return nc


def _prep_inputs(inputs):
    f = lambda a: np.ascontiguousarray(np.asarray(a, dtype=np.float32))
    d = {}
    d["w_ada"] = f(inputs["w_ada"][0])
    d["b_ada"] = f(inputs["b_ada"][0]).reshape(1, -1)
    d["norm1_g"] = f(inputs["norm1_g"][0]).reshape(1, -1)
    d["w_in"] = f(inputs["w_in"][0])
    d["rel_bias"] = f(inputs["rel_bias"])
    for n in ("lambda_q1", "lambda_k1", "lambda_q2", "lambda_k2", "subln_g"):
        d[n] = f(inputs[n][0]).reshape(1, -1)
    d["ssm_lambda_re"] = f(inputs["ssm_lambda_re"][0])
    d["ssm_lambda_im"] = f(inputs["ssm_lambda_im"][0])
    d["ssm_log_step"] = f(inputs["ssm_log_step"][0]).reshape(32, 1)
    d["ssm_b_re"] = f(inputs["ssm_b_re"][0])
    d["ssm_b_im"] = f(inputs["ssm_b_im"][0])
    d["ssm_c_re"] = f(inputs["ssm_c_re"][0])
    d["ssm_c_im"] = f(inputs["ssm_c_im"][0])
    d["ssm_d"] = f(inputs["ssm_d"][0]).reshape(512, 1)
    d["w_glu"] = f(inputs["w_glu"][0])
    d["w_proj_attn"] = f(inputs["w_proj_attn"][0])
    d["w_proj_ssm"] = f(inputs["w_proj_ssm"][0])
    d["w_out"] = f(inputs["w_out"][0])
    d["norm2_g"] = f(inputs["norm2_g"][0]).reshape(1, -1)
    d["w_router_group"] = f(inputs["w_router_group"][0])
    d["b_router_group"] = f(inputs["b_router_group"][0]).reshape(1, 4)
    d["w_router_expert"] = f(inputs["w_router_expert"][0])
    d["b_router_expert"] = f(inputs["b_router_expert"][0]).reshape(1, 32)
    d["w_expert_in"] = f(inputs["w_expert_in"][0])
    d["w_expert_out"] = f(inputs["w_expert_out"][0])
    d["final_g"] = f(inputs["final_g"]).reshape(1, -1)
    d["relmask"] = _relmask()
    return d


def _relmask():
    kk = np.arange(128)[:, None]
    qq = np.arange(128)[None, :]
    out = np.zeros((33, 2, 128, 128), np.float32)
    for blk in range(2):
        dist = qq - kk + 128 * blk
        n = np.maximum(dist, 0)
        lr = np.log(np.maximum(n, 1).astype(np.float32) / 16) / math.log(128 / 16)
        large = np.minimum(16 + (lr * 16).astype(np.int32), 31)
        bucket = np.where(n < 16, n, large)
        for bkt in range(32):
            out[bkt, blk] = ((bucket == bkt) & (dist >= 0))
        out[32, blk] = (dist < 0)
    return out.reshape(33, -1)


def kernel(**inputs):
    S = inputs["x"].shape[1]
    B = inputs["x"].shape[0]
    NS = B // NCORES
    nc = build(S, NS)
    shared = _prep_inputs(inputs)
    xs = np.ascontiguousarray(np.asarray(inputs["x"], dtype=np.float32))
    cs = np.ascontiguousarray(np.asarray(inputs["c"], dtype=np.float32))
    in_maps = []
    for i in range(NCORES):
        m = dict(shared)
        m["x"] = xs[i * NS:(i + 1) * NS]
        m["c"] = cs[i * NS:(i + 1) * NS]
        in_maps.append(m)
    res = run_bass_kernel_spmd(nc, in_maps, core_ids=list(range(NCORES)))
    return np.concatenate([r["y"] for r in res.results], axis=0)
```

```python
import math
import functools
import contextlib
import numpy as np
import concourse.bass as bass
import concourse.mybir as mybir
from concourse.bass_utils import run_bass_kernel_spmd

F32 = mybir.dt.float32
BF16 = mybir.dt.bfloat16
AF = mybir.ActivationFunctionType
ALU = mybir.AluOpType
AX = mybir.AxisListType

D = 1024
NCORES = 8
NEG = -1e30
LAMBDA_INIT = 0.8 - 0.6 * math.exp(-0.3 * 0)


class KB:
    def __init__(self, nc, es):
        self.nc = nc
        self.es = es
        self.eng = dict(pe=nc.tensor, act=nc.scalar, dve=nc.vector, pool=nc.gpsimd, sp=nc.sync)
        self.sem = {e: es.enter_context(nc.semaphore("s_" + e)) for e in ("pe", "act", "dve", "pool")}
        self.tick = {e: 0 for e in self.sem}
        self.seen = {e: {} for e in self.eng}
        self.res = {}
        self.dsem = {}
        self.dfree = []
        self.dfree_sw = []
        self.nd = 0
        self.nins = 0

    def _wait(self, e, ev, raw, isdma=False):
        sem, val, src = ev
        if (not isdma) and src == e and (e == "pe" or not raw):
            return
        if self.seen[e].get(sem, 0) >= val:
            return
        self.eng[e].wait_ge(sem, val)
        self.seen[e][sem] = val

    def _deps(self, e, reads, writes, isdma=False):
        for k in reads:
            r = self.res.get(k)
            if r and r[0]:
                self._wait(e, r[0], True, isdma)
        for k in writes:
            r = self.res.get(k)
            if r:
                if r[0]:
                    self._wait(e, r[0], False, isdma)
                for ev in r[1].values():
                    self._wait(e, ev, False, isdma)

    def _reg(self, ev, reads, writes):
        for k in reads:
            r = self.res.setdefault(k, [None, {}])
            r[1][ev[0]] = ev
        for k in writes:
            self.res[k] = [ev, {}]

    def op(self, e, fn, reads=(), writes=()):
        self._deps(e, reads, writes)
        ins = fn(self.eng[e])
        self.tick[e] += 1
        ins.then_inc(self.sem[e], 1)
        self._reg((self.sem[e], self.tick[e], e), reads, writes)
        self.nins += 1

    def dma(self, q, out, in_, key, reads=(), writes=(), **kw):
        self._deps(q, reads, writes, isdma=True)
        ins = self.eng[q].dma_start(out=out, in_=in_, **kw)
        self._finish_dma(ins, key, q == "pool", reads, writes)

    def _finish_dma(self, ins, key, sw, reads, writes):
        k2 = (key, sw)
        d = self.dsem.get(k2)
        if d is None:
            fl = self.dfree_sw if sw else self.dfree
            if fl:
                d = fl.pop()
            else:
                self.nd += 1
                d = [self.es.enter_context(self.nc.semaphore("d%d" % self.nd)), 0, sw]
            self.dsem[k2] = d
        d[1] += 16
        ins.then_inc(d[0], 16)
        self._reg((d[0], d[1], "dma"), reads, writes)
        self.nins += 1

    def idma(self, out, out_off, in_, in_off, key, reads=(), writes=(), **kw):
        self._deps("pool", reads, writes, isdma=True)
        ins = self.eng["pool"].indirect_dma_start(out=out, out_offset=out_off, in_=in_, in_offset=in_off, **kw)
        self._finish_dma(ins, key, True, reads, writes)

    def barrier(self):
        evs = [(self.sem[e], self.tick[e], e) for e in self.sem if self.tick[e] > 0]
        evs += [(d[0], d[1], "dma") for d in self.dsem.values() if d[1] > 0]
        for e in self.eng:
            for ev in evs:
                self._wait(e, ev, True, isdma=True)
        self.res = {}
        for d in self.dsem.values():
            (self.dfree_sw if d[2] else self.dfree).append(d)
        self.dsem = {}


class Rec:
    def __init__(self):
        self.L = []

    def op(self, e, fn, reads=(), writes=()):
        self.L.append((e, fn, list(reads), list(writes)))


def emit_interleaved(kb, a, b):
    ia = ib = 0
    while ia < len(a) or ib < len(b):
        if ia < len(a):
            kb.op(*a[ia])
            ia += 1
        if ib < len(b):
            kb.op(*b[ib])
            ib += 1


def build(S, NS, debug=None):
    nc = bass.Bass("TRN2", target_bir_lowering=False)
    NT = S // 128
    NG = S // 512
    dbg = debug or ()

    def din(name, shape):
        return nc.dram_tensor(name, shape, F32, kind="ExternalInput").ap()

    x = din("x", [NS, S, D])
    c = din("c", [NS, D])
    w_ada = din("w_ada", [D, 6 * D])
    b_ada = din("b_ada", [1, 6 * D])
    norm1_g = din("norm1_g", [1, D])
    w_in = din("w_in", [D, 4096])
    rel_bias = din("rel_bias", [32, 4])
    lam_q1 = din("lambda_q1", [1, 64])
    lam_k1 = din("lambda_k1", [1, 64])
    lam_q2 = din("lambda_q2", [1, 64])
    lam_k2 = din("lambda_k2", [1, 64])
    subln_g = din("subln_g", [1, 128])
    ssm_lre = din("ssm_lambda_re", [32, 64])
    ssm_lim = din("ssm_lambda_im", [32, 64])
    ssm_ls = din("ssm_log_step", [32, 1])
    ssm_bre = din("ssm_b_re", [32, 64, 16])
    ssm_bim = din("ssm_b_im", [32, 64, 16])
    ssm_cre = din("ssm_c_re", [32, 16, 64])
    ssm_cim = din("ssm_c_im", [32, 16, 64])
    ssm_d = din("ssm_d", [512, 1])
    w_glu = din("w_glu", [512, 1024])
    w_pa = din("w_proj_attn", [512, 1024])
    w_ps = din("w_proj_ssm", [512, 1024])
    w_out = din("w_out", [1024, 1024])
    norm2_g = din("norm2_g", [1, D])
    w_rg = din("w_router_group", [1024, 4])
    b_rg = din("b_router_group", [1, 4])
    w_re = din("w_router_expert", [4, 1024, 8])
    b_re = din("b_router_expert", [1, 32])
    w_ein = din("w_expert_in", [32, 1024, 1024])
    w_eout = din("w_expert_out", [32, 512, 1024])
    final_g = din("final_g", [1, D])
    relmask = din("relmask", [33, 2 * 128 * 128])
    y = nc.dram_tensor("y", [NS, S, D], F32, kind="ExternalOutput").ap()

    def scratch(name, shape, dt):
        kind = "ExternalOutput" if name in dbg else "Internal"
        return nc.dram_tensor(name, shape, dt, kind=kind).ap()

    qT_s = scratch("qT_s", [NS, 4, 128, S], BF16)
    kT_s = scratch("kT_s", [NS, 4, 128, S], BF16)
    v_s = scratch("v_s", [NS, S, 512], BF16)
    uT_s = scratch("uT_s", [NS, 4, 128, S], F32)
    sg_s = scratch("sg_s", [NS, 16, 128, S], BF16)
    mod_s = scratch("mod_s", [128, 48 * NS], F32)
    tab_s = scratch("tab_s", [4, 2 * 128 * 128], F32)
    gate_s = scratch("gate_s", [128, NS * 4 * D], F32)
    xmid_s = scratch("xmid_s", [NS, S, D], F32)
    h2T_s = scratch("h2T_s", [NS, 8, 128, S], BF16)
    comb_s = scratch("comb_s", [NS, S, 32], F32)
    TT_ = NS * S
    BLK = 512
    NB = 2 * TT_ // BLK + 32
    rt_s = scratch("rt_s", [TT_, 8], F32)
    h2tok_s = scratch("h2tok_s", [TT_, D], F32)
    rowtab_s = scratch("rowtab_s", [NB * BLK, 4], F32)
    ys_s = scratch("ys_s", [2 * TT_ + 128, D], F32)
    yaT_s = scratch("yaT_s", [NS, 4, 128, S], BF16)
    ysT_s = scratch("ysT_s", [NS, 4, 128, S], BF16)

    with contextlib.ExitStack() as es:
        kb = KB(nc, es)

        def SB(name, shape, dt, stack=es):
            return stack.enter_context(nc.sbuf_tensor(name, shape, dt))

        ps = [es.enter_context(nc.psum_tensor("ps%d" % i, [128, 512], F32)) for i in range(8)]

        ident = SB("ident", [128, 128], F32)
        ones = SB("ones", [128, 128], F32)
        modT = SB("modT", [128, 48, NS], F32)
        Tri = SB("Tri", [128, 128], F32)
        cnt_bc = SB("cnt_bc", [128, 64], F32)
        widx_in = SB("widx_in", [128, NB, 8], mybir.dt.int32)
        widx_out = SB("widx_out", [128, NB, 4], mybir.dt.int32)

        with contextlib.ExitStack() as p0:
            io = SB("io", [128, 128], F32, p0)
            cT = SB("cT", [128, 8, NS], F32, p0)
            sc = SB("sc", [128, 8, NS], F32, p0)
            sc_bc = SB("sc_bc", [128, NS, 8, 128], F32, p0)
            b_adaT = SB("b_adaT", [128, 48], F32, p0)
            bada_bc = SB("bada_bc", [128, 4, D], F32, p0)
            wst = [SB("wst%d" % i, [128, 8, 512], F32, p0) for i in range(3)]
            gate_bc = SB("gate_bc", [128, NS, 4, D], F32, p0)

            kb.op("pool", lambda e: e.iota(io[:], pattern=[[1, 128]], base=0, channel_multiplier=-1,
                                           allow_small_or_imprecise_dtypes=True), writes=["io"])
            kb.op("dve", lambda e: e.tensor_single_scalar(out=ident[:], in_=io[:], scalar=0.0, op=ALU.is_equal),
                  reads=["io"], writes=["ident"])
            kb.op("dve", lambda e: e.memset(ones[:], 1.0), writes=["ones"])
            kb.op("dve", lambda e: e.tensor_single_scalar(out=Tri[:], in_=io[:], scalar=0.0, op=ALU.is_ge),
                  reads=["io"], writes=["Tri"])
            for b in range(NS):
                kb.dma("sp", cT[:, :, b], c[b].rearrange("(k p) -> p k", p=128), "cT", writes=["cT"],
                       allow_slow_non_contiguous=True)
            kb.dma("sp", b_adaT[:], b_ada.rearrange("o (j p) -> p (o j)", p=128), "b_adaT", writes=["b_adaT"],
                   allow_slow_non_contiguous=True)
            for gi_, c0_ in enumerate((2048, 5120, 3072, 4096)):
                kb.dma("sp", bada_bc[:, gi_, :], b_ada[:, c0_:c0_ + 1024].partition_broadcast(128), "bada_bc",
                       writes=["bada_bc"])
            kb.op("act", lambda e: e.activation(out=sc[:], in_=cT[:], func=AF.Silu), reads=["cT"], writes=["sc"])
            for b in range(NS):
                for k in range(8):
                    kb.op("dve", lambda e, b=b, k=k: e.tensor_scalar_mul(out=sc_bc[:, b, k, :], in0=ones[:],
                                                                       scalar1=sc[:, k, b:b + 1]),
                          reads=["sc", "ones"], writes=["sc_bc"])
            def p0_load(jb):
                kb.dma("sp" if jb % 2 == 0 else "pool", wst[jb % 3][:],
                       w_ada[:, jb * 512:(jb + 1) * 512].rearrange("(k p) n -> p k n", p=128), ("wst", jb % 3),
                       writes=[("wst", jb % 3)])

            p0_load(0)
            p0_load(1)
            for jb in range(12):
                wt = wst[jb % 3]
                wk = ("wst", jb % 3)
                if jb + 2 < 12:
                    p0_load(jb + 2)
                pb = ps[jb % 2]
                pk = ("ps", jb % 2)
                for m in range(4):
                    for k in range(8):
                        kb.op("pe", lambda e, m=m, k=k: e.matmul(pb[:, m * NS:(m + 1) * NS],
                                                                 lhsT=wt[:, k, m * 128:(m + 1) * 128],
                                                                 rhs=sc[:, k, :], start=(k == 0), stop=(k == 7)),
                              reads=[wk, "sc"], writes=[pk])
                for b in range(NS):
                    kb.op("dve", lambda e, b=b: e.tensor_tensor(
                        out=modT[:, jb * 4:(jb + 1) * 4, b],
                        in0=pb[:, 0:4 * NS].rearrange("p (m b) -> p m b", b=NS)[:, :, b],
                        in1=b_adaT[:, jb * 4:(jb + 1) * 4], op=ALU.add),
                          reads=[pk, "b_adaT"], writes=["modT"])
                if jb in (4, 5, 10, 11, 6, 7, 8, 9):
                    gi = {4: 0, 5: 0, 10: 1, 11: 1, 6: 2, 7: 2, 8: 3, 9: 3}[jb]
                    half = jb % 2
                    for b in range(NS):
                        pg = ps[2 + b % 2]
                        pgk = ("ps", 2 + b % 2)
                        for k in range(8):
                            kb.op("pe", lambda e, k=k, b=b: e.matmul(pg[:], lhsT=sc_bc[:, b, k, :], rhs=wt[:, k, :],
                                                                     start=(k == 0), stop=(k == 7)),
                                  reads=[wk, "sc_bc"], writes=[pgk])
                        kb.op("dve", lambda e, b=b: e.tensor_tensor(
                            out=gate_bc[:, b, gi, half * 512:(half + 1) * 512], in0=pg[:],
                            in1=bada_bc[:, gi, half * 512:(half + 1) * 512], op=ALU.add),
                              reads=[pgk, "bada_bc"], writes=["gate_bc"])
            for lo in (8, 32):
                kb.op("dve", lambda e, lo=lo: e.tensor_scalar_add(out=modT[:, lo:lo + 8, :], in0=modT[:, lo:lo + 8, :],
                                                                  scalar1=1.0),
                      reads=["modT"], writes=["modT"])
            for b in range(NS):
                kb.op("dve", lambda e: e.tensor_scalar_add(out=gate_bc[:, b, 3, :], in0=gate_bc[:, b, 3, :], scalar1=1.0),
                      reads=["gate_bc"], writes=["gate_bc"])
            kb.dma("sp", gate_s, gate_bc[:].rearrange("p b g d -> p (b g d)"), "gate_st", reads=["gate_bc"])
            if "mod_s" in dbg:
                kb.dma("sp", mod_s, modT[:].rearrange("p j b -> p (j b)"), "modT_st", reads=["modT"])
            kb.barrier()

        with contextlib.ExitStack() as p1:
            winb = SB("winb", [128, 8, 4096], BF16, p1)
            g1bc = SB("g1bc", [128, D], F32, p1)
            kb.dma("sp", g1bc[:], norm1_g.partition_broadcast(128), "g1bc", writes=["g1bc"])
            xt = [SB("xt%d" % i, [128, D], F32, p1) for i in range(2)]
            junk = SB("junk", [128, D], F32, p1)
            xn = [SB("xn%d" % i, [128, D], F32, p1) for i in range(2)]
            ss = SB("ss", [128, 8], F32, p1)
            hT = [SB("hT%d" % i, [128, 8, 512], BF16, p1) for i in range(2)]
            stq = [SB("stq%d" % i, [128, 24, 512], BF16, p1) for i in range(2)]
            stu = [SB("stu%d" % i, [128, 4, 512], F32, p1) for i in range(2)]
            stv = [SB("stv%d" % i, [128, 4, 512], BF16, p1) for i in range(2)]
            for k in range(8):
                kb.dma("pool", winb[:, k, :], w_in[k * 128:(k + 1) * 128, :], ("winb", k), writes=[("winb", k)])
            tcnt = 0
            pcnt = 0
            ecnt = 0
            for b in range(NS):
                for g in range(NG):
                    gs = (b * NG + g) % 2
                    hk = ("hT", gs)
                    for t in range(4):
                        tok0 = g * 512 + t * 128
                        sl = tcnt % 2
                        tcnt += 1
                        kb.dma("sp", xt[sl][:], x[b, tok0:tok0 + 128, :], ("xt", sl), writes=[("xt", sl)])
                        kb.op("act", lambda e: e.activation(out=junk[:], in_=xt[sl][:], func=AF.Square,
                                                            accum_out=ss[:, sl:sl + 1]),
                              reads=[("xt", sl)], writes=["junk", ("ss", sl)])
                        kb.op("dve", lambda e: e.tensor_scalar(out=ss[:, 2 + sl:3 + sl], in0=ss[:, sl:sl + 1],
                                                               scalar1=1.0 / D, scalar2=1e-6, op0=ALU.mult,
                                                               op1=ALU.add),
                              reads=[("ss", sl)], writes=[("ss2", sl)])
                        kb.op("act", lambda e: e.activation(out=ss[:, 6 + sl:7 + sl], in_=ss[:, 2 + sl:3 + sl],
                                                            func=AF.Sqrt),
                              reads=[("ss2", sl)], writes=[("ss3", sl)])
                        kb.op("dve", lambda e: e.reciprocal(out=ss[:, 4 + sl:5 + sl], in_=ss[:, 6 + sl:7 + sl]),
                              reads=[("ss3", sl)], writes=[("rstd", sl)])
                        kb.op("dve", lambda e: e.scalar_tensor_tensor(out=xn[sl][:], in0=xt[sl][:],
                                                                      scalar=ss[:, 4 + sl:5 + sl], in1=g1bc[:],
                                                                      op0=ALU.mult, op1=ALU.mult),
                              reads=[("xt", sl), ("rstd", sl), "g1bc"], writes=[("xn", sl)])
                        for k in range(8):
                            pb = ps[sl * 2 + k // 4]
                            pk = ("ps", sl * 2 + k // 4)
                            kb.op("pe", lambda e, k=k, pb=pb: e.transpose(pb[:, (k % 4) * 128:(k % 4 + 1) * 128],
                                                                          xn[sl][:, k * 128:(k + 1) * 128], ident[:]),
                                  reads=[("xn", sl)], writes=[pk])
                        for k in range(8):
                            pb = ps[sl * 2 + k // 4]
                            pk = ("ps", sl * 2 + k // 4)
                            kb.op("act", lambda e, k=k, pb=pb: e.activation(
                                out=hT[gs][:, k, t * 128:(t + 1) * 128],
                                in_=pb[:, (k % 4) * 128:(k % 4 + 1) * 128], func=AF.Identity,
                                scale=modT[:, 8 + k, b:b + 1], bias=modT[:, k, b:b + 1]),
                                  reads=[pk], writes=[hk])
                    for t in range(4):
                        pi = 4 + pcnt % 4
                        pcnt += 1
                        for k in range(8):
                            kb.op("pe", lambda e, k=k: e.matmul(ps[pi][:], lhsT=hT[gs][:, k, t * 128:(t + 1) * 128],
                                                                rhs=winb[:, k, 1024:1536], start=(k == 0),
                                                                stop=(k == 7)),
                                  reads=[hk, ("winb", k)], writes=[("ps", pi)])
                        kb.op("dve", lambda e: e.tensor_copy(out=stv[gs][:, t, :], in_=ps[pi][:]),
                              reads=[("ps", pi)], writes=[("stv", gs, t)])
                    for m in list(range(0, 8)) + list(range(12, 32)):
                        pi = 4 + pcnt % 4
                        pcnt += 1
                        for k in range(8):
                            kb.op("pe", lambda e, k=k: e.matmul(ps[pi][:], lhsT=winb[:, k, m * 128:(m + 1) * 128],
                                                                rhs=hT[gs][:, k, :], start=(k == 0), stop=(k == 7)),
                                  reads=[hk, ("winb", k)], writes=[("ps", pi)])
                        if m < 8:
                            eng = "dve" if (ecnt % 2 == 0) else "act"
                            ecnt += 1
                            if eng == "dve":
                                kb.op("dve", lambda e: e.tensor_copy(out=stq[gs][:, m, :], in_=ps[pi][:]),
                                      reads=[("ps", pi)], writes=[("stq", gs, m)])
                            else:
                                kb.op("act", lambda e: e.copy(out=stq[gs][:, m, :], in_=ps[pi][:]),
                                      reads=[("ps", pi)], writes=[("stq", gs, m)])
                        elif m < 16:
                            kb.op("dve", lambda e: e.tensor_copy(out=stu[gs][:, m - 12, :], in_=ps[pi][:]),
                                  reads=[("ps", pi)], writes=[("stu", gs, m - 12)])
                        else:
                            kb.op("act", lambda e: e.activation(out=stq[gs][:, m - 8, :], in_=ps[pi][:],
                                                                func=AF.Sigmoid),
                                  reads=[("ps", pi)], writes=[("stq", gs, m - 8)])
                    c0 = g * 512
                    kb.dma("pool", qT_s[b, :, :, c0:c0 + 512].rearrange("m p t -> p m t"), stq[gs][:, 0:4, :],
                           ("stq", gs), reads=[("stq", gs, m) for m in range(0, 4)])
                    kb.dma("pool", kT_s[b, :, :, c0:c0 + 512].rearrange("m p t -> p m t"), stq[gs][:, 4:8, :],
                           ("stq", gs), reads=[("stq", gs, m) for m in range(4, 8)])
                    kb.dma("pool", sg_s[b, :, :, c0:c0 + 512].rearrange("m p t -> p m t"), stq[gs][:, 8:24, :],
                           ("stq", gs), reads=[("stq", gs, m) for m in range(8, 24)])
                    kb.dma("pool", uT_s[b, :, :, c0:c0 + 512].rearrange("m p t -> p m t"), stu[gs][:],
                           ("stu", gs), reads=[("stu", gs, m) for m in range(4)])
                    kb.dma("pool", v_s[b, c0:c0 + 512, :].rearrange("(t p) n -> p t n", p=128), stv[gs][:],
                           ("stv", gs), reads=[("stv", gs, t) for t in range(4)])
            kb.barrier()

        with contextlib.ExitStack() as p2:
            rbx = SB("rbx", [33, 4], F32, p2)
            rb32 = SB("rb32", [32, 4], F32, p2)
            b31_32 = SB("b31_32", [32, 4], F32, p2)
            b31_bc = SB("b31_bc", [128, 4], F32, p2)
            rmk = [SB("rmk%d" % i, [33, 4096], F32, p2) for i in range(2)]
            tabst = [SB("tabst%d" % i, [4, 4096], F32, p2) for i in range(2)]
            T8 = SB("T8", [128, 8, 128], F32, p2)
            E8 = SB("E8", [128, 8, 128], F32, p2)
            lamt = SB("lamt", [128, 4, 64], F32, p2)
            lams = SB("lams", [128, 8], F32, p2)
            gsub = SB("gsub", [128, 128], F32, p2)
            kT = SB("kT", [128, 4, S], BF16, p2)
            qT = SB("qT", [128, 4, S], BF16, p2)
            vsb = SB("vsb", [128, NT, 4, 130], BF16, p2)
            pT = [SB("pT%d" % i, [128, 512], BF16, p2) for i in range(4)]
            osb = [SB("osb%d" % i, [128, 512], F32, p2) for i in range(2)]
            sm = [SB("sm%d" % i, [128, 16], F32, p2) for i in range(2)]
            ojunk = SB("ojunk", [128, 128], F32, p2)
            yst = [SB("yst%d" % i, [128, 4, 512], BF16, p2) for i in range(2)]

            kb.op("dve", lambda e: e.memset(rbx[:], -8e30), writes=["rbx"])
            kb.dma("sp", rb32[:], rel_bias, "rb32", writes=["rb32"])
            kb.dma("sp", b31_32[:], rel_bias[31:32, :].partition_broadcast(32), "b31_32", writes=["b31_32"])
            kb.dma("sp", b31_bc[:], rel_bias[31:32, :].partition_broadcast(128), "b31_bc", writes=["b31_bc"])
            kb.op("dve", lambda e: e.tensor_tensor(out=rb32[:], in0=rb32[:], in1=b31_32[:], op=ALU.subtract),
                  reads=["rb32", "b31_32"], writes=["rb32"])
            kb.op("dve", lambda e: e.tensor_scalar_mul(out=rbx[0:32, :], in0=rb32[:], scalar1=8.0),
                  reads=["rb32", "rbx"], writes=["rbx"])
            for pc in range(8):
                rk = ("rmk", pc % 2)
                kb.dma("sp", rmk[pc % 2][:], relmask[:, pc * 4096:(pc + 1) * 4096], rk, writes=[rk])
                for n in range(8):
                    pi = n % 4
                    kb.op("pe", lambda e: e.matmul(ps[pi][0:4, :], lhsT=rbx[:, :], rhs=rmk[pc % 2][:, n * 512:(n + 1) * 512],
                                                   start=True, stop=True),
                          reads=["rbx", rk], writes=[("ps", pi)])
                    kb.op("dve", lambda e: e.tensor_copy(out=tabst[pc % 2][:, n * 512:(n + 1) * 512], in_=ps[pi][0:4, :]),
                          reads=[("ps", pi)], writes=[("tabst", pc % 2, n)])
                kb.dma("sp", tab_s[:, pc * 4096:(pc + 1) * 4096], tabst[pc % 2][:], ("tabst", pc % 2),
                       reads=[("tabst", pc % 2, n) for n in range(8)])
            for i, la in enumerate((lam_q1, lam_k1, lam_q2, lam_k2)):
                kb.dma("sp", lamt[:, i, :], la.partition_broadcast(128), "lamt", writes=["lamt"])
            kb.dma("sp", gsub[:], subln_g.partition_broadcast(128), "gsub", writes=["gsub"])
            kb.op("dve", lambda e: e.tensor_scalar_mul(out=gsub[:], in0=gsub[:], scalar1=1.0 - LAMBDA_INIT),
                  reads=["gsub"], writes=["gsub"])
            for i in range(2):
                kb.op("dve", lambda e: e.tensor_tensor(out=lamt[:, 2 * i, :], in0=lamt[:, 2 * i, :],
                                                       in1=lamt[:, 2 * i + 1, :], op=ALU.mult),
                      reads=["lamt"], writes=["lamt"])
                kb.op("dve", lambda e: e.reduce_sum(out=lams[:, i:i + 1], in_=lamt[:, 2 * i, :], axis=AX.X),
                      reads=["lamt"], writes=["lams"])
            kb.op("act", lambda e: e.activation(out=lams[:, 2:4], in_=lams[:, 0:2], func=AF.Exp),
                  reads=["lams"], writes=["lams"])
            kb.op("dve", lambda e: e.tensor_tensor(out=lams[:, 4:5], in0=lams[:, 3:4], in1=lams[:, 2:3], op=ALU.subtract),
                  reads=["lams"], writes=["lams"])
            kb.op("dve", lambda e: e.tensor_scalar_add(out=lams[:, 5:6], in0=lams[:, 4:5], scalar1=-LAMBDA_INIT),
                  reads=["lams"], writes=["lams"])
            kb.barrier()
            kb.dma("sp", T8[:], tab_s.rearrange("h (b k q) -> k (h b) q", b=2, k=128), "T8", writes=["T8"])
            kb.op("act", lambda e: e.activation(out=E8[:], in_=T8[:], func=AF.Exp, scale=0.125), reads=["T8"],
                  writes=["E8"])
            kb.op("dve", lambda e: e.memset(vsb[:, :, :, 128:130], 1.0), writes=["vones"])

            scnt = 0
            ptc = 0
            pend = []
            LOOK = 3

            def flush(keep):
                while len(pend) > keep:
                    pend.pop(0)()

            def emit_qk_exp(i, h, m, j0, nb, si, pt, ptk):
                for jj in range(nb):
                    j = j0 + jj
                    near = (j >= i - 1)
                    kb.op("pe", lambda e: e.matmul(
                        ps[si][:, jj * 128:(jj + 1) * 128],
                        lhsT=kT[m * 64:(m + 1) * 64, h, j * 128:(j + 1) * 128],
                        rhs=qT[m * 64:(m + 1) * 64, h, i * 128:(i + 1) * 128],
                        start=True, stop=True),
                          reads=[("kT", h), ("qT", h)], writes=[("ps", si)])
                kb.op("act", lambda e: e.activation(out=pt[:, 0:nb * 128], in_=ps[si][:, 0:nb * 128],
                                                    func=AF.Exp, scale=0.125, bias=b31_bc[:, h:h + 1]),
                      reads=[("ps", si), "b31_bc"], writes=[ptk])
                for jj in range(nb):
                    j = j0 + jj
                    if j >= i - 1:
                        kb.op("dve", lambda e: e.tensor_tensor(out=pt[:, jj * 128:(jj + 1) * 128],
                                                               in0=pt[:, jj * 128:(jj + 1) * 128],
                                                               in1=E8[:, h * 2 + (i - j), :], op=ALU.mult),
                              reads=[ptk, "E8"], writes=[ptk])

            def emit_pv(i, h, m, j0, nb, pt, ptk):
                pob = ps[4 + h % 2]
                pok = ("ps", 4 + h % 2)
                for jj in range(nb):
                    j = j0 + jj
                    kb.op("pe", lambda e: e.matmul(
                        pob[:, m * 256:m * 256 + 129], lhsT=pt[:, jj * 128:(jj + 1) * 128],
                        rhs=vsb[:, j, h, 0:129], start=(j == 0), stop=(j == i)),
                          reads=[ptk, ("vsb", h), "vones"], writes=[pok])

            def emit_fin(i, h):
                ob = osb[i % 2]
                ok = ("osb", i % 2)
                smt = sm[i % 2]
                smk = ("sm", i % 2)
                pob = ps[4 + h % 2]
                pok = ("ps", 4 + h % 2)
                kb.op("dve", lambda e: e.reciprocal(out=smt[:, 0:1], in_=pob[:, 128:129]),
                      reads=[pok], writes=[smk])
                kb.op("dve", lambda e: e.reciprocal(out=smt[:, 1:2], in_=pob[:, 384:385]),
                      reads=[pok], writes=[smk])
                kb.op("dve", lambda e: e.tensor_tensor(out=smt[:, 2:3], in0=smt[:, 1:2], in1=lams[:, 5:6],
                                                       op=ALU.mult),
                      reads=[smk, "lams"], writes=[smk])
                kb.op("dve", lambda e: e.tensor_scalar_mul(out=ob[:, h * 128:(h + 1) * 128], in0=pob[:, 0:128],
                                                           scalar1=smt[:, 0:1]),
                      reads=[pok, smk], writes=[ok])
                kb.op("dve", lambda e: e.scalar_tensor_tensor(out=ob[:, h * 128:(h + 1) * 128],
                                                              in0=pob[:, 256:384], scalar=smt[:, 2:3],
                                                              in1=ob[:, h * 128:(h + 1) * 128],
                                                              op0=ALU.mult, op1=ALU.add),
                      reads=[pok, smk, ok], writes=[ok])
                kb.op("act", lambda e: e.activation(out=ojunk[:], in_=ob[:, h * 128:(h + 1) * 128],
                                                    func=AF.Square, accum_out=smt[:, 4 + h:5 + h]),
                      reads=[ok], writes=["ojunk", (smk, "ss")])

            def emit_post(b, i):
                ob = osb[i % 2]
                ok = ("osb", i % 2)
                smt = sm[i % 2]
                smk = ("sm", i % 2)
                kb.op("dve", lambda e: e.tensor_scalar(out=smt[:, 8:12], in0=smt[:, 4:8], scalar1=1.0 / 128,
                                                       scalar2=1e-5, op0=ALU.mult, op1=ALU.add),
                      reads=[(smk, "ss")], writes=[(smk, "v")])
                kb.op("act", lambda e: e.activation(out=smt[:, 12:16], in_=smt[:, 8:12], func=AF.Ln),
                      reads=[(smk, "v")], writes=[(smk, "ln")])
                kb.op("act", lambda e: e.activation(out=smt[:, 8:12], in_=smt[:, 12:16], func=AF.Exp, scale=-0.5),
                      reads=[(smk, "ln")], writes=[(smk, "v")])
                for h in range(4):
                    kb.op("dve", lambda e: e.scalar_tensor_tensor(out=ob[:, h * 128:(h + 1) * 128],
                                                                  in0=ob[:, h * 128:(h + 1) * 128],
                                                                  scalar=smt[:, 8 + h:9 + h], in1=gsub[:],
                                                                  op0=ALU.mult, op1=ALU.mult),
                          reads=[ok, (smk, "v"), "gsub"], writes=[ok])
                ysl = (i // 4) % 2
                for h in range(4):
                    kb.op("pe", lambda e: e.transpose(ps[6][:, h * 128:(h + 1) * 128], ob[:, h * 128:(h + 1) * 128],
                                                      ident[:]),
                          reads=[ok], writes=[("ps", 6)])
                kb.op("act", lambda e: e.copy(out=yst[ysl][:, :, (i % 4) * 128:(i % 4 + 1) * 128],
                                              in_=ps[6][:].rearrange("p (h q) -> p h q", h=4)),
                      reads=[("ps", 6)], writes=[("yst", ysl, i % 4)])
                if i % 4 == 3:
                    c0 = (i // 4) * 512
                    kb.dma("pool", yaT_s[b, :, :, c0:c0 + 512].rearrange("m p t -> p m t"), yst[ysl][:],
                           ("yst", ysl), reads=[("yst", ysl, t) for t in range(4)])

            for b in range(NS):
                flush(0)
                for h in range(4):
                    kb.dma("sp", kT[:, h, :], kT_s[b, h], ("kT", h), writes=[("kT", h)])
                    kb.dma("sp", qT[:, h, :], qT_s[b, h], ("qT", h), writes=[("qT", h)])
                for h in range(4):
                    kb.dma("sp", vsb[:, :, h, 0:128],
                           v_s[b, :, h * 128:(h + 1) * 128].rearrange("(t p) d -> p t d", p=128), ("vsb", h),
                           writes=[("vsb", h)])
                for i in range(NT):
                    for h in range(4):
                        for m in range(2):
                            j0 = 0
                            while j0 <= i:
                                nb = min(4, i + 1 - j0)
                                si = scnt % 4
                                scnt += 1
                                pt = pT[ptc % 4]
                                ptk = ("pT", ptc % 4)
                                ptc += 1
                                emit_qk_exp(i, h, m, j0, nb, si, pt, ptk)
                                pend.append(functools.partial(emit_pv, i, h, m, j0, nb, pt, ptk))
                                flush(LOOK)
                                j0 += nb
                        pend.append(functools.partial(emit_fin, i, h))
                    pend.append(functools.partial(emit_post, b, i))
            flush(0)
            kb.barrier()

        with contextlib.ExitStack() as p3:
            TWO_PI = 2.0 * math.pi
            prm = SB("prm", [128, 24, 16], F32, p3)
            Bpad = SB("Bpad", [128, 2, 16, 128], F32, p3)
            Cpad = SB("Cpad", [128, 3, 16, 128], F32, p3)
            dcol = SB("dcol", [128, 4], F32, p3)
            Dg = SB("Dg", [128, 4, 128], F32, p3)
            wglub = SB("wglub", [128, 4, 1024], BF16, p3)
            carry = SB("carry", [128, 16, 2], F32, p3)
            cs_tab = SB("cs_tab", [128, 16, 512], F32, p3)
            sn_tab = SB("sn_tab", [128, 16, 512], F32, p3)
            ctmp = SB("ctmp", [128, 4], F32, p3)
            p3s = contextlib.ExitStack()
            bnat = SB("bnat", [128, 2, 16, 16], F32, p3s)
            bbT = SB("bbT", [128, 2, 16, 16], F32, p3s)
            bbX = SB("bbX", [128, 128], F32, p3s)
            CT = SB("CT", [128, 2, 16, 16], F32, p3s)
            negpi = SB("negpi", [128, 1], F32, p3s)
            tpos = SB("tpos", [128, 512], F32, p3s)
            tq_ = SB("tq_", [128, 512], F32, p3s)
            tn_ = SB("tn_", [128, 512], F32, p3s)
            tc_ = SB("tc_", [128, 512], F32, p3s)
            tph = SB("tph", [128, 512], F32, p3s)

            P = lambda i: prm[:, i, :]
            LRE, LIM, STEP, MAG, ANG, ARE, AIM, DEN, NRE, CRE, CIM, FT, TA, TB, TC, F512, S512, C512 = range(18)
            kb.dma("sp", P(LRE), ssm_lre.rearrange("(gt gi) p -> gi p gt", gi=2)[0], "prm0", writes=["prm"],
                   allow_slow_non_contiguous=True) if False else None
            for gi in range(2):
                kb.dma("sp", prm[gi * 64:(gi + 1) * 64, LRE, :], ssm_lre.rearrange("(gt gi) p -> gi p gt", gi=2)[gi],
                       "prm", writes=["prm"], allow_slow_non_contiguous=True)
                kb.dma("sp", prm[gi * 64:(gi + 1) * 64, LIM, :], ssm_lim.rearrange("(gt gi) p -> gi p gt", gi=2)[gi],
                       "prm", writes=["prm"], allow_slow_non_contiguous=True)
                kb.dma("sp", prm[gi * 64:(gi + 1) * 64, STEP, :],
                       ssm_ls.rearrange("(gt gi) o -> gi (o gt)", gi=2)[gi:gi + 1, :].partition_broadcast(64),
                       "prm", writes=["prm"], allow_slow_non_contiguous=True)
                for ri, src in enumerate((ssm_bre, ssm_bim)):
                    kb.dma("sp", bnat[gi * 64:(gi + 1) * 64, ri, :, :],
                           src.rearrange("(gt gi) p h -> gi p gt h", gi=2)[gi], "bnat", writes=["bnat"])
                for ri, src in enumerate((ssm_cre, ssm_cim)):
                    for gt in range(16):
                        kb.dma("sp", CT[gi * 64:(gi + 1) * 64, ri, gt, :],
                               src[2 * gt + gi].rearrange("h p -> p h"), "CT", writes=["CT"],
                               allow_slow_non_contiguous=True)
            kb.dma("sp", dcol[:], ssm_d.rearrange("(cc p) o -> p (cc o)", p=128), "dcol", writes=["dcol"],
                   allow_slow_non_contiguous=True)
            for k in range(4):
                kb.dma("pool", wglub[:, k, :], w_glu[k * 128:(k + 1) * 128, :], ("wglub", k), writes=[("wglub", k)])
            kb.op("pool", lambda e: e.iota(tpos[:], pattern=[[1, 512]], base=0, channel_multiplier=0,
                                           allow_small_or_imprecise_dtypes=True), writes=["tpos"])
            kb.op("dve", lambda e: e.memset(negpi[:], -math.pi), writes=["negpi"])

            def V(fn, rd=("prm",), wr=("prm",)):
                kb.op("dve", fn, reads=list(rd), writes=list(wr))
            V(lambda e: e.tensor_scalar_min(out=P(LRE), in0=P(LRE), scalar1=-1e-4))
            kb.op("act", lambda e: e.activation(out=P(STEP), in_=P(STEP), func=AF.Exp), reads=["prm"], writes=["prm"])
            V(lambda e: e.tensor_tensor(out=P(TA), in0=P(LRE), in1=P(STEP), op=ALU.mult))
            kb.op("act", lambda e: e.activation(out=P(MAG), in_=P(TA), func=AF.Exp), reads=["prm"], writes=["prm"])
            V(lambda e: e.tensor_tensor(out=P(ANG), in0=P(LIM), in1=P(STEP), op=ALU.mult))
            MAGIC = 12582912.0

            def sincos(out_sin, out_cos, turns, tq, tn, tc, rk, wk):
                kb.op("pool", lambda e: e.tensor_scalar_add(out=tq, in0=turns, scalar1=MAGIC), reads=rk, writes=wk)
                kb.op("dve", lambda e: e.scalar_tensor_tensor(out=tn, in0=tq, scalar=MAGIC, in1=turns,
                                                              op0=ALU.subtract, op1=ALU.subtract), reads=rk + wk, writes=wk)
                kb.op("act", lambda e: e.activation(out=out_sin, in_=tn, func=AF.Sin, scale=-TWO_PI), reads=wk, writes=wk)
                kb.op("pool", lambda e: e.tensor_scalar_add(out=tc, in0=turns, scalar1=0.25), reads=rk + wk, writes=wk)
                kb.op("pool", lambda e: e.tensor_scalar_add(out=tq, in0=tc, scalar1=MAGIC), reads=wk, writes=wk)
                kb.op("dve", lambda e: e.scalar_tensor_tensor(out=tn, in0=tq, scalar=MAGIC, in1=tc,
                                                              op0=ALU.subtract, op1=ALU.subtract), reads=wk, writes=wk)
                kb.op("act", lambda e: e.activation(out=out_cos, in_=tn, func=AF.Sin, scale=-TWO_PI), reads=wk, writes=wk)

            V(lambda e: e.tensor_scalar_mul(out=P(FT), in0=P(ANG), scalar1=1.0 / TWO_PI))
            sincos(P(AIM), P(ARE), P(FT), P(TA), P(TB), P(TC), ["prm"], ["prm"])
            V(lambda e: e.tensor_scalar_mul(out=P(F512), in0=P(FT), scalar1=512.0))
            sincos(P(S512), P(C512), P(F512), P(TA), P(TB), P(TC), ["prm"], ["prm"])
            V(lambda e: e.tensor_tensor(out=P(AIM), in0=P(AIM), in1=P(MAG), op=ALU.mult))
            V(lambda e: e.tensor_tensor(out=P(ARE), in0=P(ARE), in1=P(MAG), op=ALU.mult))
            V(lambda e: e.tensor_tensor(out=P(DEN), in0=P(LRE), in1=P(LRE), op=ALU.mult))
            V(lambda e: e.tensor_tensor(out=P(TA), in0=P(LIM), in1=P(LIM), op=ALU.mult))
            V(lambda e: e.tensor_tensor(out=P(DEN), in0=P(DEN), in1=P(TA), op=ALU.add))
            V(lambda e: e.reciprocal(out=P(DEN), in_=P(DEN)))
            V(lambda e: e.tensor_scalar_add(out=P(NRE), in0=P(ARE), scalar1=-1.0))
            V(lambda e: e.tensor_tensor(out=P(TA), in0=P(NRE), in1=P(LRE), op=ALU.mult))
            V(lambda e: e.tensor_tensor(out=P(TB), in0=P(AIM), in1=P(LIM), op=ALU.mult))
            V(lambda e: e.tensor_tensor(out=P(TA), in0=P(TA), in1=P(TB), op=ALU.add))
            V(lambda e: e.tensor_tensor(out=P(CRE), in0=P(TA), in1=P(DEN), op=ALU.mult))
            V(lambda e: e.tensor_tensor(out=P(TA), in0=P(AIM), in1=P(LRE), op=ALU.mult))
            V(lambda e: e.tensor_tensor(out=P(TB), in0=P(NRE), in1=P(LIM), op=ALU.mult))
            V(lambda e: e.tensor_tensor(out=P(TA), in0=P(TA), in1=P(TB), op=ALU.subtract))
            V(lambda e: e.tensor_tensor(out=P(CIM), in0=P(TA), in1=P(DEN), op=ALU.mult))
            for gt in range(16):
                cre = prm[:, CRE, gt:gt + 1]
                cim = prm[:, CIM, gt:gt + 1]
                V(lambda e: e.tensor_scalar_mul(out=bbT[:, 0, gt, :], in0=bnat[:, 1, gt, :], scalar1=cim),
                  rd=("prm", "bnat"), wr=("bbT",))
                V(lambda e: e.scalar_tensor_tensor(out=bbT[:, 0, gt, :], in0=bnat[:, 0, gt, :], scalar=cre,
                                                   in1=bbT[:, 0, gt, :], op0=ALU.mult, op1=ALU.subtract),
                  rd=("prm", "bnat", "bbT"), wr=("bbT",))
                V(lambda e: e.tensor_scalar_mul(out=bbT[:, 1, gt, :], in0=bnat[:, 0, gt, :], scalar1=cim),
                  rd=("prm", "bnat"), wr=("bbT",))
                V(lambda e: e.scalar_tensor_tensor(out=bbT[:, 1, gt, :], in0=bnat[:, 1, gt, :], scalar=cre,
                                                   in1=bbT[:, 1, gt, :], op0=ALU.mult, op1=ALU.add),
                  rd=("prm", "bnat", "bbT"), wr=("bbT",))
            kb.op("pool", lambda e: e.memset(Cpad[:], 0.0), writes=["Cpad"])
            for gt in range(16):
                for ri in range(2):
                    V(lambda e: e.memset(bbX[:], 0.0), rd=(), wr=("bbX",))
                    for gi in range(2):
                        c0 = (gt % 4) * 32 + gi * 16
                        V(lambda e: e.tensor_copy(out=bbX[gi * 64:(gi + 1) * 64, c0:c0 + 16],
                                                  in_=bbT[gi * 64:(gi + 1) * 64, ri, gt, :]),
                          rd=("bbT", "bbX"), wr=("bbX",))
                    pi = (gt * 2 + ri) % 4
                    kb.op("pe", lambda e: e.transpose(ps[pi][:, 0:128], bbX[:], ident[:]), reads=["bbX"],
                          writes=[("ps", pi)])
                    kb.op("act", lambda e: e.copy(out=Bpad[:, ri, gt, :], in_=ps[pi][:, 0:128]),
                          reads=[("ps", pi)], writes=["Bpad"])
                for gi in range(2):
                    c0 = (gt % 4) * 32 + gi * 16
                    rows = slice(gi * 64, (gi + 1) * 64)
                    kb.op("pool", lambda e: e.tensor_copy(out=Cpad[rows, 0, gt, c0:c0 + 16], in_=CT[rows, 0, gt, :]),
                          reads=["CT", "Cpad"], writes=["Cpad"])
                    kb.op("pool", lambda e: e.tensor_scalar_mul(out=Cpad[rows, 1, gt, c0:c0 + 16],
                                                                in0=CT[rows, 0, gt, :], scalar1=-1.0),
                          reads=["CT", "Cpad"], writes=["Cpad"])
                    kb.op("pool", lambda e: e.tensor_scalar_mul(out=Cpad[rows, 2, gt, c0:c0 + 16],
                                                                in0=CT[rows, 1, gt, :], scalar1=-1.0),
                          reads=["CT", "Cpad"], writes=["Cpad"])
            for cc in range(4):
                V(lambda e: e.tensor_scalar_mul(out=Dg[:, cc, :], in0=ident[:], scalar1=dcol[:, cc:cc + 1]),
                  rd=("dcol",), wr=("Dg",))

            for gt in range(16):
                kb.op("dve", lambda e: e.tensor_scalar_mul(out=tph[:], in0=tpos[:], scalar1=prm[:, FT, gt:gt + 1]),
                      reads=["tpos", "prm", "tabw"], writes=["tph"])
                sincos(sn_tab[:, gt, :], cs_tab[:, gt, :], tph[:], tq_[:], tn_[:], tc_[:], ["tph"], ["tabw"])

            kb.barrier()
            p3s.close()
            uTt = [SB("uTt%d" % i, [128, 4, 512], F32, p3) for i in range(2)]
            W = {}
            for nm in ("t1", "t2", "t3", "t4", "bre", "bim", "wre", "wim", "p1", "p2", "p3", "p4"):
                W[nm] = [SB("w_%s%d" % (nm, i), [128, 512], F32, p3) for i in range(2)]
            ysb = [SB("ysb0", [128, 512], F32, p3)] * 2
            gt1 = [SB("gt1_0", [128, 512], F32, p3)] * 2
            gth = [SB("gth0", [128, 512], F32, p3)] * 2
            gyT = [SB("gyT%d" % i, [128, 4, 512], BF16, p3) for i in range(2)]
            sgt = [SB("sgt0", [128, 512], F32, p3)] * 2
            ysT = [SB("ysT0", [128, 4, 512], BF16, p3)] * 2
            TT = lambda e, o, a, b_, op: e.tensor_tensor(out=o, in0=a, in1=b_, op=op)

            def ssm_s1(us, cc, gt, s2):
                uk = ("uTt", us)
                pa, pb_ = ps[s2 * 2], ps[s2 * 2 + 1]
                pak, pbk = ("ps", s2 * 2), ("ps", s2 * 2 + 1)
                kb.op("pe", lambda e: e.matmul(pa[:], lhsT=Bpad[:, 0, gt, :], rhs=uTt[us][:, cc, :],
                                               start=True, stop=True), reads=["Bpad", uk], writes=[pak])
                kb.op("pe", lambda e: e.matmul(pb_[:], lhsT=Bpad[:, 1, gt, :], rhs=uTt[us][:, cc, :],
                                               start=True, stop=True), reads=["Bpad", uk], writes=[pbk])

            def ssm_s23(gt, s2, kb):
                w = {k_: v_[s2] for k_, v_ in W.items()}
                K_ = lambda nm: (nm, s2)
                cs_, sn_ = cs_tab[:, gt, :], sn_tab[:, gt, :]
                pa, pb_ = ps[s2 * 2], ps[s2 * 2 + 1]
                pak, pbk = ("ps", s2 * 2), ("ps", s2 * 2 + 1)
                kb.op("dve", lambda e: TT(e, w["t1"][:], pa[:], cs_, ALU.mult), reads=[pak, "tabw"], writes=[K_("t1")])
                kb.op("dve", lambda e: TT(e, w["t2"][:], pb_[:], sn_, ALU.mult), reads=[pbk, "tabw"], writes=[K_("t2")])
                kb.op("dve", lambda e: TT(e, w["t3"][:], pb_[:], cs_, ALU.mult), reads=[pbk, "tabw"], writes=[K_("t3")])
                kb.op("dve", lambda e: TT(e, w["t4"][:], pa[:], sn_, ALU.mult), reads=[pak, "tabw"], writes=[K_("t4")])
                kb.op("dve", lambda e: TT(e, w["bre"][:], w["t1"][:], w["t2"][:], ALU.add),
                      reads=[K_("t1"), K_("t2")], writes=[K_("bre")])
                kb.op("dve", lambda e: TT(e, w["bim"][:], w["t3"][:], w["t4"][:], ALU.subtract),
                      reads=[K_("t3"), K_("t4")], writes=[K_("bim")])

            def ssm_back(gt, s2, yb, first, kb):
                w = {k_: v_[s2] for k_, v_ in W.items()}
                K_ = lambda nm: (nm, s2)
                cs_, sn_ = cs_tab[:, gt, :], sn_tab[:, gt, :]
                for ri, (bn, wn) in enumerate((("bre", "wre"), ("bim", "wim"))):
                    kb.op("dve", lambda e, ri=ri, bn=bn, wn=wn: e.tensor_tensor_scan(
                        out=w[wn][:], data0=prm[:, MAG, gt:gt + 1].to_broadcast([128, 512]),
                        data1=w[bn][:], initial=carry[:, gt, ri:ri + 1], op0=ALU.mult, op1=ALU.add),
                          reads=[K_(bn), "prm", ("carry", gt)], writes=[K_(wn)])
                wl_re, wl_im = w["wre"][:, 511:512], w["wim"][:, 511:512]
                c5, s5 = prm[:, C512, gt:gt + 1], prm[:, S512, gt:gt + 1]
                kb.op("dve", lambda e: e.tensor_tensor(out=ctmp[:, 0:1], in0=wl_im, in1=s5, op=ALU.mult),
                      reads=[K_("wim"), "prm", "ctmp"], writes=["ctmp"])
                kb.op("dve", lambda e: e.tensor_tensor(out=ctmp[:, 1:2], in0=wl_im, in1=c5, op=ALU.mult),
                      reads=[K_("wim"), "prm", "ctmp"], writes=["ctmp"])
                kb.op("dve", lambda e: e.scalar_tensor_tensor(out=carry[:, gt, 0:1], in0=wl_re, scalar=c5,
                                                              in1=ctmp[:, 0:1], op0=ALU.mult, op1=ALU.subtract),
                      reads=[K_("wre"), "prm", "ctmp", ("carry", gt)], writes=[("carry", gt)])
                kb.op("dve", lambda e: e.scalar_tensor_tensor(out=carry[:, gt, 1:2], in0=wl_re, scalar=s5,
                                                              in1=ctmp[:, 1:2], op0=ALU.mult, op1=ALU.add),
                      reads=[K_("wre"), "prm", "ctmp", ("carry", gt)], writes=[("carry", gt)])
                kb.op("dve", lambda e: TT(e, w["p1"][:], cs_, w["wre"][:], ALU.mult),
                      reads=["tabw", K_("wre")], writes=[K_("p1")])
                kb.op("pool", lambda e: TT(e, w["p2"][:], sn_, w["wim"][:], ALU.mult),
                      reads=["tabw", K_("wim")], writes=[K_("p2")])
                kb.op("dve", lambda e: TT(e, w["p3"][:], sn_, w["wre"][:], ALU.mult),
                      reads=["tabw", K_("wre")], writes=[K_("p3")])
                kb.op("pool", lambda e: TT(e, w["p4"][:], cs_, w["wim"][:], ALU.mult),
                      reads=["tabw", K_("wim")], writes=[K_("p4")])
                for qi, (pn, ci) in enumerate((("p1", 0), ("p2", 1), ("p3", 2), ("p4", 2))):
                    kb.op("pe", lambda e, qi=qi, pn=pn, ci=ci: e.matmul(ps[yb][:], lhsT=Cpad[:, ci, gt, :], rhs=w[pn][:],
                                                                        start=(first and qi == 0), stop=False),
                          reads=["Cpad", K_(pn)], writes=[("ps", yb)])

            def ssm_ccend(us, cc, yb):
                uk = ("uTt", us)
                kb.op("pe", lambda e: e.matmul(ps[yb][:], lhsT=Dg[:, cc, :], rhs=uTt[us][:, cc, :],
                                               start=False, stop=True),
                      reads=["Dg", uk], writes=[("ps", yb)])
                kb.op("act", lambda e: e.activation(out=gyT[us][:, cc, :], in_=ps[yb][:], func=AF.Gelu_apprx_tanh),
                      reads=[("ps", yb)], writes=[("gyT", us, cc)])

            def ssm_glu(b, n, us):
                for m in range(4):
                    for half in range(2):
                        pi = 6 + half
                        mm = m + 4 * half
                        for k in range(4):
                            kb.op("pe", lambda e: e.matmul(ps[pi][:], lhsT=wglub[:, k, mm * 128:(mm + 1) * 128],
                                                           rhs=gyT[us][:, k, :], start=(k == 0), stop=(k == 3)),
                                  reads=[("wglub", k), ("gyT", us, k)], writes=[("ps", pi)])
                    sg_ = sgt[m % 2]
                    kb.op("act", lambda e: e.activation(out=sg_[:], in_=ps[7][:], func=AF.Sigmoid),
                          reads=[("ps", 7)], writes=[("sgt", 0)])
                    kb.op("dve", lambda e: e.tensor_tensor(out=ysT[us][:, m, :], in0=ps[6][:], in1=sg_[:],
                                                           op=ALU.mult),
                          reads=[("ps", 6), ("sgt", 0)], writes=[("ysT", 0, m)])
                kb.dma("act", ysT_s[b, :, :, n * 512:(n + 1) * 512].rearrange("m p t -> p m t"), ysT[us][:],
                       ("ysT", 0), reads=[("ysT", 0, m) for m in range(4)])

            items = [(b, n, cc, g4) for b in range(NS) for n in range(NG) for cc in range(4) for g4 in range(4)]
            NI = len(items)

            def do_s1(idx):
                b, n, cc, g4 = items[idx]
                us = (b * NG + n) % 2
                if cc == 0 and g4 == 0:
                    uk = ("uTt", us)
                    kb.dma("sp", uTt[us][:], uT_s[b, :, :, n * 512:(n + 1) * 512].rearrange("m p t -> p m t"), uk,
                           writes=[uk])
                ssm_s1(us, cc, cc * 4 + g4, idx % 2)

            def do_s23(idx, kb_):
                b, n, cc, g4 = items[idx]
                ssm_s23(cc * 4 + g4, idx % 2, kb_)

            do_s1(0)
            if NI > 1:
                do_s1(1)
            do_s23(0, kb)
            for idx in range(NI):
                b, n, cc, g4 = items[idx]
                us = (b * NG + n) % 2
                if idx + 2 < NI:
                    do_s1(idx + 2)
                rfront = Rec()
                if idx + 1 < NI:
                    do_s23(idx + 1, rfront)
                if n == 0 and cc == 0 and g4 == 0:
                    kb.op("dve", lambda e: e.memset(carry[:], 0.0),
                          reads=["carry"] + [("carry", g_) for g_ in range(16)],
                          writes=["carry"] + [("carry", g_) for g_ in range(16)])
                yb = 4 + ((b * NG + n) * 4 + cc) % 2
                rback = Rec()
                ssm_back(cc * 4 + g4, idx % 2, yb, g4 == 0, rback)
                emit_interleaved(kb, rback.L, rfront.L)
                if g4 == 3:
                    ssm_ccend(us, cc, yb)
                    if cc == 3:
                        ssm_glu(b, n, us)
            kb.barrier()

        with contextlib.ExitStack() as p4:
            wpab = SB("wpab", [128, 4, 1024], BF16, p4)
            wpsb = SB("wpsb", [128, 4, 1024], BF16, p4)
            woutb = SB("woutb", [128, 8, 1024], BF16, p4)
            wr = SB("wr", [128, 8, 36], F32, p4)
            br_bc = SB("br_bc", [128, 36], F32, p4)
            g2bc = SB("g2bc", [128, D], F32, p4)
            gate1 = SB("gate1", [128, NS, D], F32, p4)
            yaT_t = [SB("yaT_t%d" % i, [128, 4, 512], BF16, p4) for i in range(2)]
            ysT_t = [SB("ysT_t%d" % i, [128, 4, 512], BF16, p4) for i in range(2)]
            sg_t = [SB("sg_t%d" % i, [128, 16, 512], BF16, p4) for i in range(2)]
            mga = [SB("mga%d" % i, [128, 512], F32, p4) for i in range(2)]
            mgb = [SB("mgb%d" % i, [128, 512], F32, p4) for i in range(2)]
            mrgT = SB("mrgT", [128, 8, 512], BF16, p4)
            x4 = [SB("x4_%d" % i, [128, D], F32, p4) for i in range(3)]
            xm = [SB("xm%d" % i, [128, D], F32, p4) for i in range(2)]
            xn2 = [SB("xn2_%d" % i, [128, D], F32, p4) for i in range(2)]
            junk4 = SB("junk4", [128, D], F32, p4)
            st4 = [SB("st4_%d" % i, [128, 8], F32, p4) for i in range(2)]
            h2f = [SB("h2f%d" % i, [128, 8, 128], F32, p4) for i in range(2)]
            rt = [SB("rt%d" % i, [128, 160], F32, p4) for i in range(2)]
            mod2 = SB("mod2", [128, NS, 2, D], F32, p4)
            h2t = [SB("h2t%d" % i, [128, D], F32, p4) for i in range(2)]
            Sprev = SB("Sprev", [128, 64], F32, p4)
            iota_e = SB("iota_e", [128, 64], F32, p4)
            iota_p = SB("iota_p", [128, 1], F32, p4)
            tmp64 = [SB("tmp64_%d" % i, [128, 64], F32, p4) for i in range(2)]
            rtt = [SB("rtt%d" % i, [128, 8], F32, p4) for i in range(2)]
            for b in range(NS):
                kb.dma("sp", mod2[:, b, :, :], gate_s[:, (b * 4 + 2) * D:(b * 4 + 4) * D].rearrange("p (g d) -> p g d", g=2),
                       "mod2", writes=["mod2"])
            kb.op("dve", lambda e: e.memset(Sprev[:], 0.0), writes=["Sprev"])
            kb.op("pool", lambda e: e.iota(iota_e[:], pattern=[[0, 2], [1, 32]], base=0, channel_multiplier=0,
                                           allow_small_or_imprecise_dtypes=True), writes=["iota_e"])
            kb.op("pool", lambda e: e.iota(iota_p[:], pattern=[[0, 1]], base=0, channel_multiplier=1,
                                           allow_small_or_imprecise_dtypes=True), writes=["iota_p"])

            for k in range(4):
                kb.dma("pool", wpab[:, k, :], w_pa[k * 128:(k + 1) * 128, :], ("wpab", k), writes=[("wpab", k)])
                kb.dma("pool", wpsb[:, k, :], w_ps[k * 128:(k + 1) * 128, :], ("wpsb", k), writes=[("wpsb", k)])
            for k in range(8):
                kb.dma("pool", woutb[:, k, :], w_out[k * 128:(k + 1) * 128, :], ("woutb", k), writes=[("woutb", k)])
            kb.dma("sp", wr[:, :, 0:4], w_rg.rearrange("(k p) n -> p k n", p=128), "wr", writes=["wr"],
                   allow_slow_non_contiguous=True)
            for g in range(4):
                kb.dma("sp", wr[:, :, 4 + g * 8:12 + g * 8], w_re[g].rearrange("(k p) n -> p k n", p=128), "wr",
                       writes=["wr"], allow_slow_non_contiguous=True)
            kb.dma("sp", br_bc[:, 0:4], b_rg.partition_broadcast(128), "br_bc", writes=["br_bc"])
            kb.dma("sp", br_bc[:, 4:36], b_re.partition_broadcast(128), "br_bc", writes=["br_bc"])
            kb.dma("sp", g2bc[:], norm2_g.partition_broadcast(128), "g2bc", writes=["g2bc"])
            for b in range(NS):
                kb.dma("sp", gate1[:, b, :], gate_s[:, (b * 4) * D:(b * 4 + 1) * D], "gate1", writes=["gate1"])

            mcnt = [0]

            def p4_loadg(b, n):
                gs = (b * NG + n) % 2
                c0 = n * 512
                kb.dma("sp", yaT_t[gs][:], yaT_s[b, :, :, c0:c0 + 512].rearrange("m p t -> p m t"), ("yaT_t", gs),
                       writes=[("yaT_t", gs)])
                kb.dma("sp", ysT_t[gs][:], ysT_s[b, :, :, c0:c0 + 512].rearrange("m p t -> p m t"), ("ysT_t", gs),
                       writes=[("ysT_t", gs)])
                kb.dma("sp", sg_t[gs][:], sg_s[b, :, :, c0:c0 + 512].rearrange("m p t -> p m t"), ("sg_t", gs),
                       writes=[("sg_t", gs)])

            def p4_M(b, n):
                gs = (b * NG + n) % 2
                c0 = n * 512
                for m in range(8):
                    ia, ib = 4 + mcnt[0] % 3, 4 + (mcnt[0] + 1) % 3
                    mcnt[0] += 2
                    pA, pB = ps[ia], ps[ib]
                    pAk, pBk = ("ps", ia), ("ps", ib)
                    for k in range(4):
                        kb.op("pe", lambda e: e.matmul(pA[:], lhsT=wpab[:, k, m * 128:(m + 1) * 128],
                                                       rhs=yaT_t[gs][:, k, :], start=(k == 0), stop=(k == 3)),
                              reads=[("wpab", k), ("yaT_t", gs)], writes=[pAk])
                    for k in range(4):
                        kb.op("pe", lambda e: e.matmul(pB[:], lhsT=wpsb[:, k, m * 128:(m + 1) * 128],
                                                       rhs=ysT_t[gs][:, k, :], start=(k == 0), stop=(k == 3)),
                              reads=[("wpsb", k), ("ysT_t", gs)], writes=[pBk])
                    kb.op("dve", lambda e: e.tensor_tensor(out=mga[m % 2][:], in0=pA[:], in1=sg_t[gs][:, m, :],
                                                           op=ALU.mult),
                          reads=[pAk, ("sg_t", gs)], writes=[("mga", m % 2)])
                    kb.op("dve", lambda e: e.tensor_tensor(out=mgb[m % 2][:], in0=pB[:], in1=sg_t[gs][:, 8 + m, :],
                                                           op=ALU.mult),
                          reads=[pBk, ("sg_t", gs)], writes=[("mgb", m % 2)])
                    kb.op("dve", lambda e: e.tensor_tensor(out=mrgT[:, m, :], in0=mga[m % 2][:], in1=mgb[m % 2][:],
                                                            op=ALU.add),
                          reads=[("mga", m % 2), ("mgb", m % 2)], writes=[("mrgT", m)])

            def p4_loadx(idx):
                b, n, t = tiles4[idx]
                tok0 = n * 512 + t * 128
                kb.dma("sp", x4[idx % 3][:], x[b, tok0:tok0 + 128, :], ("x4", idx % 3), writes=[("x4", idx % 3)])

            def p4_A(b, n, t, sl, xsl):
                gs = (b * NG + n) % 2
                c0 = n * 512
                tok0 = c0 + t * 128
                st = st4[sl]
                for half in range(2):
                    pi = half
                    for k in range(8):
                        kb.op("pe", lambda e: e.matmul(ps[pi][:], lhsT=mrgT[:, k, t * 128:(t + 1) * 128],
                                                       rhs=woutb[:, k, half * 512:(half + 1) * 512],
                                                       start=(k == 0), stop=(k == 7)),
                              reads=[("mrgT", k), ("woutb", k)], writes=[("ps", pi)])
                    hs = slice(half * 512, (half + 1) * 512)
                    kb.op("dve", lambda e: e.tensor_tensor(out=xm[sl][:, hs], in0=ps[pi][:], in1=gate1[:, b, hs],
                                                           op=ALU.mult),
                          reads=[("ps", pi), "gate1"], writes=[("xm", sl, half)])
                    kb.op("dve", lambda e: e.tensor_tensor(out=xm[sl][:, hs], in0=xm[sl][:, hs],
                                                            in1=x4[xsl][:, hs], op=ALU.add),
                          reads=[("xm", sl, half), ("x4", xsl)], writes=[("xm", sl, half)])
                xmk = [("xm", sl, 0), ("xm", sl, 1)]
                kb.dma("sp", xmid_s[b, tok0:tok0 + 128, :], xm[sl][:], ("xm", sl), reads=xmk)
                kb.op("act", lambda e: e.activation(out=junk4[:], in_=xm[sl][:], func=AF.Square,
                                                    accum_out=st[:, 0:1]),
                      reads=xmk, writes=["junk4", ("st4", sl)])
                kb.op("dve", lambda e: e.tensor_scalar(out=st[:, 1:2], in0=st[:, 0:1], scalar1=1.0 / D,
                                                       scalar2=1e-6, op0=ALU.mult, op1=ALU.add),
                      reads=[("st4", sl)], writes=[("st4", sl)])
                kb.op("act", lambda e: e.activation(out=st[:, 2:3], in_=st[:, 1:2], func=AF.Ln),
                      reads=[("st4", sl)], writes=[("st4", sl)])
                kb.op("act", lambda e: e.activation(out=st[:, 3:4], in_=st[:, 2:3], func=AF.Exp, scale=-0.5),
                      reads=[("st4", sl)], writes=[("st4", sl)])
                kb.op("dve", lambda e: e.scalar_tensor_tensor(out=xn2[sl][:], in0=xm[sl][:], scalar=st[:, 3:4],
                                                              in1=g2bc[:], op0=ALU.mult, op1=ALU.mult),
                      reads=xmk + [("st4", sl), "g2bc"], writes=[("xn2", sl)])
                for k in range(8):
                    pi = 2 + k // 4
                    kb.op("pe", lambda e: e.transpose(ps[pi][:, (k % 4) * 128:(k % 4 + 1) * 128],
                                                      xn2[sl][:, k * 128:(k + 1) * 128], ident[:]),
                          reads=[("xn2", sl)], writes=[("ps", pi)])
                for k in range(8):
                    pi = 2 + k // 4
                    kb.op("act", lambda e: e.activation(out=h2f[sl][:, k, :],
                                                        in_=ps[pi][:, (k % 4) * 128:(k % 4 + 1) * 128],
                                                        func=AF.Identity, scale=modT[:, 32 + k, b:b + 1],
                                                        bias=modT[:, 24 + k, b:b + 1]),
                          reads=[("ps", pi)], writes=[("h2f", sl)])
                kb.op("dve", lambda e: e.tensor_tensor(out=h2t[sl][:], in0=xn2[sl][:], in1=mod2[:, b, 1, :],
                                                       op=ALU.mult),
                      reads=[("xn2", sl), "mod2"], writes=[("h2t", sl)])
                kb.op("dve", lambda e: e.tensor_tensor(out=h2t[sl][:], in0=h2t[sl][:], in1=mod2[:, b, 0, :],
                                                        op=ALU.add),
                      reads=[("h2t", sl), "mod2"], writes=[("h2t", sl)])
                kb.dma("sp", h2tok_s[b * S + tok0:b * S + tok0 + 128, :], h2t[sl][:], ("h2t", sl),
                       reads=[("h2t", sl)])

            def p4_B(b, n, t, sl, kb):
                gs = (b * NG + n) % 2
                c0 = n * 512
                tok0 = c0 + t * 128
                st = st4[sl]
                R = rt[sl]
                rk = ("rt", sl)
                bkB = 7 if sl == 0 else 3
                pr = ps[bkB][:, 0:128]
                for k in range(8):
                    kb.op("pe", lambda e, k=k: e.matmul(pr[:, 0:36], lhsT=h2f[sl][:, k, :], rhs=wr[:, k, :],
                                                        start=(k == 0), stop=(k == 7)),
                          reads=[("h2f", sl), "wr"], writes=[("ps", bkB)])

                def RV(fn, extra=()):
                    kb.op("dve", fn, reads=[rk] + list(extra), writes=[rk])
                L = R[:, 0:36]
                RV(lambda e: e.tensor_tensor(out=L, in0=pr[:, 0:36], in1=br_bc[:], op=ALU.add),
                   extra=[("ps", bkB), "br_bc"])
                RV(lambda e: e.reduce_max(out=R[:, 36:37], in_=R[:, 0:4], axis=AX.X))
                RV(lambda e: e.tensor_scalar(out=R[:, 40:44], in0=R[:, 0:4], scalar1=R[:, 36:37], scalar2=None,
                                             op0=ALU.is_equal))
                RV(lambda e: e.tensor_scalar_mul(out=R[:, 37:38], in0=R[:, 36:37], scalar1=-1.0))
                kb.op("act", lambda e: e.activation(out=R[:, 44:48], in_=R[:, 0:4], func=AF.Exp,
                                                    bias=R[:, 37:38], accum_out=R[:, 38:39]),
                      reads=[rk], writes=[rk])
                RV(lambda e: e.reciprocal(out=R[:, 39:40], in_=R[:, 38:39]))
                RV(lambda e: e.tensor_scalar(out=R[:, 48:52], in0=R[:, 40:44], scalar1=-1.0, scalar2=1e30,
                                             op0=ALU.add, op1=ALU.mult))
                for g in range(4):
                    RV(lambda e, g=g: e.tensor_scalar(out=R[:, 64 + g * 8:72 + g * 8], in0=R[:, 4 + g * 8:12 + g * 8],
                                                      scalar1=R[:, 48 + g:49 + g], scalar2=None, op0=ALU.add))
                LM = R[:, 64:96]
                RV(lambda e: e.reduce_max(out=R[:, 52:53], in_=LM, axis=AX.X))
                RV(lambda e: e.tensor_scalar(out=R[:, 96:128], in0=LM, scalar1=R[:, 52:53], scalar2=None,
                                             op0=ALU.is_equal))
                RV(lambda e: e.scalar_tensor_tensor(out=R[:, 128:160], in0=R[:, 96:128], scalar=-1e30, in1=LM,
                                                    op0=ALU.mult, op1=ALU.add))
                RV(lambda e: e.reduce_max(out=R[:, 53:54], in_=R[:, 128:160], axis=AX.X))
                RV(lambda e: e.tensor_scalar(out=R[:, 64:96], in0=R[:, 128:160], scalar1=R[:, 53:54],
                                             scalar2=None, op0=ALU.is_equal))
                RV(lambda e: e.tensor_tensor(out=R[:, 54:55], in0=R[:, 53:54], in1=R[:, 52:53], op=ALU.subtract))
                kb.op("act", lambda e: e.activation(out=R[:, 55:56], in_=R[:, 54:55], func=AF.Exp),
                      reads=[rk], writes=[rk])
                RV(lambda e: e.tensor_scalar_add(out=R[:, 56:57], in0=R[:, 55:56], scalar1=1.0))
                RV(lambda e: e.reciprocal(out=R[:, 57:58], in_=R[:, 56:57]))
                RV(lambda e: e.tensor_tensor(out=R[:, 58:59], in0=R[:, 57:58], in1=R[:, 55:56], op=ALU.mult))
                RV(lambda e: e.tensor_tensor(out=R[:, 59:60], in0=R[:, 57:58], in1=R[:, 39:40], op=ALU.mult))
                RV(lambda e: e.tensor_tensor(out=R[:, 60:61], in0=R[:, 58:59], in1=R[:, 39:40], op=ALU.mult))
                OH = R[:, 64:128]
                pc = ps[bkB][:, 64:128]
                if hasattr(kb, "L"):
                    kb.split = len(kb.L)
                kb.op("pe", lambda e: e.matmul(pc, lhsT=Tri[:], rhs=OH, start=True, stop=False),
                      reads=[rk, "Tri"], writes=[("ps", bkB)])
                kb.op("pe", lambda e: e.matmul(pc, lhsT=ones[:], rhs=Sprev[:], start=False, stop=True),
                      reads=["Sprev", "ones"], writes=[("ps", bkB)])
                T64 = tmp64[sl]
                RTT = rtt[sl]
                tk, rtk = ("tmp64", sl), ("rtt", sl)
                kb.op("dve", lambda e: e.tensor_tensor(out=T64[:], in0=pc, in1=OH, op=ALU.mult),
                      reads=[("ps", bkB), rk], writes=[tk])
                kb.op("dve", lambda e: e.reduce_sum(out=RTT[:, 2:4], in_=T64[:].rearrange("p (s e) -> p s e", s=2),
                                                    axis=AX.X), reads=[tk], writes=[rtk])
                kb.op("dve", lambda e: e.tensor_scalar_add(out=RTT[:, 2:4], in0=RTT[:, 2:4], scalar1=-1.0),
                      reads=[rtk], writes=[rtk])
                kb.op("dve", lambda e: e.tensor_tensor(out=T64[:], in0=iota_e[:], in1=OH, op=ALU.mult),
                      reads=["iota_e", rk, tk], writes=[tk])
                kb.op("dve", lambda e: e.reduce_sum(out=RTT[:, 0:2], in_=T64[:].rearrange("p (s e) -> p s e", s=2),
                                                    axis=AX.X), reads=[tk, rtk], writes=[rtk])
                kb.op("dve", lambda e: e.tensor_copy(out=RTT[:, 4:5], in_=R[:, 60:61]), reads=[rk, rtk], writes=[rtk])
                kb.op("dve", lambda e: e.tensor_copy(out=RTT[:, 5:6], in_=R[:, 59:60]), reads=[rk, rtk], writes=[rtk])
                kb.op("dve", lambda e: e.tensor_scalar_add(out=RTT[:, 6:7], in0=iota_p[:],
                                                           scalar1=float(b * S + tok0)),
                      reads=["iota_p", rtk], writes=[rtk])
                kb.op("dve", lambda e: e.memset(RTT[:, 7:8], 0.0), reads=[rtk], writes=[rtk])
                kb.op("dve", lambda e: e.tensor_tensor(out=Sprev[:], in0=Sprev[:], in1=OH, op=ALU.add),
                      reads=["Sprev", rk], writes=["Sprev"])
                return functools.partial(kb_real.dma, "sp", rt_s[b * S + tok0:b * S + tok0 + 128, :], RTT[:], rtk,
                                         reads=[rtk])

            kb_real = kb
            tiles4 = [(b, n, t) for b in range(NS) for n in range(NG) for t in range(4)]
            groups4 = [(b, n) for b in range(NS) for n in range(NG)]
            NT4 = len(tiles4)
            p4_loadg(*groups4[0])
            p4_loadx(0)
            if NT4 > 1:
                p4_loadx(1)
            late = []
            for ti in range(0, NT4, 2):
                pair = [ti] + ([ti + 1] if ti + 1 < NT4 else [])
                for tj in pair:
                    b, n, t = tiles4[tj]
                    if tj + 2 < NT4:
                        p4_loadx(tj + 2)
                    if t == 1 and tj // 4 + 1 < len(groups4):
                        p4_loadg(*groups4[tj // 4 + 1])
                    if t == 0:
                        p4_M(b, n)
                    p4_A(b, n, t, tj % 2, tj % 3)
                for f in late:
                    f()
                late = []
                recs = []
                for tj in pair:
                    b, n, t = tiles4[tj]
                    r_ = Rec()
                    late.append(p4_B(b, n, t, tj % 2, r_))
                    recs.append(r_)
                if len(recs) == 2:
                    emit_interleaved(kb, recs[0].L[:recs[0].split], recs[1].L[:recs[1].split])
                    emit_interleaved(kb, recs[0].L[recs[0].split:], [])
                    emit_interleaved(kb, recs[1].L[recs[1].split:], [])
                else:
                    emit_interleaved(kb, recs[0].L, [])
            for f in late:
                f()
            kb.op("pe", lambda e: e.matmul(ps[1][:, 64:128], lhsT=ones[:], rhs=Sprev[:], start=True, stop=True),
                  reads=["Sprev", "ones"], writes=[("ps", 1)])
            kb.op("dve", lambda e: e.tensor_copy(out=cnt_bc[:], in_=ps[1][:, 64:128]), reads=[("ps", 1)],
                  writes=["cnt_bc"])
            kb.barrier()

        with contextlib.ExitStack() as p4b:
            I32 = mybir.dt.int32
            MAGIC_ = 12582912.0
            NRI = NB * BLK // 128
            rinit = SB("rinit", [128, NRI, 4], F32, p4b)
            bt = SB("bt", [128, 8, 32], F32, p4b)
            base = SB("base", [128, 64], F32, p4b)
            ebt = SB("ebt", [128, NB], F32, p4b)
            eb2 = SB("eb2", [128, 2, NB], F32, p4b)
            kp = SB("kp", [128, 12], F32, p4b)
            iota32 = SB("iota32", [128, 32], F32, p4b)
            rti = [SB("rti%d" % i, [128, 8], F32, p4b) for i in range(2)]
            ohs = [SB("ohs%d" % i, [128, 64], F32, p4b) for i in range(2)]
            dstf = [SB("dstf%d" % i, [128, 2], F32, p4b) for i in range(2)]
            dsti = [SB("dsti%d" % i, [128, 2], I32, p4b) for i in range(2)]
            pay = [SB("pay%d" % i, [128, 2, 4], F32, p4b) for i in range(2)]
            kb.op("pool", lambda e: e.memset(rinit[:], 0.0), writes=["rinit"])
            trash = SB("trash", [128, NRI], F32, p4b)
            kb.op("pool", lambda e: e.iota(trash[:], pattern=[[0, NRI]], base=2 * TT_, channel_multiplier=1,
                                           allow_small_or_imprecise_dtypes=True), writes=["trash"])
            kb.op("pool", lambda e: e.tensor_copy(out=rinit[:, :, 2], in_=trash[:]), reads=["rinit", "trash"],
                  writes=["rinit"])
            kb.dma("sp", rowtab_s.rearrange("(r p) c -> p r c", p=128), rinit[:], "rinit", reads=["rinit"],
                   writes=["rowtab_init"])
            kb.op("pool", lambda e: e.iota(iota32[:], pattern=[[1, 32]], base=0, channel_multiplier=0,
                                           allow_small_or_imprecise_dtypes=True), writes=["iota32"])
            kb.op("pool", lambda e: e.iota(kp[:, 0:8], pattern=[[128, 8]], base=0, channel_multiplier=1,
                                           allow_small_or_imprecise_dtypes=True), writes=["kp"])
            kb.op("pool", lambda e: e.iota(kp[:, 8:12], pattern=[[128, 4]], base=0, channel_multiplier=1,
                                           allow_small_or_imprecise_dtypes=True), reads=["kp"], writes=["kp"])
            B_ = lambda i: bt[:, i, :]

            def BV(fn, extra=()):
                kb.op("dve", fn, reads=["bt", "cnt_bc"] + list(extra), writes=["bt"])
            BV(lambda e: e.tensor_tensor(out=B_(0), in0=cnt_bc[:, 0:32], in1=cnt_bc[:, 32:64], op=ALU.add))
            BV(lambda e: e.tensor_scalar(out=B_(1), in0=B_(0), scalar1=BLK / 2 - 0.5, scalar2=1.0 / BLK, op0=ALU.add,
                                         op1=ALU.mult))
            BV(lambda e: e.tensor_scalar_add(out=B_(5), in0=B_(1), scalar1=MAGIC_))
            BV(lambda e: e.tensor_scalar_add(out=B_(2), in0=B_(5), scalar1=-MAGIC_))
            BV(lambda e: e.tensor_tensor_scan(out=B_(3), data0=ones[:, 0:32], data1=B_(2), initial=0.0, op0=ALU.mult,
                                              op1=ALU.add), extra=["ones"])
            BV(lambda e: e.tensor_tensor(out=B_(4), in0=B_(3), in1=B_(2), op=ALU.subtract))
            kb.op("dve", lambda e: e.tensor_scalar_mul(out=base[:, 0:32], in0=B_(4), scalar1=float(BLK)),
                  reads=["bt"], writes=["base"])
            kb.op("dve", lambda e: e.tensor_tensor(out=base[:, 32:64], in0=base[:, 0:32], in1=cnt_bc[:, 0:32], op=ALU.add),
                  reads=["base", "cnt_bc"], writes=["base"])
            for blk in range(NB):
                BV(lambda e: e.tensor_single_scalar(out=B_(5), in_=B_(3), scalar=float(blk), op=ALU.is_le))
                kb.op("dve", lambda e: e.reduce_sum(out=ebt[:, blk:blk + 1], in_=B_(5), axis=AX.X), reads=["bt", "ebt"],
                      writes=["ebt"])
            kb.op("dve", lambda e: e.tensor_scalar_mul(out=eb2[:, 0, :], in0=ebt[:], scalar1=1024.0), reads=["ebt"],
                  writes=["eb2"])
            kb.op("dve", lambda e: e.tensor_scalar_mul(out=eb2[:, 1, :], in0=ebt[:], scalar1=512.0), reads=["ebt", "eb2"],
                  writes=["eb2"])
            for blk in range(NB):
                kb.op("dve", lambda e: e.tensor_scalar(out=widx_in[:, blk, :], in0=kp[:, 0:8],
                                                       scalar1=eb2[:, 0, blk:blk + 1], scalar2=None, op0=ALU.add),
                      reads=["kp", "eb2", "widx"], writes=["widx"])
                kb.op("dve", lambda e: e.tensor_scalar(out=widx_out[:, blk, :], in0=kp[:, 8:12],
                                                       scalar1=eb2[:, 1, blk:blk + 1], scalar2=None, op0=ALU.add),
                      reads=["kp", "eb2", "widx"], writes=["widx"])
            for ti in range(TT_ // 128):
                sl = ti % 2
                RI = rti[sl]
                rik, ohk, dk, pk_ = ("rti", sl), ("ohs", sl), ("dst", sl), ("pay", sl)
                kb.dma("sp", RI[:], rt_s[ti * 128:(ti + 1) * 128, :], rik, writes=[rik])
                for sidx in range(2):
                    hsl = slice(sidx * 32, (sidx + 1) * 32)
                    kb.op("dve", lambda e: e.tensor_scalar(out=ohs[sl][:, hsl], in0=iota32[:],
                                                           scalar1=RI[:, sidx:sidx + 1], scalar2=None, op0=ALU.is_equal),
                          reads=["iota32", rik, ohk], writes=[ohk])
                kb.op("dve", lambda e: e.tensor_tensor(out=ohs[sl][:], in0=ohs[sl][:], in1=base[:], op=ALU.mult),
                      reads=[ohk, "base"], writes=[ohk])
                kb.op("dve", lambda e: e.reduce_sum(out=dstf[sl][:], in_=ohs[sl][:].rearrange("p (s e) -> p s e", s=2),
                                                    axis=AX.X), reads=[ohk, dk], writes=[dk])
                kb.op("dve", lambda e: e.tensor_tensor(out=dstf[sl][:], in0=dstf[sl][:], in1=RI[:, 2:4], op=ALU.add),
                      reads=[dk, rik], writes=[dk])
                kb.op("dve", lambda e: e.tensor_copy(out=dsti[sl][:], in_=dstf[sl][:]), reads=[dk, ("dsti", sl)],
                      writes=[("dsti", sl)])
                kb.op("dve", lambda e: e.memset(pay[sl][:], 0.0), reads=[pk_], writes=[pk_])
                for sidx in range(2):
                    kb.op("dve", lambda e: e.tensor_copy(out=pay[sl][:, sidx, 0:1], in_=RI[:, 6:7]), reads=[rik, pk_],
                          writes=[pk_])
                    kb.op("dve", lambda e: e.tensor_copy(out=pay[sl][:, sidx, 1:2], in_=RI[:, 4 + sidx:5 + sidx]),
                          reads=[rik, pk_], writes=[pk_])
                    kb.op("dve", lambda e: e.tensor_scalar_add(out=pay[sl][:, sidx, 2:3], in0=RI[:, 6:7],
                                                               scalar1=float(sidx * TT_)),
                          reads=[rik, pk_], writes=[pk_])
                for sidx in range(2):
                    kb.idma(rowtab_s[:, :], bass.IndirectOffsetOnAxis(ap=dsti[sl][:, sidx:sidx + 1], axis=0),
                            pay[sl][:, sidx, :], None, pk_, reads=[pk_, ("dsti", sl), "rowtab_init"])
            kb.barrier()

        with contextlib.ExitStack() as p5:
            I32 = mybir.dt.int32
            w_ein_f = w_ein.rearrange("e k n -> (e k) n")
            w_eout_f = w_eout.rearrange("e k n -> (e k) n")
            wei = [SB("wei%d" % i, [128, 8, 1024], BF16, p5) for i in range(3)]
            weo = [SB("weo%d" % i, [128, 4, 1024], BF16, p5) for i in range(3)]
            rtab = SB("rtab", [128, 3, 4, 4], F32, p5)
            rti32 = SB("rti32", [128, 3, 4, 2], I32, p5)
            xg = [SB("xg%d" % i, [128, D], F32, p5) for i in range(8)]
            xT = [SB("xT%d" % i, [128, 8, 512], BF16, p5) for i in range(2)]
            sa = [SB("sa%d" % i, [128, 512], F32, p5) for i in range(2)]
            hid = [SB("hid%d" % i, [128, 4, 512], BF16, p5) for i in range(2)]
            yb_ = [SB("yb%d" % i, [128, D], F32, p5) for i in range(8)]
            ocnt_ = [0]

            reg_in = nc.gpsimd.to_reg(32 * 1024 - 1)
            reg_out = nc.gpsimd.to_reg(32 * 512 - 1)

            def LOADW(blk):
                ws = blk % 3
                for k in range(8):
                    kb.idma(wei[ws][:, k, :], None, w_ein_f[:, :],
                            bass.IndirectOffsetOnAxis(ap=widx_in[:, blk, k:k + 1], axis=0), ("wei", ws, k),
                            reads=["widx"], writes=[("wei", ws, k)], bounds_check=reg_in, oob_is_err=False)
                for k in range(4):
                    kb.idma(weo[ws][:, k, :], None, w_eout_f[:, :],
                            bass.IndirectOffsetOnAxis(ap=widx_out[:, blk, k:k + 1], axis=0), ("weo", ws, k),
                            reads=["widx"], writes=[("weo", ws, k)], bounds_check=reg_out, oob_is_err=False)

            def LOADX(blk):
                rs = blk % 3
                for sub in range(4):
                    r0 = blk * BLK + sub * 128
                    rk_ = ("rtab", rs, sub)
                    gi_ = (blk % 2) * 4 + sub
                    kb.dma("sp", rtab[:, rs, sub, :], rowtab_s[r0:r0 + 128, :], rk_, writes=[rk_])
                    kb.op("dve", lambda e: e.tensor_copy(out=rti32[:, rs, sub, 0:1], in_=rtab[:, rs, sub, 0:1]),
                          reads=[rk_, ("rti32", rs, sub)], writes=[("rti32", rs, sub)])
                    kb.op("dve", lambda e: e.tensor_copy(out=rti32[:, rs, sub, 1:2], in_=rtab[:, rs, sub, 2:3]),
                          reads=[rk_, ("rti32", rs, sub)], writes=[("rti32", rs, sub)])
                    kb.idma(xg[gi_][:, :], None, h2tok_s[:, :],
                            bass.IndirectOffsetOnAxis(ap=rti32[:, rs, sub, 0:1], axis=0), ("xg", gi_),
                            reads=[("rti32", rs, sub)], writes=[("xg", gi_)])

            def COMP(blk):
                ws = blk % 3
                rs = blk % 3
                xs = blk % 2
                for sub in range(4):
                    gi_ = (blk % 2) * 4 + sub
                    for k in range(8):
                        pi = 6 + k // 4
                        kb.op("pe", lambda e: e.transpose(ps[pi][:, (k % 4) * 128:(k % 4 + 1) * 128],
                                                          xg[gi_][:, k * 128:(k + 1) * 128], ident[:]),
                              reads=[("xg", gi_)], writes=[("ps", pi)])
                    kb.op("act", lambda e: e.copy(out=xT[xs][:, 0:4, sub * 128:(sub + 1) * 128],
                                                  in_=ps[6][:].rearrange("p (k t) -> p k t", k=4)),
                          reads=[("ps", 6)], writes=[("xT", xs, sub, 0)])
                    kb.op("dve", lambda e: e.tensor_copy(out=xT[xs][:, 4:8, sub * 128:(sub + 1) * 128],
                                                         in_=ps[7][:].rearrange("p (k t) -> p k t", k=4)),
                          reads=[("ps", 7)], writes=[("xT", xs, sub, 1)])
                xkeys = [("xT", xs, sub, hh) for sub in range(4) for hh in range(2)]
                for j in range(4):
                    pa_i, pg_i = (j % 2) * 2, (j % 2) * 2 + 1
                    for k in range(8):
                        kb.op("pe", lambda e: e.matmul(ps[pa_i][:], lhsT=wei[ws][:, k, j * 128:(j + 1) * 128],
                                                       rhs=xT[xs][:, k, :], start=(k == 0), stop=(k == 7)),
                              reads=[("wei", ws, k)] + xkeys, writes=[("ps", pa_i)])
                    for k in range(8):
                        kb.op("pe", lambda e: e.matmul(ps[pg_i][:],
                                                       lhsT=wei[ws][:, k, 512 + j * 128:512 + (j + 1) * 128],
                                                       rhs=xT[xs][:, k, :], start=(k == 0), stop=(k == 7)),
                              reads=[("wei", ws, k)] + xkeys, writes=[("ps", pg_i)])
                    kb.op("act", lambda e: e.activation(out=sa[j % 2][:], in_=ps[pa_i][:], func=AF.Silu),
                          reads=[("ps", pa_i)], writes=[("sa", j % 2)])
                    kb.op("dve", lambda e: e.tensor_tensor(out=hid[xs][:, j, :], in0=ps[pg_i][:],
                                                           in1=sa[j % 2][:], op=ALU.mult),
                          reads=[("ps", pg_i), ("sa", j % 2)], writes=[("hid", xs, j)])
                for sub in range(4):
                    yi = (blk % 2) * 4 + sub
                    for half in range(2):
                        po = 4 + ocnt_[0] % 2
                        ocnt_[0] += 1
                        for j in range(4):
                            kb.op("pe", lambda e: e.matmul(ps[po][:], lhsT=hid[xs][:, j, sub * 128:(sub + 1) * 128],
                                                           rhs=weo[ws][:, j, half * 512:(half + 1) * 512],
                                                           start=(j == 0), stop=(j == 3)),
                                  reads=[("hid", xs, j), ("weo", ws, j)], writes=[("ps", po)])
                        kb.op("dve" if half == 0 else "act",
                              (lambda e: e.tensor_scalar_mul(out=yb_[yi][:, 0:512], in0=ps[po][:],
                                                             scalar1=rtab[:, rs, sub, 1:2])) if half == 0 else
                              (lambda e: e.activation(out=yb_[yi][:, 512:1024], in_=ps[po][:], func=AF.Copy,
                                                      scale=rtab[:, rs, sub, 1:2])),
                              reads=[("ps", po), ("rtab", rs, sub)], writes=[("yb", yi, half)])

            def SCAT(blk):
                rs = blk % 3
                for sub in range(4):
                    yi = (blk % 2) * 4 + sub
                    kb.idma(ys_s[:, :], bass.IndirectOffsetOnAxis(ap=rti32[:, rs, sub, 1:2], axis=0),
                            yb_[yi][:, :], None, ("yb", yi),
                            reads=[("yb", yi, 0), ("yb", yi, 1), ("rti32", rs, sub)])

            LOADW(0)
            LOADX(0)
            if NB > 1:
                LOADW(1)
            for blk in range(NB):
                if blk + 1 < NB:
                    LOADX(blk + 1)
                if blk + 2 < NB:
                    LOADW(blk + 2)
                COMP(blk)
                if blk >= 1:
                    SCAT(blk - 1)
            SCAT(NB - 1)
            kb.barrier()

        with contextlib.ExitStack() as p6:
            gate2 = SB("gate2", [128, NS, D], F32, p6)
            gfbc = SB("gfbc", [128, D], F32, p6)
            x5 = [SB("x5_%d" % i, [128, D], F32, p6) for i in range(3)]
            y0 = [SB("y0_%d" % i, [128, D], F32, p6) for i in range(3)]
            y1 = [SB("y1_%d" % i, [128, D], F32, p6) for i in range(3)]
            xo = [SB("xo%d" % i, [128, D], F32, p6) for i in range(3)]
            junk5 = SB("junk5", [128, D], F32, p6)
            st5 = [SB("st5_%d" % i, [128, 8], F32, p6) for i in range(3)]
            kb.dma("sp", gfbc[:], final_g.partition_broadcast(128), "gfbc", writes=["gfbc"])
            for b in range(NS):
                kb.dma("sp", gate2[:, b, :], gate_s[:, (b * 4 + 1) * D:(b * 4 + 2) * D], "gate2", writes=["gate2"])
            tiles6 = [(b, ti) for b in range(NS) for ti in range(NT)]

            def p6_load(idx):
                b, ti = tiles6[idx]
                sl = idx % 3
                tok0 = ti * 128
                tg = b * S + tok0
                kb.dma("sp", x5[sl][:], xmid_s[b, tok0:tok0 + 128, :], ("x5", sl), writes=[("x5", sl)])
                kb.dma("sp", y0[sl][:], ys_s[tg:tg + 128, :], ("y0", sl), writes=[("y0", sl)])
                kb.dma("sp", y1[sl][:], ys_s[TT_ + tg:TT_ + tg + 128, :], ("y1", sl), writes=[("y1", sl)])

            p6_load(0)
            if len(tiles6) > 1:
                p6_load(1)
            for idx in range(len(tiles6)):
                if True:
                    b, ti = tiles6[idx]
                    sl = idx % 3
                    tok0 = ti * 128
                    st = st5[sl]
                    if idx + 2 < len(tiles6):
                        p6_load(idx + 2)
                    kb.op("dve", lambda e: e.tensor_tensor(out=y0[sl][:], in0=y0[sl][:], in1=y1[sl][:], op=ALU.add),
                          reads=[("y0", sl), ("y1", sl)], writes=[("y0", sl)])
                    kb.op("dve", lambda e: e.tensor_tensor(out=xo[sl][:], in0=y0[sl][:], in1=gate2[:, b, :], op=ALU.mult),
                          reads=[("y0", sl), "gate2"], writes=[("xo", sl)])
                    kb.op("dve", lambda e: e.tensor_tensor(out=xo[sl][:], in0=xo[sl][:], in1=x5[sl][:], op=ALU.add),
                          reads=[("xo", sl), ("x5", sl)], writes=[("xo", sl)])
                    kb.op("act", lambda e: e.activation(out=junk5[:], in_=xo[sl][:], func=AF.Square,
                                                        accum_out=st[:, 0:1]),
                          reads=[("xo", sl)], writes=["junk5", ("st5", sl)])
                    kb.op("dve", lambda e: e.tensor_scalar(out=st[:, 1:2], in0=st[:, 0:1], scalar1=1.0 / D,
                                                           scalar2=1e-6, op0=ALU.mult, op1=ALU.add),
                          reads=[("st5", sl)], writes=[("st5", sl)])
                    kb.op("act", lambda e: e.activation(out=st[:, 2:3], in_=st[:, 1:2], func=AF.Ln),
                          reads=[("st5", sl)], writes=[("st5", sl)])
                    kb.op("act", lambda e: e.activation(out=st[:, 3:4], in_=st[:, 2:3], func=AF.Exp, scale=-0.5),
                          reads=[("st5", sl)], writes=[("st5", sl)])
                    kb.op("dve", lambda e: e.scalar_tensor_tensor(out=xo[sl][:], in0=xo[sl][:], scalar=st[:, 3:4],
                                                                  in1=gfbc[:], op0=ALU.mult, op1=ALU.mult),
                          reads=[("xo", sl), ("st5", sl), "gfbc"], writes=[("xo", sl)])
                    kb.dma("sp", y[b, tok0:tok0 + 128, :], xo[sl][:], ("xo", sl), reads=[("xo", sl)])
            kb.barrier()

        kb.barrier()
    return nc


def _prep_inputs(inputs):
    f = lambda a: np.ascontiguousarray(np.asarray(a, dtype=np.float32))
    d = {}
    d["w_ada"] = f(inputs["w_ada"][0])
    d["b_ada"] = f(inputs["b_ada"][0]).reshape(1, -1)
    d["norm1_g"] = f(inputs["norm1_g"][0]).reshape(1, -1)
    d["w_in"] = f(inputs["w_in"][0])
    d["rel_bias"] = f(inputs["rel_bias"])
    for n in ("lambda_q1", "lambda_k1", "lambda_q2", "lambda_k2", "subln_g"):
        d[n] = f(inputs[n][0]).reshape(1, -1)
    d["ssm_lambda_re"] = f(inputs["ssm_lambda_re"][0])
    d["ssm_lambda_im"] = f(inputs["ssm_lambda_im"][0])
    d["ssm_log_step"] = f(inputs["ssm_log_step"][0]).reshape(32, 1)
    d["ssm_b_re"] = f(inputs["ssm_b_re"][0])
    d["ssm_b_im"] = f(inputs["ssm_b_im"][0])
    d["ssm_c_re"] = f(inputs["ssm_c_re"][0])
    d["ssm_c_im"] = f(inputs["ssm_c_im"][0])
    d["ssm_d"] = f(inputs["ssm_d"][0]).reshape(512, 1)
    d["w_glu"] = f(inputs["w_glu"][0])
    d["w_proj_attn"] = f(inputs["w_proj_attn"][0])
    d["w_proj_ssm"] = f(inputs["w_proj_ssm"][0])
    d["w_out"] = f(inputs["w_out"][0])
    d["norm2_g"] = f(inputs["norm2_g"][0]).reshape(1, -1)
    d["w_router_group"] = f(inputs["w_router_group"][0])
    d["b_router_group"] = f(inputs["b_router_group"][0]).reshape(1, 4)
    d["w_router_expert"] = f(inputs["w_router_expert"][0])
    d["b_router_expert"] = f(inputs["b_router_expert"][0]).reshape(1, 32)
    d["w_expert_in"] = f(inputs["w_expert_in"][0])
    d["w_expert_out"] = f(inputs["w_expert_out"][0])
    d["final_g"] = f(inputs["final_g"]).reshape(1, -1)
    d["relmask"] = _relmask()
    return d


def _relmask():
    kk = np.arange(128)[:, None]
    qq = np.arange(128)[None, :]
    out = np.zeros((33, 2, 128, 128), np.float32)
    for blk in range(2):
        dist = qq - kk + 128 * blk
        n = np.maximum(dist, 0)
        lr = np.log(np.maximum(n, 1).astype(np.float32) / 16) / math.log(128 / 16)
        large = np.minimum(16 + (lr * 16).astype(np.int32), 31)
        bucket = np.where(n < 16, n, large)
        for bkt in range(32):
            out[bkt, blk] = ((bucket == bkt) & (dist >= 0))
        out[32, blk] = (dist < 0)
    return out.reshape(33, -1)


def kernel(**inputs):
    S = inputs["x"].shape[1]
    B = inputs["x"].shape[0]
    NS = B // NCORES
    nc = build(S, NS)
    shared = _prep_inputs(inputs)
    xs = np.ascontiguousarray(np.asarray(inputs["x"], dtype=np.float32))
    cs = np.ascontiguousarray(np.asarray(inputs["c"], dtype=np.float32))
    in_maps = []
    for i in range(NCORES):
        m = dict(shared)
        m["x"] = xs[i * NS:(i + 1) * NS]
        m["c"] = cs[i * NS:(i + 1) * NS]
        in_maps.append(m)
    res = run_bass_kernel_spmd(nc, in_maps, core_ids=list(range(NCORES)))
    return np.concatenate([r["y"] for r in res.results], axis=0)
```

```python
import math
import functools
import contextlib
import numpy as np
import concourse.bass as bass
import concourse.mybir as mybir
from concourse.bass_utils import run_bass_kernel_spmd

F32 = mybir.dt.float32
BF16 = mybir.dt.bfloat16
AF = mybir.ActivationFunctionType
ALU = mybir.AluOpType
AX = mybir.AxisListType

D = 1024
NCORES = 8
NEG = -1e30
LAMBDA_INIT = 0.8 - 0.6 * math.exp(-0.3 * 0)


class KB:
    def __init__(self, nc, es):
        self.nc = nc
        self.es = es
        self.eng = dict(pe=nc.tensor, act=nc.scalar, dve=nc.vector, pool=nc.gpsimd, sp=nc.sync)
        self.sem = {e: es.enter_context(nc.semaphore("s_" + e)) for e in ("pe", "act", "dve", "pool")}
        self.tick = {e: 0 for e in self.sem}
        self.seen = {e: {} for e in self.eng}
        self.res = {}
        self.dsem = {}
        self.dfree = []
        self.dfree_sw = []
        self.nd = 0
        self.nins = 0

    def _wait(self, e, ev, raw, isdma=False):
        sem, val, src = ev
        if (not isdma) and src == e and (e == "pe" or not raw):
            return
        if self.seen[e].get(sem, 0) >= val:
            return
        self.eng[e].wait_ge(sem, val)
        self.seen[e][sem] = val

    def _deps(self, e, reads, writes, isdma=False):
        for k in reads:
            r = self.res.get(k)
            if r and r[0]:
                self._wait(e, r[0], True, isdma)
        for k in writes:
            r = self.res.get(k)
            if r:
                if r[0]:
                    self._wait(e, r[0], False, isdma)
                for ev in r[1].values():
                    self._wait(e, ev, False, isdma)

    def _reg(self, ev, reads, writes):
        for k in reads:
            r = self.res.setdefault(k, [None, {}])
            r[1][ev[0]] = ev
        for k in writes:
            self.res[k] = [ev, {}]

    def op(self, e, fn, reads=(), writes=()):
        self._deps(e, reads, writes)
        ins = fn(self.eng[e])
        self.tick[e] += 1
        ins.then_inc(self.sem[e], 1)
        self._reg((self.sem[e], self.tick[e], e), reads, writes)
        self.nins += 1

    def dma(self, q, out, in_, key, reads=(), writes=(), **kw):
        self._deps(q, reads, writes, isdma=True)
        ins = self.eng[q].dma_start(out=out, in_=in_, **kw)
        self._finish_dma(ins, key, q == "pool", reads, writes)

    def _finish_dma(self, ins, key, sw, reads, writes):
        k2 = (key, sw)
        d = self.dsem.get(k2)
        if d is None:
            fl = self.dfree_sw if sw else self.dfree
            if fl:
                d = fl.pop()
            else:
                self.nd += 1
                d = [self.es.enter_context(self.nc.semaphore("d%d" % self.nd)), 0, sw]
            self.dsem[k2] = d
        d[1] += 16
        ins.then_inc(d[0], 16)
        self._reg((d[0], d[1], "dma"), reads, writes)
        self.nins += 1

    def idma(self, out, out_off, in_, in_off, key, reads=(), writes=(), **kw):
        self._deps("pool", reads, writes, isdma=True)
        ins = self.eng["pool"].indirect_dma_start(out=out, out_offset=out_off, in_=in_, in_offset=in_off, **kw)
        self._finish_dma(ins, key, True, reads, writes)

    def barrier(self):
        evs = [(self.sem[e], self.tick[e], e) for e in self.sem if self.tick[e] > 0]
        evs += [(d[0], d[1], "dma") for d in self.dsem.values() if d[1] > 0]
        for e in self.eng:
            for ev in evs:
                self._wait(e, ev, True, isdma=True)
        self.res = {}
        for d in self.dsem.values():
            (self.dfree_sw if d[2] else self.dfree).append(d)
        self.dsem = {}


class Rec:
    def __init__(self):
        self.L = []

    def op(self, e, fn, reads=(), writes=()):
        self.L.append((e, fn, list(reads), list(writes)))


def emit_interleaved(kb, a, b):
    ia = ib = 0
    while ia < len(a) or ib < len(b):
        if ia < len(a):
            kb.op(*a[ia])
            ia += 1
        if ib < len(b):
            kb.op(*b[ib])
            ib += 1


def build(S, NS, debug=None):
    nc = bass.Bass("TRN2", target_bir_lowering=False)
    NT = S // 128
    NG = S // 512
    dbg = debug or ()

    def din(name, shape):
        return nc.dram_tensor(name, shape, F32, kind="ExternalInput").ap()

    x = din("x", [NS, S, D])
    c = din("c", [NS, D])
    w_ada = din("w_ada", [D, 6 * D])
    b_ada = din("b_ada", [1, 6 * D])
    norm1_g = din("norm1_g", [1, D])
    w_in = din("w_in", [D, 4096])
    rel_bias = din("rel_bias", [32, 4])
    lam_q1 = din("lambda_q1", [1, 64])
    lam_k1 = din("lambda_k1", [1, 64])
    lam_q2 = din("lambda_q2", [1, 64])
    lam_k2 = din("lambda_k2", [1, 64])
    subln_g = din("subln_g", [1, 128])
    ssm_lre = din("ssm_lambda_re", [32, 64])
    ssm_lim = din("ssm_lambda_im", [32, 64])
    ssm_ls = din("ssm_log_step", [32, 1])
    ssm_bre = din("ssm_b_re", [32, 64, 16])
    ssm_bim = din("ssm_b_im", [32, 64, 16])
    ssm_cre = din("ssm_c_re", [32, 16, 64])
    ssm_cim = din("ssm_c_im", [32, 16, 64])
    ssm_d = din("ssm_d", [512, 1])
    w_glu = din("w_glu", [512, 1024])
    w_pa = din("w_proj_attn", [512, 1024])
    w_ps = din("w_proj_ssm", [512, 1024])
    w_out = din("w_out", [1024, 1024])
    norm2_g = din("norm2_g", [1, D])
    w_rg = din("w_router_group", [1024, 4])
    b_rg = din("b_router_group", [1, 4])
    w_re = din("w_router_expert", [4, 1024, 8])
    b_re = din("b_router_expert", [1, 32])
    w_ein = din("w_expert_in", [32, 1024, 1024])
    w_eout = din("w_expert_out", [32, 512, 1024])
    final_g = din("final_g", [1, D])
    relmask = din("relmask", [33, 2 * 128 * 128])
    y = nc.dram_tensor("y", [NS, S, D], F32, kind="ExternalOutput").ap()

    def scratch(name, shape, dt):
        kind = "ExternalOutput" if name in dbg else "Internal"
        return nc.dram_tensor(name, shape, dt, kind=kind).ap()

    qT_s = scratch("qT_s", [NS, 4, 128, S], BF16)
    kT_s = scratch("kT_s", [NS, 4, 128, S], BF16)
    v_s = scratch("v_s", [NS, S, 512], BF16)
    uT_s = scratch("uT_s", [NS, 4, 128, S], F32)
    sg_s = scratch("sg_s", [NS, 16, 128, S], BF16)
    mod_s = scratch("mod_s", [128, 48 * NS], F32)
    tab_s = scratch("tab_s", [4, 2 * 128 * 128], F32)
    gate_s = scratch("gate_s", [128, NS * 4 * D], F32)
    xmid_s = scratch("xmid_s", [NS, S, D], F32)
    h2T_s = scratch("h2T_s", [NS, 8, 128, S], BF16)
    comb_s = scratch("comb_s", [NS, S, 32], F32)
    TT_ = NS * S
    BLK = 512
    NB = 2 * TT_ // BLK + 32
    rt_s = scratch("rt_s", [TT_, 8], F32)
    h2tok_s = scratch("h2tok_s", [TT_, D], F32)
    rowtab_s = scratch("rowtab_s", [NB * BLK, 4], F32)
    ys_s = scratch("ys_s", [2 * TT_ + 128, D], F32)
    yaT_s = scratch("yaT_s", [NS, 4, 128, S], BF16)
    ysT_s = scratch("ysT_s", [NS, 4, 128, S], BF16)

    with contextlib.ExitStack() as es:
        kb = KB(nc, es)

        def SB(name, shape, dt, stack=es):
            return stack.enter_context(nc.sbuf_tensor(name, shape, dt))

        ps = [es.enter_context(nc.psum_tensor("ps%d" % i, [128, 512], F32)) for i in range(8)]

        ident = SB("ident", [128, 128], F32)
        ones = SB("ones", [128, 128], F32)
        modT = SB("modT", [128, 48, NS], F32)
        Tri = SB("Tri", [128, 128], F32)
        cnt_bc = SB("cnt_bc", [128, 64], F32)
        widx_in = SB("widx_in", [128, NB, 8], mybir.dt.int32)
        widx_out = SB("widx_out", [128, NB, 4], mybir.dt.int32)

        with contextlib.ExitStack() as p0:
            io = SB("io", [128, 128], F32, p0)
            cT = SB("cT", [128, 8, NS], F32, p0)
            sc = SB("sc", [128, 8, NS], F32, p0)
            sc_bc = SB("sc_bc", [128, NS, 8, 128], F32, p0)
            b_adaT = SB("b_adaT", [128, 48], F32, p0)
            bada_bc = SB("bada_bc", [128, 4, D], F32, p0)
            wst = [SB("wst%d" % i, [128, 8, 512], F32, p0) for i in range(3)]
            gate_bc = SB("gate_bc", [128, NS, 4, D], F32, p0)

            kb.op("pool", lambda e: e.iota(io[:], pattern=[[1, 128]], base=0, channel_multiplier=-1,
                                           allow_small_or_imprecise_dtypes=True), writes=["io"])
            kb.op("dve", lambda e: e.tensor_single_scalar(out=ident[:], in_=io[:], scalar=0.0, op=ALU.is_equal),
                  reads=["io"], writes=["ident"])
            kb.op("dve", lambda e: e.memset(ones[:], 1.0), writes=["ones"])
            kb.op("dve", lambda e: e.tensor_single_scalar(out=Tri[:], in_=io[:], scalar=0.0, op=ALU.is_ge),
                  reads=["io"], writes=["Tri"])
            for b in range(NS):
                kb.dma("sp", cT[:, :, b], c[b].rearrange("(k p) -> p k", p=128), "cT", writes=["cT"],
                       allow_slow_non_contiguous=True)
            kb.dma("sp", b_adaT[:], b_ada.rearrange("o (j p) -> p (o j)", p=128), "b_adaT", writes=["b_adaT"],
                   allow_slow_non_contiguous=True)
            for gi_, c0_ in enumerate((2048, 5120, 3072, 4096)):
                kb.dma("sp", bada_bc[:, gi_, :], b_ada[:, c0_:c0_ + 1024].partition_broadcast(128), "bada_bc",
                       writes=["bada_bc"])
            kb.op("act", lambda e: e.activation(out=sc[:], in_=cT[:], func=AF.Silu), reads=["cT"], writes=["sc"])
            for b in range(NS):
                for k in range(8):
                    kb.op("dve", lambda e, b=b, k=k: e.tensor_scalar_mul(out=sc_bc[:, b, k, :], in0=ones[:],
                                                                       scalar1=sc[:, k, b:b + 1]),
                          reads=["sc", "ones"], writes=["sc_bc"])
            def p0_load(jb):
                kb.dma("sp" if jb % 2 == 0 else "pool", wst[jb % 3][:],
                       w_ada[:, jb * 512:(jb + 1) * 512].rearrange("(k p) n -> p k n", p=128), ("wst", jb % 3),
                       writes=[("wst", jb % 3)])

            p0_load(0)
            p0_load(1)
            for jb in range(12):
                wt = wst[jb % 3]
                wk = ("wst", jb % 3)
                if jb + 2 < 12:
                    p0_load(jb + 2)
                pb = ps[jb % 2]
                pk = ("ps", jb % 2)
                for m in range(4):
                    for k in range(8):
                        kb.op("pe", lambda e, m=m, k=k: e.matmul(pb[:, m * NS:(m + 1) * NS],
                                                                 lhsT=wt[:, k, m * 128:(m + 1) * 128],
                                                                 rhs=sc[:, k, :], start=(k == 0), stop=(k == 7)),
                              reads=[wk, "sc"], writes=[pk])
                for b in range(NS):
                    kb.op("dve", lambda e, b=b: e.tensor_tensor(
                        out=modT[:, jb * 4:(jb + 1) * 4, b],
                        in0=pb[:, 0:4 * NS].rearrange("p (m b) -> p m b", b=NS)[:, :, b],
                        in1=b_adaT[:, jb * 4:(jb + 1) * 4], op=ALU.add),
                          reads=[pk, "b_adaT"], writes=["modT"])
                if jb in (4, 5, 10, 11, 6, 7, 8, 9):
                    gi = {4: 0, 5: 0, 10: 1, 11: 1, 6: 2, 7: 2, 8: 3, 9: 3}[jb]
                    half = jb % 2
                    for b in range(NS):
                        pg = ps[2 + b % 2]
                        pgk = ("ps", 2 + b % 2)
                        for k in range(8):
                            kb.op("pe", lambda e, k=k, b=b: e.matmul(pg[:], lhsT=sc_bc[:, b, k, :], rhs=wt[:, k, :],
                                                                     start=(k == 0), stop=(k == 7)),
                                  reads=[wk, "sc_bc"], writes=[pgk])
                        kb.op("dve", lambda e, b=b: e.tensor_tensor(
                            out=gate_bc[:, b, gi, half * 512:(half + 1) * 512], in0=pg[:],
                            in1=bada_bc[:, gi, half * 512:(half + 1) * 512], op=ALU.add),
                              reads=[pgk, "bada_bc"], writes=["gate_bc"])
            for lo in (8, 32):
                kb.op("dve", lambda e, lo=lo: e.tensor_scalar_add(out=modT[:, lo:lo + 8, :], in0=modT[:, lo:lo + 8, :],
                                                                  scalar1=1.0),
                      reads=["modT"], writes=["modT"])
            for b in range(NS):
                kb.op("dve", lambda e: e.tensor_scalar_add(out=gate_bc[:, b, 3, :], in0=gate_bc[:, b, 3, :], scalar1=1.0),
                      reads=["gate_bc"], writes=["gate_bc"])
            kb.dma("sp", gate_s, gate_bc[:].rearrange("p b g d -> p (b g d)"), "gate_st", reads=["gate_bc"])
            if "mod_s" in dbg:
                kb.dma("sp", mod_s, modT[:].rearrange("p j b -> p (j b)"), "modT_st", reads=["modT"])
            kb.barrier()

        with contextlib.ExitStack() as p1:
            winb = SB("winb", [128, 8, 4096], BF16, p1)
            g1bc = SB("g1bc", [128, D], F32, p1)
            kb.dma("sp", g1bc[:], norm1_g.partition_broadcast(128), "g1bc", writes=["g1bc"])
            xt = [SB("xt%d" % i, [128, D], F32, p1) for i in range(2)]
            junk = SB("junk", [128, D], F32, p1)
            xn = [SB("xn%d" % i, [128, D], F32, p1) for i in range(2)]
            ss = SB("ss", [128, 8], F32, p1)
            hT = [SB("hT%d" % i, [128, 8, 512], BF16, p1) for i in range(2)]
            stq = [SB("stq%d" % i, [128, 24, 512], BF16, p1) for i in range(2)]
            stu = [SB("stu%d" % i, [128, 4, 512], F32, p1) for i in range(2)]
            stv = [SB("stv%d" % i, [128, 4, 512], BF16, p1) for i in range(2)]
            for k in range(8):
                kb.dma("pool", winb[:, k, :], w_in[k * 128:(k + 1) * 128, :], ("winb", k), writes=[("winb", k)])
            tcnt = 0
            pcnt = 0
            ecnt = 0
            for b in range(NS):
                for g in range(NG):
                    gs = (b * NG + g) % 2
                    hk = ("hT", gs)
                    for t in range(4):
                        tok0 = g * 512 + t * 128
                        sl = tcnt % 2
                        tcnt += 1
                        kb.dma("sp", xt[sl][:], x[b, tok0:tok0 + 128, :], ("xt", sl), writes=[("xt", sl)])
                        kb.op("act", lambda e: e.activation(out=junk[:], in_=xt[sl][:], func=AF.Square,
                                                            accum_out=ss[:, sl:sl + 1]),
                              reads=[("xt", sl)], writes=["junk", ("ss", sl)])
                        kb.op("dve", lambda e: e.tensor_scalar(out=ss[:, 2 + sl:3 + sl], in0=ss[:, sl:sl + 1],
                                                               scalar1=1.0 / D, scalar2=1e-6, op0=ALU.mult,
                                                               op1=ALU.add),
                              reads=[("ss", sl)], writes=[("ss2", sl)])
                        kb.op("act", lambda e: e.activation(out=ss[:, 6 + sl:7 + sl], in_=ss[:, 2 + sl:3 + sl],
                                                            func=AF.Sqrt),
                              reads=[("ss2", sl)], writes=[("ss3", sl)])
                        kb.op("dve", lambda e: e.reciprocal(out=ss[:, 4 + sl:5 + sl], in_=ss[:, 6 + sl:7 + sl]),
                              reads=[("ss3", sl)], writes=[("rstd", sl)])
                        kb.op("dve", lambda e: e.scalar_tensor_tensor(out=xn[sl][:], in0=xt[sl][:],
                                                                      scalar=ss[:, 4 + sl:5 + sl], in1=g1bc[:],
                                                                      op0=ALU.mult, op1=ALU.mult),
                              reads=[("xt", sl), ("rstd", sl), "g1bc"], writes=[("xn", sl)])
                        for k in range(8):
                            pb = ps[sl * 2 + k // 4]
                            pk = ("ps", sl * 2 + k // 4)
                            kb.op("pe", lambda e, k=k, pb=pb: e.transpose(pb[:, (k % 4) * 128:(k % 4 + 1) * 128],
                                                                          xn[sl][:, k * 128:(k + 1) * 128], ident[:]),
                                  reads=[("xn", sl)], writes=[pk])
                        for k in range(8):
                            pb = ps[sl * 2 + k // 4]
                            pk = ("ps", sl * 2 + k // 4)
                            kb.op("act", lambda e, k=k, pb=pb: e.activation(
                                out=hT[gs][:, k, t * 128:(t + 1) * 128],
                                in_=pb[:, (k % 4) * 128:(k % 4 + 1) * 128], func=AF.Identity,
                                scale=modT[:, 8 + k, b:b + 1], bias=modT[:, k, b:b + 1]),
                                  reads=[pk], writes=[hk])
                    for t in range(4):
                        pi = 4 + pcnt % 4
                        pcnt += 1
                        for k in range(8):
                            kb.op("pe", lambda e, k=k: e.matmul(ps[pi][:], lhsT=hT[gs][:, k, t * 128:(t + 1) * 128],
                                                                rhs=winb[:, k, 1024:1536], start=(k == 0),
                                                                stop=(k == 7)),
                                  reads=[hk, ("winb", k)], writes=[("ps", pi)])
                        kb.op("dve", lambda e: e.tensor_copy(out=stv[gs][:, t, :], in_=ps[pi][:]),
                              reads=[("ps", pi)], writes=[("stv", gs, t)])
                    for m in list(range(0, 8)) + list(range(12, 32)):
                        pi = 4 + pcnt % 4
                        pcnt += 1
                        for k in range(8):
                            kb.op("pe", lambda e, k=k: e.matmul(ps[pi][:], lhsT=winb[:, k, m * 128:(m + 1) * 128],
                                                                rhs=hT[gs][:, k, :], start=(k == 0), stop=(k == 7)),
                                  reads=[hk, ("winb", k)], writes=[("ps", pi)])
                        if m < 8:
                            eng = "dve" if (ecnt % 2 == 0) else "act"
                            ecnt += 1
                            if eng == "dve":
                                kb.op("dve", lambda e: e.tensor_copy(out=stq[gs][:, m, :], in_=ps[pi][:]),
                                      reads=[("ps", pi)], writes=[("stq", gs, m)])
                            else:
                                kb.op("act", lambda e: e.copy(out=stq[gs][:, m, :], in_=ps[pi][:]),
                                      reads=[("ps", pi)], writes=[("stq", gs, m)])
                        elif m < 16:
                            kb.op("dve", lambda e: e.tensor_copy(out=stu[gs][:, m - 12, :], in_=ps[pi][:]),
                                  reads=[("ps", pi)], writes=[("stu", gs, m - 12)])
                        else:
                            kb.op("act", lambda e: e.activation(out=stq[gs][:, m - 8, :], in_=ps[pi][:],
                                                                func=AF.Sigmoid),
                                  reads=[("ps", pi)], writes=[("stq", gs, m - 8)])
                    c0 = g * 512
                    kb.dma("pool", qT_s[b, :, :, c0:c0 + 512].rearrange("m p t -> p m t"), stq[gs][:, 0:4, :],
                           ("stq", gs), reads=[("stq", gs, m) for m in range(0, 4)])
                    kb.dma("pool", kT_s[b, :, :, c0:c0 + 512].rearrange("m p t -> p m t"), stq[gs][:, 4:8, :],
                           ("stq", gs), reads=[("stq", gs, m) for m in range(4, 8)])
                    kb.dma("pool", sg_s[b, :, :, c0:c0 + 512].rearrange("m p t -> p m t"), stq[gs][:, 8:24, :],
                           ("stq", gs), reads=[("stq", gs, m) for m in range(8, 24)])
                    kb.dma("pool", uT_s[b, :, :, c0:c0 + 512].rearrange("m p t -> p m t"), stu[gs][:],
                           ("stu", gs), reads=[("stu", gs, m) for m in range(4)])
                    kb.dma("pool", v_s[b, c0:c0 + 512, :].rearrange("(t p) n -> p t n", p=128), stv[gs][:],
                           ("stv", gs), reads=[("stv", gs, t) for t in range(4)])
            kb.barrier()

        with contextlib.ExitStack() as p2:
            rbx = SB("rbx", [33, 4], F32, p2)
            rb32 = SB("rb32", [32, 4], F32, p2)
            b31_32 = SB("b31_32", [32, 4], F32, p2)
            b31_bc = SB("b31_bc", [128, 4], F32, p2)
            rmk = [SB("rmk%d" % i, [33, 4096], F32, p2) for i in range(2)]
            tabst = [SB("tabst%d" % i, [4, 4096], F32, p2) for i in range(2)]
            T8 = SB("T8", [128, 8, 128], F32, p2)
            E8 = SB("E8", [128, 8, 128], F32, p2)
            lamt = SB("lamt", [128, 4, 64], F32, p2)
            lams = SB("lams", [128, 8], F32, p2)
            gsub = SB("gsub", [128, 128], F32, p2)
            kT = SB("kT", [128, 4, S], BF16, p2)
            qT = SB("qT", [128, 4, S], BF16, p2)
            vsb = SB("vsb", [128, NT, 4, 130], BF16, p2)
            pT = [SB("pT%d" % i, [128, 512], BF16, p2) for i in range(6)]
            osb = [SB("osb%d" % i, [128, 512], F32, p2) for i in range(2)]
            sm = [SB("sm%d" % i, [128, 16], F32, p2) for i in range(2)]
            ojunk = SB("ojunk", [128, 128], F32, p2)
            yst = [SB("yst%d" % i, [128, 4, 512], BF16, p2) for i in range(2)]

            kb.op("dve", lambda e: e.memset(rbx[:], -8e30), writes=["rbx"])
            kb.dma("sp", rb32[:], rel_bias, "rb32", writes=["rb32"])
            kb.dma("sp", b31_32[:], rel_bias[31:32, :].partition_broadcast(32), "b31_32", writes=["b31_32"])
            kb.dma("sp", b31_bc[:], rel_bias[31:32, :].partition_broadcast(128), "b31_bc", writes=["b31_bc"])
            kb.op("dve", lambda e: e.tensor_tensor(out=rb32[:], in0=rb32[:], in1=b31_32[:], op=ALU.subtract),
                  reads=["rb32", "b31_32"], writes=["rb32"])
            kb.op("dve", lambda e: e.tensor_scalar_mul(out=rbx[0:32, :], in0=rb32[:], scalar1=8.0),
                  reads=["rb32", "rbx"], writes=["rbx"])
            for pc in range(8):
                rk = ("rmk", pc % 2)
                kb.dma("sp", rmk[pc % 2][:], relmask[:, pc * 4096:(pc + 1) * 4096], rk, writes=[rk])
                for n in range(8):
                    pi = n % 4
                    kb.op("pe", lambda e: e.matmul(ps[pi][0:4, :], lhsT=rbx[:, :], rhs=rmk[pc % 2][:, n * 512:(n + 1) * 512],
                                                   start=True, stop=True),
                          reads=["rbx", rk], writes=[("ps", pi)])
                    kb.op("dve", lambda e: e.tensor_copy(out=tabst[pc % 2][:, n * 512:(n + 1) * 512], in_=ps[pi][0:4, :]),
                          reads=[("ps", pi)], writes=[("tabst", pc % 2, n)])
                kb.dma("sp", tab_s[:, pc * 4096:(pc + 1) * 4096], tabst[pc % 2][:], ("tabst", pc % 2),
                       reads=[("tabst", pc % 2, n) for n in range(8)])
            for i, la in enumerate((lam_q1, lam_k1, lam_q2, lam_k2)):
                kb.dma("sp", lamt[:, i, :], la.partition_broadcast(128), "lamt", writes=["lamt"])
            kb.dma("sp", gsub[:], subln_g.partition_broadcast(128), "gsub", writes=["gsub"])
            kb.op("dve", lambda e: e.tensor_scalar_mul(out=gsub[:], in0=gsub[:], scalar1=1.0 - LAMBDA_INIT),
                  reads=["gsub"], writes=["gsub"])
            for i in range(2):
                kb.op("dve", lambda e: e.tensor_tensor(out=lamt[:, 2 * i, :], in0=lamt[:, 2 * i, :],
                                                       in1=lamt[:, 2 * i + 1, :], op=ALU.mult),
                      reads=["lamt"], writes=["lamt"])
                kb.op("dve", lambda e: e.reduce_sum(out=lams[:, i:i + 1], in_=lamt[:, 2 * i, :], axis=AX.X),
                      reads=["lamt"], writes=["lams"])
            kb.op("act", lambda e: e.activation(out=lams[:, 2:4], in_=lams[:, 0:2], func=AF.Exp),
                  reads=["lams"], writes=["lams"])
            kb.op("dve", lambda e: e.tensor_tensor(out=lams[:, 4:5], in0=lams[:, 3:4], in1=lams[:, 2:3], op=ALU.subtract),
                  reads=["lams"], writes=["lams"])
            kb.op("dve", lambda e: e.tensor_scalar_add(out=lams[:, 5:6], in0=lams[:, 4:5], scalar1=-LAMBDA_INIT),
                  reads=["lams"], writes=["lams"])
            kb.barrier()
            kb.dma("sp", T8[:], tab_s.rearrange("h (b k q) -> k (h b) q", b=2, k=128), "T8", writes=["T8"])
            kb.op("act", lambda e: e.activation(out=E8[:], in_=T8[:], func=AF.Exp, scale=0.125), reads=["T8"],
                  writes=["E8"])
            kb.op("dve", lambda e: e.memset(vsb[:, :, :, 128:130], 1.0), writes=["vones"])

            scnt = 0
            ptc = 0
            pend = []
            LOOK = 5

            def flush(keep):
                while len(pend) > keep:
                    pend.pop(0)()

            def emit_qk_exp(i, h, m, j0, nb, si, pt, ptk):
                for jj in range(nb):
                    j = j0 + jj
                    near = (j >= i - 1)
                    kb.op("pe", lambda e: e.matmul(
                        ps[si][:, jj * 128:(jj + 1) * 128],
                        lhsT=kT[m * 64:(m + 1) * 64, h, j * 128:(j + 1) * 128],
                        rhs=qT[m * 64:(m + 1) * 64, h, i * 128:(i + 1) * 128],
                        start=True, stop=True),
                          reads=[("kT", h), ("qT", h)], writes=[("ps", si)])
                kb.op("act", lambda e: e.activation(out=pt[:, 0:nb * 128], in_=ps[si][:, 0:nb * 128],
                                                    func=AF.Exp, scale=0.125, bias=b31_bc[:, h:h + 1]),
                      reads=[("ps", si), "b31_bc"], writes=[ptk])
                for jj in range(nb):
                    j = j0 + jj
                    if j >= i - 1:
                        kb.op("dve", lambda e: e.tensor_tensor(out=pt[:, jj * 128:(jj + 1) * 128],
                                                               in0=pt[:, jj * 128:(jj + 1) * 128],
                                                               in1=E8[:, h * 2 + (i - j), :], op=ALU.mult),
                              reads=[ptk, "E8"], writes=[ptk])

            def emit_pv(i, h, m, j0, nb, pt, ptk):
                pob = ps[4 + h % 2]
                pok = ("ps", 4 + h % 2)
                for jj in range(nb):
                    j = j0 + jj
                    kb.op("pe", lambda e: e.matmul(
                        pob[:, m * 256:m * 256 + 129], lhsT=pt[:, jj * 128:(jj + 1) * 128],
                        rhs=vsb[:, j, h, 0:129], start=(j == 0), stop=(j == i)),
                          reads=[ptk, ("vsb", h), "vones"], writes=[pok])

            def emit_fin(i, h):
                ob = osb[i % 2]
                ok = ("osb", i % 2)
                smt = sm[i % 2]
                smk = ("sm", i % 2)
                pob = ps[4 + h % 2]
                pok = ("ps", 4 + h % 2)
                kb.op("dve", lambda e: e.reciprocal(out=smt[:, 0:1], in_=pob[:, 128:129]),
                      reads=[pok], writes=[smk])
                kb.op("dve", lambda e: e.reciprocal(out=smt[:, 1:2], in_=pob[:, 384:385]),
                      reads=[pok], writes=[smk])
                kb.op("dve", lambda e: e.tensor_tensor(out=smt[:, 2:3], in0=smt[:, 1:2], in1=lams[:, 5:6],
                                                       op=ALU.mult),
                      reads=[smk, "lams"], writes=[smk])
                kb.op("dve", lambda e: e.tensor_scalar_mul(out=ob[:, h * 128:(h + 1) * 128], in0=pob[:, 0:128],
                                                           scalar1=smt[:, 0:1]),
                      reads=[pok, smk], writes=[ok])
                kb.op("dve", lambda e: e.scalar_tensor_tensor(out=ob[:, h * 128:(h + 1) * 128],
                                                              in0=pob[:, 256:384], scalar=smt[:, 2:3],
                                                              in1=ob[:, h * 128:(h + 1) * 128],
                                                              op0=ALU.mult, op1=ALU.add),
                      reads=[pok, smk, ok], writes=[ok])
                kb.op("act", lambda e: e.activation(out=ojunk[:], in_=ob[:, h * 128:(h + 1) * 128],
                                                    func=AF.Square, accum_out=smt[:, 4 + h:5 + h]),
                      reads=[ok], writes=["ojunk", (smk, "ss")])

            def emit_post(b, i):
                ob = osb[i % 2]
                ok = ("osb", i % 2)
                smt = sm[i % 2]
                smk = ("sm", i % 2)
                kb.op("dve", lambda e: e.tensor_scalar(out=smt[:, 8:12], in0=smt[:, 4:8], scalar1=1.0 / 128,
                                                       scalar2=1e-5, op0=ALU.mult, op1=ALU.add),
                      reads=[(smk, "ss")], writes=[(smk, "v")])
                kb.op("act", lambda e: e.activation(out=smt[:, 12:16], in_=smt[:, 8:12], func=AF.Ln),
                      reads=[(smk, "v")], writes=[(smk, "ln")])
                kb.op("act", lambda e: e.activation(out=smt[:, 8:12], in_=smt[:, 12:16], func=AF.Exp, scale=-0.5),
                      reads=[(smk, "ln")], writes=[(smk, "v")])
                for h in range(4):
                    kb.op("dve", lambda e: e.scalar_tensor_tensor(out=ob[:, h * 128:(h + 1) * 128],
                                                                  in0=ob[:, h * 128:(h + 1) * 128],
                                                                  scalar=smt[:, 8 + h:9 + h], in1=gsub[:],
                                                                  op0=ALU.mult, op1=ALU.mult),
                          reads=[ok, (smk, "v"), "gsub"], writes=[ok])
                ysl = (i // 4) % 2
                for h in range(4):
                    kb.op("pe", lambda e: e.transpose(ps[6][:, h * 128:(h + 1) * 128], ob[:, h * 128:(h + 1) * 128],
                                                      ident[:]),
                          reads=[ok], writes=[("ps", 6)])
                kb.op("act", lambda e: e.copy(out=yst[ysl][:, :, (i % 4) * 128:(i % 4 + 1) * 128],
                                              in_=ps[6][:].rearrange("p (h q) -> p h q", h=4)),
                      reads=[("ps", 6)], writes=[("yst", ysl, i % 4)])
                if i % 4 == 3:
                    c0 = (i // 4) * 512
                    kb.dma("pool", yaT_s[b, :, :, c0:c0 + 512].rearrange("m p t -> p m t"), yst[ysl][:],
                           ("yst", ysl), reads=[("yst", ysl, t) for t in range(4)])

            for b in range(NS):
                flush(0)
                for h in range(4):
                    kb.dma("sp", kT[:, h, :], kT_s[b, h], ("kT", h), writes=[("kT", h)])
                    kb.dma("sp", qT[:, h, :], qT_s[b, h], ("qT", h), writes=[("qT", h)])
                for h in range(4):
                    kb.dma("sp", vsb[:, :, h, 0:128],
                           v_s[b, :, h * 128:(h + 1) * 128].rearrange("(t p) d -> p t d", p=128), ("vsb", h),
                           writes=[("vsb", h)])
                for i in range(NT):
                    for h in range(4):
                        for m in range(2):
                            j0 = 0
                            while j0 <= i:
                                nb = min(4, i + 1 - j0)
                                si = scnt % 4
                                scnt += 1
                                pt = pT[ptc % 6]
                                ptk = ("pT", ptc % 6)
                                ptc += 1
                                emit_qk_exp(i, h, m, j0, nb, si, pt, ptk)
                                pend.append(functools.partial(emit_pv, i, h, m, j0, nb, pt, ptk))
                                flush(LOOK)
                                j0 += nb
                        pend.append(functools.partial(emit_fin, i, h))
                    pend.append(functools.partial(emit_post, b, i))
            flush(0)
            kb.barrier()

        with contextlib.ExitStack() as p3:
            TWO_PI = 2.0 * math.pi
            prm = SB("prm", [128, 24, 16], F32, p3)
            Bpad = SB("Bpad", [128, 2, 16, 128], F32, p3)
            Cpad = SB("Cpad", [128, 3, 16, 128], F32, p3)
            dcol = SB("dcol", [128, 4], F32, p3)
            Dg = SB("Dg", [128, 4, 128], F32, p3)
            wglub = SB("wglub", [128, 4, 1024], BF16, p3)
            carry = SB("carry", [128, 16, 2], F32, p3)
            cs_tab = SB("cs_tab", [128, 16, 512], F32, p3)
            sn_tab = SB("sn_tab", [128, 16, 512], F32, p3)
            ctmp = SB("ctmp", [128, 4], F32, p3)
            p3s = contextlib.ExitStack()
            bnat = SB("bnat", [128, 2, 16, 16], F32, p3s)
            bbT = SB("bbT", [128, 2, 16, 16], F32, p3s)
            bbX = SB("bbX", [128, 128], F32, p3s)
            CT = SB("CT", [128, 2, 16, 16], F32, p3s)
            negpi = SB("negpi", [128, 1], F32, p3s)
            tpos = SB("tpos", [128, 512], F32, p3s)
            tq_ = SB("tq_", [128, 512], F32, p3s)
            tn_ = SB("tn_", [128, 512], F32, p3s)
            tc_ = SB("tc_", [128, 512], F32, p3s)
            tph = SB("tph", [128, 512], F32, p3s)

            P = lambda i: prm[:, i, :]
            LRE, LIM, STEP, MAG, ANG, ARE, AIM, DEN, NRE, CRE, CIM, FT, TA, TB, TC, F512, S512, C512 = range(18)
            kb.dma("sp", P(LRE), ssm_lre.rearrange("(gt gi) p -> gi p gt", gi=2)[0], "prm0", writes=["prm"],
                   allow_slow_non_contiguous=True) if False else None
            for gi in range(2):
                kb.dma("sp", prm[gi * 64:(gi + 1) * 64, LRE, :], ssm_lre.rearrange("(gt gi) p -> gi p gt", gi=2)[gi],
                       "prm", writes=["prm"], allow_slow_non_contiguous=True)
                kb.dma("sp", prm[gi * 64:(gi + 1) * 64, LIM, :], ssm_lim.rearrange("(gt gi) p -> gi p gt", gi=2)[gi],
                       "prm", writes=["prm"], allow_slow_non_contiguous=True)
                kb.dma("sp", prm[gi * 64:(gi + 1) * 64, STEP, :],
                       ssm_ls.rearrange("(gt gi) o -> gi (o gt)", gi=2)[gi:gi + 1, :].partition_broadcast(64),
                       "prm", writes=["prm"], allow_slow_non_contiguous=True)
                for ri, src in enumerate((ssm_bre, ssm_bim)):
                    kb.dma("sp", bnat[gi * 64:(gi + 1) * 64, ri, :, :],
                           src.rearrange("(gt gi) p h -> gi p gt h", gi=2)[gi], "bnat", writes=["bnat"])
                for ri, src in enumerate((ssm_cre, ssm_cim)):
                    for gt in range(16):
                        kb.dma("sp", CT[gi * 64:(gi + 1) * 64, ri, gt, :],
                               src[2 * gt + gi].rearrange("h p -> p h"), "CT", writes=["CT"],
                               allow_slow_non_contiguous=True)
            kb.dma("sp", dcol[:], ssm_d.rearrange("(cc p) o -> p (cc o)", p=128), "dcol", writes=["dcol"],
                   allow_slow_non_contiguous=True)
            for k in range(4):
                kb.dma("pool", wglub[:, k, :], w_glu[k * 128:(k + 1) * 128, :], ("wglub", k), writes=[("wglub", k)])
            kb.op("pool", lambda e: e.iota(tpos[:], pattern=[[1, 512]], base=0, channel_multiplier=0,
                                           allow_small_or_imprecise_dtypes=True), writes=["tpos"])
            kb.op("dve", lambda e: e.memset(negpi[:], -math.pi), writes=["negpi"])

            def V(fn, rd=("prm",), wr=("prm",)):
                kb.op("dve", fn, reads=list(rd), writes=list(wr))
            V(lambda e: e.tensor_scalar_min(out=P(LRE), in0=P(LRE), scalar1=-1e-4))
            kb.op("act", lambda e: e.activation(out=P(STEP), in_=P(STEP), func=AF.Exp), reads=["prm"], writes=["prm"])
            V(lambda e: e.tensor_tensor(out=P(TA), in0=P(LRE), in1=P(STEP), op=ALU.mult))
            kb.op("act", lambda e: e.activation(out=P(MAG), in_=P(TA), func=AF.Exp), reads=["prm"], writes=["prm"])
            V(lambda e: e.tensor_tensor(out=P(ANG), in0=P(LIM), in1=P(STEP), op=ALU.mult))
            MAGIC = 12582912.0

            def sincos(out_sin, out_cos, turns, tq, tn, tc, rk, wk):
                kb.op("pool", lambda e: e.tensor_scalar_add(out=tq, in0=turns, scalar1=MAGIC), reads=rk, writes=wk)
                kb.op("dve", lambda e: e.scalar_tensor_tensor(out=tn, in0=tq, scalar=MAGIC, in1=turns,
                                                              op0=ALU.subtract, op1=ALU.subtract), reads=rk + wk, writes=wk)
                kb.op("act", lambda e: e.activation(out=out_sin, in_=tn, func=AF.Sin, scale=-TWO_PI), reads=wk, writes=wk)
                kb.op("pool", lambda e: e.tensor_scalar_add(out=tc, in0=turns, scalar1=0.25), reads=rk + wk, writes=wk)
                kb.op("pool", lambda e: e.tensor_scalar_add(out=tq, in0=tc, scalar1=MAGIC), reads=wk, writes=wk)
                kb.op("dve", lambda e: e.scalar_tensor_tensor(out=tn, in0=tq, scalar=MAGIC, in1=tc,
                                                              op0=ALU.subtract, op1=ALU.subtract), reads=wk, writes=wk)
                kb.op("act", lambda e: e.activation(out=out_cos, in_=tn, func=AF.Sin, scale=-TWO_PI), reads=wk, writes=wk)

            V(lambda e: e.tensor_scalar_mul(out=P(FT), in0=P(ANG), scalar1=1.0 / TWO_PI))
            sincos(P(AIM), P(ARE), P(FT), P(TA), P(TB), P(TC), ["prm"], ["prm"])
            V(lambda e: e.tensor_scalar_mul(out=P(F512), in0=P(FT), scalar1=512.0))
            sincos(P(S512), P(C512), P(F512), P(TA), P(TB), P(TC), ["prm"], ["prm"])
            V(lambda e: e.tensor_tensor(out=P(AIM), in0=P(AIM), in1=P(MAG), op=ALU.mult))
            V(lambda e: e.tensor_tensor(out=P(ARE), in0=P(ARE), in1=P(MAG), op=ALU.mult))
            V(lambda e: e.tensor_tensor(out=P(DEN), in0=P(LRE), in1=P(LRE), op=ALU.mult))
            V(lambda e: e.tensor_tensor(out=P(TA), in0=P(LIM), in1=P(LIM), op=ALU.mult))
            V(lambda e: e.tensor_tensor(out=P(DEN), in0=P(DEN), in1=P(TA), op=ALU.add))
            V(lambda e: e.reciprocal(out=P(DEN), in_=P(DEN)))
            V(lambda e: e.tensor_scalar_add(out=P(NRE), in0=P(ARE), scalar1=-1.0))
            V(lambda e: e.tensor_tensor(out=P(TA), in0=P(NRE), in1=P(LRE), op=ALU.mult))
            V(lambda e: e.tensor_tensor(out=P(TB), in0=P(AIM), in1=P(LIM), op=ALU.mult))
            V(lambda e: e.tensor_tensor(out=P(TA), in0=P(TA), in1=P(TB), op=ALU.add))
            V(lambda e: e.tensor_tensor(out=P(CRE), in0=P(TA), in1=P(DEN), op=ALU.mult))
            V(lambda e: e.tensor_tensor(out=P(TA), in0=P(AIM), in1=P(LRE), op=ALU.mult))
            V(lambda e: e.tensor_tensor(out=P(TB), in0=P(NRE), in1=P(LIM), op=ALU.mult))
            V(lambda e: e.tensor_tensor(out=P(TA), in0=P(TA), in1=P(TB), op=ALU.subtract))
            V(lambda e: e.tensor_tensor(out=P(CIM), in0=P(TA), in1=P(DEN), op=ALU.mult))
            for gt in range(16):
                cre = prm[:, CRE, gt:gt + 1]
                cim = prm[:, CIM, gt:gt + 1]
                V(lambda e: e.tensor_scalar_mul(out=bbT[:, 0, gt, :], in0=bnat[:, 1, gt, :], scalar1=cim),
                  rd=("prm", "bnat"), wr=("bbT",))
                V(lambda e: e.scalar_tensor_tensor(out=bbT[:, 0, gt, :], in0=bnat[:, 0, gt, :], scalar=cre,
                                                   in1=bbT[:, 0, gt, :], op0=ALU.mult, op1=ALU.subtract),
                  rd=("prm", "bnat", "bbT"), wr=("bbT",))
                V(lambda e: e.tensor_scalar_mul(out=bbT[:, 1, gt, :], in0=bnat[:, 0, gt, :], scalar1=cim),
                  rd=("prm", "bnat"), wr=("bbT",))
                V(lambda e: e.scalar_tensor_tensor(out=bbT[:, 1, gt, :], in0=bnat[:, 1, gt, :], scalar=cre,
                                                   in1=bbT[:, 1, gt, :], op0=ALU.mult, op1=ALU.add),
                  rd=("prm", "bnat", "bbT"), wr=("bbT",))
            kb.op("pool", lambda e: e.memset(Cpad[:], 0.0), writes=["Cpad"])
            for gt in range(16):
                for ri in range(2):
                    V(lambda e: e.memset(bbX[:], 0.0), rd=(), wr=("bbX",))
                    for gi in range(2):
                        c0 = (gt % 4) * 32 + gi * 16
                        V(lambda e: e.tensor_copy(out=bbX[gi * 64:(gi + 1) * 64, c0:c0 + 16],
                                                  in_=bbT[gi * 64:(gi + 1) * 64, ri, gt, :]),
                          rd=("bbT", "bbX"), wr=("bbX",))
                    pi = (gt * 2 + ri) % 4
                    kb.op("pe", lambda e: e.transpose(ps[pi][:, 0:128], bbX[:], ident[:]), reads=["bbX"],
                          writes=[("ps", pi)])
                    kb.op("act", lambda e: e.copy(out=Bpad[:, ri, gt, :], in_=ps[pi][:, 0:128]),
                          reads=[("ps", pi)], writes=["Bpad"])
                for gi in range(2):
                    c0 = (gt % 4) * 32 + gi * 16
                    rows = slice(gi * 64, (gi + 1) * 64)
                    kb.op("pool", lambda e: e.tensor_copy(out=Cpad[rows, 0, gt, c0:c0 + 16], in_=CT[rows, 0, gt, :]),
                          reads=["CT", "Cpad"], writes=["Cpad"])
                    kb.op("pool", lambda e: e.tensor_scalar_mul(out=Cpad[rows, 1, gt, c0:c0 + 16],
                                                                in0=CT[rows, 0, gt, :], scalar1=-1.0),
                          reads=["CT", "Cpad"], writes=["Cpad"])
                    kb.op("pool", lambda e: e.tensor_scalar_mul(out=Cpad[rows, 2, gt, c0:c0 + 16],
                                                                in0=CT[rows, 1, gt, :], scalar1=-1.0),
                          reads=["CT", "Cpad"], writes=["Cpad"])
            for cc in range(4):
                V(lambda e: e.tensor_scalar_mul(out=Dg[:, cc, :], in0=ident[:], scalar1=dcol[:, cc:cc + 1]),
                  rd=("dcol",), wr=("Dg",))

            for gt in range(16):
                kb.op("dve", lambda e: e.tensor_scalar_mul(out=tph[:], in0=tpos[:], scalar1=prm[:, FT, gt:gt + 1]),
                      reads=["tpos", "prm", "tabw"], writes=["tph"])
                sincos(sn_tab[:, gt, :], cs_tab[:, gt, :], tph[:], tq_[:], tn_[:], tc_[:], ["tph"], ["tabw"])

            kb.barrier()
            p3s.close()
            uTt = [SB("uTt%d" % i, [128, 4, 512], F32, p3) for i in range(2)]
            W = {}
            for nm in ("t1", "t2", "t3", "t4", "bre", "bim", "wre", "wim", "p1", "p2", "p3", "p4"):
                W[nm] = [SB("w_%s%d" % (nm, i), [128, 512], F32, p3) for i in range(2)]
            ysb = [SB("ysb0", [128, 512], F32, p3)] * 2
            gt1 = [SB("gt1_0", [128, 512], F32, p3)] * 2
            gth = [SB("gth0", [128, 512], F32, p3)] * 2
            gyT = [SB("gyT%d" % i, [128, 4, 512], BF16, p3) for i in range(2)]
            sgt = [SB("sgt0", [128, 512], F32, p3)] * 2
            ysT = [SB("ysT0", [128, 4, 512], BF16, p3)] * 2
            TT = lambda e, o, a, b_, op: e.tensor_tensor(out=o, in0=a, in1=b_, op=op)

            def ssm_s1(us, cc, gt, s2):
                uk = ("uTt", us)
                pa, pb_ = ps[s2 * 2], ps[s2 * 2 + 1]
                pak, pbk = ("ps", s2 * 2), ("ps", s2 * 2 + 1)
                kb.op("pe", lambda e: e.matmul(pa[:], lhsT=Bpad[:, 0, gt, :], rhs=uTt[us][:, cc, :],
                                               start=True, stop=True), reads=["Bpad", uk], writes=[pak])
                kb.op("pe", lambda e: e.matmul(pb_[:], lhsT=Bpad[:, 1, gt, :], rhs=uTt[us][:, cc, :],
                                               start=True, stop=True), reads=["Bpad", uk], writes=[pbk])

            def ssm_s23(gt, s2, kb):
                w = {k_: v_[s2] for k_, v_ in W.items()}
                K_ = lambda nm: (nm, s2)
                cs_, sn_ = cs_tab[:, gt, :], sn_tab[:, gt, :]
                pa, pb_ = ps[s2 * 2], ps[s2 * 2 + 1]
                pak, pbk = ("ps", s2 * 2), ("ps", s2 * 2 + 1)
                kb.op("dve", lambda e: TT(e, w["t1"][:], pa[:], cs_, ALU.mult), reads=[pak, "tabw"], writes=[K_("t1")])
                kb.op("dve", lambda e: TT(e, w["t2"][:], pb_[:], sn_, ALU.mult), reads=[pbk, "tabw"], writes=[K_("t2")])
                kb.op("dve", lambda e: TT(e, w["t3"][:], pb_[:], cs_, ALU.mult), reads=[pbk, "tabw"], writes=[K_("t3")])
                kb.op("dve", lambda e: TT(e, w["t4"][:], pa[:], sn_, ALU.mult), reads=[pak, "tabw"], writes=[K_("t4")])
                kb.op("dve", lambda e: TT(e, w["bre"][:], w["t1"][:], w["t2"][:], ALU.add),
                      reads=[K_("t1"), K_("t2")], writes=[K_("bre")])
                kb.op("dve", lambda e: TT(e, w["bim"][:], w["t3"][:], w["t4"][:], ALU.subtract),
                      reads=[K_("t3"), K_("t4")], writes=[K_("bim")])

            def ssm_back(gt, s2, yb, first, kb):
                w = {k_: v_[s2] for k_, v_ in W.items()}
                K_ = lambda nm: (nm, s2)
                cs_, sn_ = cs_tab[:, gt, :], sn_tab[:, gt, :]
                for ri, (bn, wn) in enumerate((("bre", "wre"), ("bim", "wim"))):
                    kb.op("dve", lambda e, ri=ri, bn=bn, wn=wn: e.tensor_tensor_scan(
                        out=w[wn][:], data0=prm[:, MAG, gt:gt + 1].to_broadcast([128, 512]),
                        data1=w[bn][:], initial=carry[:, gt, ri:ri + 1], op0=ALU.mult, op1=ALU.add),
                          reads=[K_(bn), "prm", ("carry", gt)], writes=[K_(wn)])
                wl_re, wl_im = w["wre"][:, 511:512], w["wim"][:, 511:512]
                c5, s5 = prm[:, C512, gt:gt + 1], prm[:, S512, gt:gt + 1]
                kb.op("dve", lambda e: e.tensor_tensor(out=ctmp[:, 0:1], in0=wl_im, in1=s5, op=ALU.mult),
                      reads=[K_("wim"), "prm", "ctmp"], writes=["ctmp"])
                kb.op("dve", lambda e: e.tensor_tensor(out=ctmp[:, 1:2], in0=wl_im, in1=c5, op=ALU.mult),
                      reads=[K_("wim"), "prm", "ctmp"], writes=["ctmp"])
                kb.op("dve", lambda e: e.scalar_tensor_tensor(out=carry[:, gt, 0:1], in0=wl_re, scalar=c5,
                                                              in1=ctmp[:, 0:1], op0=ALU.mult, op1=ALU.subtract),
                      reads=[K_("wre"), "prm", "ctmp", ("carry", gt)], writes=[("carry", gt)])
                kb.op("dve", lambda e: e.scalar_tensor_tensor(out=carry[:, gt, 1:2], in0=wl_re, scalar=s5,
                                                              in1=ctmp[:, 1:2], op0=ALU.mult, op1=ALU.add),
                      reads=[K_("wre"), "prm", "ctmp", ("carry", gt)], writes=[("carry", gt)])
                kb.op("dve", lambda e: TT(e, w["p1"][:], cs_, w["wre"][:], ALU.mult),
                      reads=["tabw", K_("wre")], writes=[K_("p1")])
                kb.op("pool", lambda e: TT(e, w["p2"][:], sn_, w["wim"][:], ALU.mult),
                      reads=["tabw", K_("wim")], writes=[K_("p2")])
                kb.op("dve", lambda e: TT(e, w["p3"][:], sn_, w["wre"][:], ALU.mult),
                      reads=["tabw", K_("wre")], writes=[K_("p3")])
                kb.op("pool", lambda e: TT(e, w["p4"][:], cs_, w["wim"][:], ALU.mult),
                      reads=["tabw", K_("wim")], writes=[K_("p4")])
                for qi, (pn, ci) in enumerate((("p1", 0), ("p2", 1), ("p3", 2), ("p4", 2))):
                    kb.op("pe", lambda e, qi=qi, pn=pn, ci=ci: e.matmul(ps[yb][:], lhsT=Cpad[:, ci, gt, :], rhs=w[pn][:],
                                                                        start=(first and qi == 0), stop=False),
                          reads=["Cpad", K_(pn)], writes=[("ps", yb)])

            def ssm_ccend(us, cc, yb):
                uk = ("uTt", us)
                kb.op("pe", lambda e: e.matmul(ps[yb][:], lhsT=Dg[:, cc, :], rhs=uTt[us][:, cc, :],
                                               start=False, stop=True),
                      reads=["Dg", uk], writes=[("ps", yb)])
                kb.op("act", lambda e: e.activation(out=gyT[us][:, cc, :], in_=ps[yb][:], func=AF.Gelu_apprx_tanh),
                      reads=[("ps", yb)], writes=[("gyT", us, cc)])

            def ssm_glu(b, n, us):
                for m in range(4):
                    for half in range(2):
                        pi = 6 + half
                        mm = m + 4 * half
                        for k in range(4):
                            kb.op("pe", lambda e: e.matmul(ps[pi][:], lhsT=wglub[:, k, mm * 128:(mm + 1) * 128],
                                                           rhs=gyT[us][:, k, :], start=(k == 0), stop=(k == 3)),
                                  reads=[("wglub", k), ("gyT", us, k)], writes=[("ps", pi)])
                    sg_ = sgt[m % 2]
                    kb.op("act", lambda e: e.activation(out=sg_[:], in_=ps[7][:], func=AF.Sigmoid),
                          reads=[("ps", 7)], writes=[("sgt", 0)])
                    kb.op("dve", lambda e: e.tensor_tensor(out=ysT[us][:, m, :], in0=ps[6][:], in1=sg_[:],
                                                           op=ALU.mult),
                          reads=[("ps", 6), ("sgt", 0)], writes=[("ysT", 0, m)])
                kb.dma("act", ysT_s[b, :, :, n * 512:(n + 1) * 512].rearrange("m p t -> p m t"), ysT[us][:],
                       ("ysT", 0), reads=[("ysT", 0, m) for m in range(4)])

            items = [(b, n, cc, g4) for b in range(NS) for n in range(NG) for cc in range(4) for g4 in range(4)]
            NI = len(items)

            def do_s1(idx):
                b, n, cc, g4 = items[idx]
                us = (b * NG + n) % 2
                if cc == 0 and g4 == 0:
                    uk = ("uTt", us)
                    kb.dma("sp", uTt[us][:], uT_s[b, :, :, n * 512:(n + 1) * 512].rearrange("m p t -> p m t"), uk,
                           writes=[uk])
                ssm_s1(us, cc, cc * 4 + g4, idx % 2)

            def do_s23(idx, kb_):
                b, n, cc, g4 = items[idx]
                ssm_s23(cc * 4 + g4, idx % 2, kb_)

            do_s1(0)
            if NI > 1:
                do_s1(1)
            do_s23(0, kb)
            for idx in range(NI):
                b, n, cc, g4 = items[idx]
                us = (b * NG + n) % 2
                if idx + 2 < NI:
                    do_s1(idx + 2)
                rfront = Rec()
                if idx + 1 < NI:
                    do_s23(idx + 1, rfront)
                if n == 0 and cc == 0 and g4 == 0:
                    kb.op("dve", lambda e: e.memset(carry[:], 0.0),
                          reads=["carry"] + [("carry", g_) for g_ in range(16)],
                          writes=["carry"] + [("carry", g_) for g_ in range(16)])
                yb = 4 + ((b * NG + n) * 4 + cc) % 2
                rback = Rec()
                ssm_back(cc * 4 + g4, idx % 2, yb, g4 == 0, rback)
                emit_interleaved(kb, rback.L, rfront.L)
                if g4 == 3:
                    ssm_ccend(us, cc, yb)
                    if cc == 3:
                        ssm_glu(b, n, us)
            kb.barrier()

        with contextlib.ExitStack() as p4:
            wpab = SB("wpab", [128, 4, 1024], BF16, p4)
            wpsb = SB("wpsb", [128, 4, 1024], BF16, p4)
            woutb = SB("woutb", [128, 8, 1024], BF16, p4)
            wr = SB("wr", [128, 8, 36], F32, p4)
            br_bc = SB("br_bc", [128, 36], F32, p4)
            g2bc = SB("g2bc", [128, D], F32, p4)
            gate1 = SB("gate1", [128, NS, D], F32, p4)
            yaT_t = [SB("yaT_t%d" % i, [128, 4, 512], BF16, p4) for i in range(2)]
            ysT_t = [SB("ysT_t%d" % i, [128, 4, 512], BF16, p4) for i in range(2)]
            sg_t = [SB("sg_t%d" % i, [128, 16, 512], BF16, p4) for i in range(2)]
            mga = [SB("mga%d" % i, [128, 512], F32, p4) for i in range(2)]
            mgb = [SB("mgb%d" % i, [128, 512], F32, p4) for i in range(2)]
            mrgT = SB("mrgT", [128, 8, 512], BF16, p4)
            x4 = [SB("x4_%d" % i, [128, D], F32, p4) for i in range(3)]
            xm = [SB("xm%d" % i, [128, D], F32, p4) for i in range(2)]
            xn2 = [SB("xn2_%d" % i, [128, D], F32, p4) for i in range(2)]
            junk4 = SB("junk4", [128, D], F32, p4)
            st4 = [SB("st4_%d" % i, [128, 8], F32, p4) for i in range(2)]
            h2f = [SB("h2f%d" % i, [128, 8, 128], F32, p4) for i in range(2)]
            rt = [SB("rt%d" % i, [128, 160], F32, p4) for i in range(2)]
            mod2 = SB("mod2", [128, NS, 2, D], F32, p4)
            h2t = [SB("h2t%d" % i, [128, D], F32, p4) for i in range(2)]
            Sprev = SB("Sprev", [128, 64], F32, p4)
            iota_e = SB("iota_e", [128, 64], F32, p4)
            iota_p = SB("iota_p", [128, 1], F32, p4)
            tmp64 = [SB("tmp64_%d" % i, [128, 64], F32, p4) for i in range(2)]
            rtt = [SB("rtt%d" % i, [128, 8], F32, p4) for i in range(2)]
            for b in range(NS):
                kb.dma("sp", mod2[:, b, :, :], gate_s[:, (b * 4 + 2) * D:(b * 4 + 4) * D].rearrange("p (g d) -> p g d", g=2),
                       "mod2", writes=["mod2"])
            kb.op("dve", lambda e: e.memset(Sprev[:], 0.0), writes=["Sprev"])
            kb.op("pool", lambda e: e.iota(iota_e[:], pattern=[[0, 2], [1, 32]], base=0, channel_multiplier=0,
                                           allow_small_or_imprecise_dtypes=True), writes=["iota_e"])
            kb.op("pool", lambda e: e.iota(iota_p[:], pattern=[[0, 1]], base=0, channel_multiplier=1,
                                           allow_small_or_imprecise_dtypes=True), writes=["iota_p"])

            for k in range(4):
                kb.dma("pool", wpab[:, k, :], w_pa[k * 128:(k + 1) * 128, :], ("wpab", k), writes=[("wpab", k)])
                kb.dma("pool", wpsb[:, k, :], w_ps[k * 128:(k + 1) * 128, :], ("wpsb", k), writes=[("wpsb", k)])
            for k in range(8):
                kb.dma("pool", woutb[:, k, :], w_out[k * 128:(k + 1) * 128, :], ("woutb", k), writes=[("woutb", k)])
            kb.dma("sp", wr[:, :, 0:4], w_rg.rearrange("(k p) n -> p k n", p=128), "wr", writes=["wr"],
                   allow_slow_non_contiguous=True)
            for g in range(4):
                kb.dma("sp", wr[:, :, 4 + g * 8:12 + g * 8], w_re[g].rearrange("(k p) n -> p k n", p=128), "wr",
                       writes=["wr"], allow_slow_non_contiguous=True)
            kb.dma("sp", br_bc[:, 0:4], b_rg.partition_broadcast(128), "br_bc", writes=["br_bc"])
            kb.dma("sp", br_bc[:, 4:36], b_re.partition_broadcast(128), "br_bc", writes=["br_bc"])
            kb.dma("sp", g2bc[:], norm2_g.partition_broadcast(128), "g2bc", writes=["g2bc"])
            for b in range(NS):
                kb.dma("sp", gate1[:, b, :], gate_s[:, (b * 4) * D:(b * 4 + 1) * D], "gate1", writes=["gate1"])

            mcnt = [0]

            def p4_loadg(b, n):
                gs = (b * NG + n) % 2
                c0 = n * 512
                kb.dma("sp", yaT_t[gs][:], yaT_s[b, :, :, c0:c0 + 512].rearrange("m p t -> p m t"), ("yaT_t", gs),
                       writes=[("yaT_t", gs)])
                kb.dma("sp", ysT_t[gs][:], ysT_s[b, :, :, c0:c0 + 512].rearrange("m p t -> p m t"), ("ysT_t", gs),
                       writes=[("ysT_t", gs)])
                kb.dma("sp", sg_t[gs][:], sg_s[b, :, :, c0:c0 + 512].rearrange("m p t -> p m t"), ("sg_t", gs),
                       writes=[("sg_t", gs)])

            def p4_M(b, n):
                gs = (b * NG + n) % 2
                c0 = n * 512
                for m in range(8):
                    ia, ib = 4 + mcnt[0] % 3, 4 + (mcnt[0] + 1) % 3
                    mcnt[0] += 2
                    pA, pB = ps[ia], ps[ib]
                    pAk, pBk = ("ps", ia), ("ps", ib)
                    for k in range(4):
                        kb.op("pe", lambda e: e.matmul(pA[:], lhsT=wpab[:, k, m * 128:(m + 1) * 128],
                                                       rhs=yaT_t[gs][:, k, :], start=(k == 0), stop=(k == 3)),
                              reads=[("wpab", k), ("yaT_t", gs)], writes=[pAk])
                    for k in range(4):
                        kb.op("pe", lambda e: e.matmul(pB[:], lhsT=wpsb[:, k, m * 128:(m + 1) * 128],
                                                       rhs=ysT_t[gs][:, k, :], start=(k == 0), stop=(k == 3)),
                              reads=[("wpsb", k), ("ysT_t", gs)], writes=[pBk])
                    kb.op("dve", lambda e: e.tensor_tensor(out=mga[m % 2][:], in0=pA[:], in1=sg_t[gs][:, m, :],
                                                           op=ALU.mult),
                          reads=[pAk, ("sg_t", gs)], writes=[("mga", m % 2)])
                    kb.op("dve", lambda e: e.tensor_tensor(out=mgb[m % 2][:], in0=pB[:], in1=sg_t[gs][:, 8 + m, :],
                                                           op=ALU.mult),
                          reads=[pBk, ("sg_t", gs)], writes=[("mgb", m % 2)])
                    kb.op("dve", lambda e: e.tensor_tensor(out=mrgT[:, m, :], in0=mga[m % 2][:], in1=mgb[m % 2][:],
                                                            op=ALU.add),
                          reads=[("mga", m % 2), ("mgb", m % 2)], writes=[("mrgT", m)])

            def p4_loadx(idx):
                b, n, t = tiles4[idx]
                tok0 = n * 512 + t * 128
                kb.dma("sp", x4[idx % 3][:], x[b, tok0:tok0 + 128, :], ("x4", idx % 3), writes=[("x4", idx % 3)])

            def p4_A(b, n, t, sl, xsl):
                gs = (b * NG + n) % 2
                c0 = n * 512
                tok0 = c0 + t * 128
                st = st4[sl]
                for half in range(2):
                    pi = half
                    for k in range(8):
                        kb.op("pe", lambda e: e.matmul(ps[pi][:], lhsT=mrgT[:, k, t * 128:(t + 1) * 128],
                                                       rhs=woutb[:, k, half * 512:(half + 1) * 512],
                                                       start=(k == 0), stop=(k == 7)),
                              reads=[("mrgT", k), ("woutb", k)], writes=[("ps", pi)])
                    hs = slice(half * 512, (half + 1) * 512)
                    kb.op("dve", lambda e: e.tensor_tensor(out=xm[sl][:, hs], in0=ps[pi][:], in1=gate1[:, b, hs],
                                                           op=ALU.mult),
                          reads=[("ps", pi), "gate1"], writes=[("xm", sl, half)])
                    kb.op("dve", lambda e: e.tensor_tensor(out=xm[sl][:, hs], in0=xm[sl][:, hs],
                                                            in1=x4[xsl][:, hs], op=ALU.add),
                          reads=[("xm", sl, half), ("x4", xsl)], writes=[("xm", sl, half)])
                xmk = [("xm", sl, 0), ("xm", sl, 1)]
                kb.dma("sp", xmid_s[b, tok0:tok0 + 128, :], xm[sl][:], ("xm", sl), reads=xmk)
                kb.op("act", lambda e: e.activation(out=junk4[:], in_=xm[sl][:], func=AF.Square,
                                                    accum_out=st[:, 0:1]),
                      reads=xmk, writes=["junk4", ("st4", sl)])
                kb.op("dve", lambda e: e.tensor_scalar(out=st[:, 1:2], in0=st[:, 0:1], scalar1=1.0 / D,
                                                       scalar2=1e-6, op0=ALU.mult, op1=ALU.add),
                      reads=[("st4", sl)], writes=[("st4", sl)])
                kb.op("act", lambda e: e.activation(out=st[:, 2:3], in_=st[:, 1:2], func=AF.Ln),
                      reads=[("st4", sl)], writes=[("st4", sl)])
                kb.op("act", lambda e: e.activation(out=st[:, 3:4], in_=st[:, 2:3], func=AF.Exp, scale=-0.5),
                      reads=[("st4", sl)], writes=[("st4", sl)])
                kb.op("dve", lambda e: e.scalar_tensor_tensor(out=xn2[sl][:], in0=xm[sl][:], scalar=st[:, 3:4],
                                                              in1=g2bc[:], op0=ALU.mult, op1=ALU.mult),
                      reads=xmk + [("st4", sl), "g2bc"], writes=[("xn2", sl)])
                for k in range(8):
                    pi = 2 + k // 4
                    kb.op("pe", lambda e: e.transpose(ps[pi][:, (k % 4) * 128:(k % 4 + 1) * 128],
                                                      xn2[sl][:, k * 128:(k + 1) * 128], ident[:]),
                          reads=[("xn2", sl)], writes=[("ps", pi)])
                for k in range(8):
                    pi = 2 + k // 4
                    kb.op("act", lambda e: e.activation(out=h2f[sl][:, k, :],
                                                        in_=ps[pi][:, (k % 4) * 128:(k % 4 + 1) * 128],
                                                        func=AF.Identity, scale=modT[:, 32 + k, b:b + 1],
                                                        bias=modT[:, 24 + k, b:b + 1]),
                          reads=[("ps", pi)], writes=[("h2f", sl)])
                kb.op("dve", lambda e: e.tensor_tensor(out=h2t[sl][:], in0=xn2[sl][:], in1=mod2[:, b, 1, :],
                                                       op=ALU.mult),
                      reads=[("xn2", sl), "mod2"], writes=[("h2t", sl)])
                kb.op("dve", lambda e: e.tensor_tensor(out=h2t[sl][:], in0=h2t[sl][:], in1=mod2[:, b, 0, :],
                                                        op=ALU.add),
                      reads=[("h2t", sl), "mod2"], writes=[("h2t", sl)])
                kb.dma("sp", h2tok_s[b * S + tok0:b * S + tok0 + 128, :], h2t[sl][:], ("h2t", sl),
                       reads=[("h2t", sl)])

            def p4_B(b, n, t, sl, kb):
                gs = (b * NG + n) % 2
                c0 = n * 512
                tok0 = c0 + t * 128
                st = st4[sl]
                R = rt[sl]
                rk = ("rt", sl)
                bkB = 7 if sl == 0 else 3
                pr = ps[bkB][:, 0:128]
                for k in range(8):
                    kb.op("pe", lambda e, k=k: e.matmul(pr[:, 0:36], lhsT=h2f[sl][:, k, :], rhs=wr[:, k, :],
                                                        start=(k == 0), stop=(k == 7)),
                          reads=[("h2f", sl), "wr"], writes=[("ps", bkB)])

                def RV(fn, extra=()):
                    kb.op("dve", fn, reads=[rk] + list(extra), writes=[rk])
                L = R[:, 0:36]
                RV(lambda e: e.tensor_tensor(out=L, in0=pr[:, 0:36], in1=br_bc[:], op=ALU.add),
                   extra=[("ps", bkB), "br_bc"])
                RV(lambda e: e.reduce_max(out=R[:, 36:37], in_=R[:, 0:4], axis=AX.X))
                RV(lambda e: e.tensor_scalar(out=R[:, 40:44], in0=R[:, 0:4], scalar1=R[:, 36:37], scalar2=None,
                                             op0=ALU.is_equal))
                RV(lambda e: e.tensor_scalar_mul(out=R[:, 37:38], in0=R[:, 36:37], scalar1=-1.0))
                kb.op("act", lambda e: e.activation(out=R[:, 44:48], in_=R[:, 0:4], func=AF.Exp,
                                                    bias=R[:, 37:38], accum_out=R[:, 38:39]),
                      reads=[rk], writes=[rk])
                RV(lambda e: e.reciprocal(out=R[:, 39:40], in_=R[:, 38:39]))
                RV(lambda e: e.tensor_scalar(out=R[:, 48:52], in0=R[:, 40:44], scalar1=-1.0, scalar2=1e30,
                                             op0=ALU.add, op1=ALU.mult))
                for g in range(4):
                    RV(lambda e, g=g: e.tensor_scalar(out=R[:, 64 + g * 8:72 + g * 8], in0=R[:, 4 + g * 8:12 + g * 8],
                                                      scalar1=R[:, 48 + g:49 + g], scalar2=None, op0=ALU.add))
                LM = R[:, 64:96]
                RV(lambda e: e.reduce_max(out=R[:, 52:53], in_=LM, axis=AX.X))
                RV(lambda e: e.tensor_scalar(out=R[:, 96:128], in0=LM, scalar1=R[:, 52:53], scalar2=None,
                                             op0=ALU.is_equal))
                RV(lambda e: e.scalar_tensor_tensor(out=R[:, 128:160], in0=R[:, 96:128], scalar=-1e30, in1=LM,
                                                    op0=ALU.mult, op1=ALU.add))
                RV(lambda e: e.reduce_max(out=R[:, 53:54], in_=R[:, 128:160], axis=AX.X))
                RV(lambda e: e.tensor_scalar(out=R[:, 64:96], in0=R[:, 128:160], scalar1=R[:, 53:54],
                                             scalar2=None, op0=ALU.is_equal))
                RV(lambda e: e.tensor_tensor(out=R[:, 54:55], in0=R[:, 53:54], in1=R[:, 52:53], op=ALU.subtract))
                kb.op("act", lambda e: e.activation(out=R[:, 55:56], in_=R[:, 54:55], func=AF.Exp),
                      reads=[rk], writes=[rk])
                RV(lambda e: e.tensor_scalar_add(out=R[:, 56:57], in0=R[:, 55:56], scalar1=1.0))
                RV(lambda e: e.reciprocal(out=R[:, 57:58], in_=R[:, 56:57]))
                RV(lambda e: e.tensor_tensor(out=R[:, 58:59], in0=R[:, 57:58], in1=R[:, 55:56], op=ALU.mult))
                RV(lambda e: e.tensor_tensor(out=R[:, 59:60], in0=R[:, 57:58], in1=R[:, 39:40], op=ALU.mult))
                RV(lambda e: e.tensor_tensor(out=R[:, 60:61], in0=R[:, 58:59], in1=R[:, 39:40], op=ALU.mult))
                OH = R[:, 64:128]
                pc = ps[bkB][:, 64:128]
                if hasattr(kb, "L"):
                    kb.split = len(kb.L)
                kb.op("pe", lambda e: e.matmul(pc, lhsT=Tri[:], rhs=OH, start=True, stop=False),
                      reads=[rk, "Tri"], writes=[("ps", bkB)])
                kb.op("pe", lambda e: e.matmul(pc, lhsT=ones[:], rhs=Sprev[:], start=False, stop=True),
                      reads=["Sprev", "ones"], writes=[("ps", bkB)])
                T64 = tmp64[sl]
                RTT = rtt[sl]
                tk, rtk = ("tmp64", sl), ("rtt", sl)
                kb.op("dve", lambda e: e.tensor_tensor(out=T64[:], in0=pc, in1=OH, op=ALU.mult),
                      reads=[("ps", bkB), rk], writes=[tk])
                kb.op("dve", lambda e: e.reduce_sum(out=RTT[:, 2:4], in_=T64[:].rearrange("p (s e) -> p s e", s=2),
                                                    axis=AX.X), reads=[tk], writes=[rtk])
                kb.op("dve", lambda e: e.tensor_scalar_add(out=RTT[:, 2:4], in0=RTT[:, 2:4], scalar1=-1.0),
                      reads=[rtk], writes=[rtk])
                kb.op("dve", lambda e: e.tensor_tensor(out=T64[:], in0=iota_e[:], in1=OH, op=ALU.mult),
                      reads=["iota_e", rk, tk], writes=[tk])
                kb.op("dve", lambda e: e.reduce_sum(out=RTT[:, 0:2], in_=T64[:].rearrange("p (s e) -> p s e", s=2),
                                                    axis=AX.X), reads=[tk, rtk], writes=[rtk])
                kb.op("dve", lambda e: e.tensor_copy(out=RTT[:, 4:5], in_=R[:, 60:61]), reads=[rk, rtk], writes=[rtk])
                kb.op("dve", lambda e: e.tensor_copy(out=RTT[:, 5:6], in_=R[:, 59:60]), reads=[rk, rtk], writes=[rtk])
                kb.op("dve", lambda e: e.tensor_scalar_add(out=RTT[:, 6:7], in0=iota_p[:],
                                                           scalar1=float(b * S + tok0)),
                      reads=["iota_p", rtk], writes=[rtk])
                kb.op("dve", lambda e: e.memset(RTT[:, 7:8], 0.0), reads=[rtk], writes=[rtk])
                kb.op("dve", lambda e: e.tensor_tensor(out=Sprev[:], in0=Sprev[:], in1=OH, op=ALU.add),
                      reads=["Sprev", rk], writes=["Sprev"])
                return functools.partial(kb_real.dma, "sp", rt_s[b * S + tok0:b * S + tok0 + 128, :], RTT[:], rtk,
                                         reads=[rtk])

            kb_real = kb
            tiles4 = [(b, n, t) for b in range(NS) for n in range(NG) for t in range(4)]
            groups4 = [(b, n) for b in range(NS) for n in range(NG)]
            NT4 = len(tiles4)
            p4_loadg(*groups4[0])
            p4_loadx(0)
            if NT4 > 1:
                p4_loadx(1)
            late = []
            for ti in range(0, NT4, 2):
                pair = [ti] + ([ti + 1] if ti + 1 < NT4 else [])
                for tj in pair:
                    b, n, t = tiles4[tj]
                    if tj + 2 < NT4:
                        p4_loadx(tj + 2)
                    if t == 1 and tj // 4 + 1 < len(groups4):
                        p4_loadg(*groups4[tj // 4 + 1])
                    if t == 0:
                        p4_M(b, n)
                    p4_A(b, n, t, tj % 2, tj % 3)
                for f in late:
                    f()
                late = []
                recs = []
                for tj in pair:
                    b, n, t = tiles4[tj]
                    r_ = Rec()
                    late.append(p4_B(b, n, t, tj % 2, r_))
                    recs.append(r_)
                if len(recs) == 2:
                    emit_interleaved(kb, recs[0].L[:recs[0].split], recs[1].L[:recs[1].split])
                    emit_interleaved(kb, recs[0].L[recs[0].split:], [])
                    emit_interleaved(kb, recs[1].L[recs[1].split:], [])
                else:
                    emit_interleaved(kb, recs[0].L, [])
            for f in late:
                f()
            kb.op("pe", lambda e: e.matmul(ps[1][:, 64:128], lhsT=ones[:], rhs=Sprev[:], start=True, stop=True),
                  reads=["Sprev", "ones"], writes=[("ps", 1)])
            kb.op("dve", lambda e: e.tensor_copy(out=cnt_bc[:], in_=ps[1][:, 64:128]), reads=[("ps", 1)],
                  writes=["cnt_bc"])
            kb.barrier()

        with contextlib.ExitStack() as p4b:
            I32 = mybir.dt.int32
            MAGIC_ = 12582912.0
            NRI = NB * BLK // 128
            rinit = SB("rinit", [128, NRI, 4], F32, p4b)
            bt = SB("bt", [128, 8, 32], F32, p4b)
            base = SB("base", [128, 64], F32, p4b)
            ebt = SB("ebt", [128, NB], F32, p4b)
            eb2 = SB("eb2", [128, 2, NB], F32, p4b)
            kp = SB("kp", [128, 12], F32, p4b)
            iota32 = SB("iota32", [128, 32], F32, p4b)
            rti = [SB("rti%d" % i, [128, 8], F32, p4b) for i in range(2)]
            ohs = [SB("ohs%d" % i, [128, 64], F32, p4b) for i in range(2)]
            dstf = [SB("dstf%d" % i, [128, 2], F32, p4b) for i in range(2)]
            dsti = [SB("dsti%d" % i, [128, 2], I32, p4b) for i in range(2)]
            pay = [SB("pay%d" % i, [128, 2, 4], F32, p4b) for i in range(2)]
            kb.op("pool", lambda e: e.memset(rinit[:], 0.0), writes=["rinit"])
            trash = SB("trash", [128, NRI], F32, p4b)
            kb.op("pool", lambda e: e.iota(trash[:], pattern=[[0, NRI]], base=2 * TT_, channel_multiplier=1,
                                           allow_small_or_imprecise_dtypes=True), writes=["trash"])
            kb.op("pool", lambda e: e.tensor_copy(out=rinit[:, :, 2], in_=trash[:]), reads=["rinit", "trash"],
                  writes=["rinit"])
            kb.dma("sp", rowtab_s.rearrange("(r p) c -> p r c", p=128), rinit[:], "rinit", reads=["rinit"],
                   writes=["rowtab_init"])
            kb.op("pool", lambda e: e.iota(iota32[:], pattern=[[1, 32]], base=0, channel_multiplier=0,
                                           allow_small_or_imprecise_dtypes=True), writes=["iota32"])
            kb.op("pool", lambda e: e.iota(kp[:, 0:8], pattern=[[128, 8]], base=0, channel_multiplier=1,
                                           allow_small_or_imprecise_dtypes=True), writes=["kp"])
            kb.op("pool", lambda e: e.iota(kp[:, 8:12], pattern=[[128, 4]], base=0, channel_multiplier=1,
                                           allow_small_or_imprecise_dtypes=True), reads=["kp"], writes=["kp"])
            B_ = lambda i: bt[:, i, :]

            def BV(fn, extra=()):
                kb.op("dve", fn, reads=["bt", "cnt_bc"] + list(extra), writes=["bt"])
            BV(lambda e: e.tensor_tensor(out=B_(0), in0=cnt_bc[:, 0:32], in1=cnt_bc[:, 32:64], op=ALU.add))
            BV(lambda e: e.tensor_scalar(out=B_(1), in0=B_(0), scalar1=BLK / 2 - 0.5, scalar2=1.0 / BLK, op0=ALU.add,
                                         op1=ALU.mult))
            BV(lambda e: e.tensor_scalar_add(out=B_(5), in0=B_(1), scalar1=MAGIC_))
            BV(lambda e: e.tensor_scalar_add(out=B_(2), in0=B_(5), scalar1=-MAGIC_))
            BV(lambda e: e.tensor_tensor_scan(out=B_(3), data0=ones[:, 0:32], data1=B_(2), initial=0.0, op0=ALU.mult,
                                              op1=ALU.add), extra=["ones"])
            BV(lambda e: e.tensor_tensor(out=B_(4), in0=B_(3), in1=B_(2), op=ALU.subtract))
            kb.op("dve", lambda e: e.tensor_scalar_mul(out=base[:, 0:32], in0=B_(4), scalar1=float(BLK)),
                  reads=["bt"], writes=["base"])
            kb.op("dve", lambda e: e.tensor_tensor(out=base[:, 32:64], in0=base[:, 0:32], in1=cnt_bc[:, 0:32], op=ALU.add),
                  reads=["base", "cnt_bc"], writes=["base"])
            for blk in range(NB):
                BV(lambda e: e.tensor_single_scalar(out=B_(5), in_=B_(3), scalar=float(blk), op=ALU.is_le))
                kb.op("dve", lambda e: e.reduce_sum(out=ebt[:, blk:blk + 1], in_=B_(5), axis=AX.X), reads=["bt", "ebt"],
                      writes=["ebt"])
            kb.op("dve", lambda e: e.tensor_scalar_mul(out=eb2[:, 0, :], in0=ebt[:], scalar1=1024.0), reads=["ebt"],
                  writes=["eb2"])
            kb.op("dve", lambda e: e.tensor_scalar_mul(out=eb2[:, 1, :], in0=ebt[:], scalar1=512.0), reads=["ebt", "eb2"],
                  writes=["eb2"])
            for blk in range(NB):
                kb.op("dve", lambda e: e.tensor_scalar(out=widx_in[:, blk, :], in0=kp[:, 0:8],
                                                       scalar1=eb2[:, 0, blk:blk + 1], scalar2=None, op0=ALU.add),
                      reads=["kp", "eb2", "widx"], writes=["widx"])
                kb.op("dve", lambda e: e.tensor_scalar(out=widx_out[:, blk, :], in0=kp[:, 8:12],
                                                       scalar1=eb2[:, 1, blk:blk + 1], scalar2=None, op0=ALU.add),
                      reads=["kp", "eb2", "widx"], writes=["widx"])
            for ti in range(TT_ // 128):
                sl = ti % 2
                RI = rti[sl]
                rik, ohk, dk, pk_ = ("rti", sl), ("ohs", sl), ("dst", sl), ("pay", sl)
                kb.dma("sp", RI[:], rt_s[ti * 128:(ti + 1) * 128, :], rik, writes=[rik])
                for sidx in range(2):
                    hsl = slice(sidx * 32, (sidx + 1) * 32)
                    kb.op("dve", lambda e: e.tensor_scalar(out=ohs[sl][:, hsl], in0=iota32[:],
                                                           scalar1=RI[:, sidx:sidx + 1], scalar2=None, op0=ALU.is_equal),
                          reads=["iota32", rik, ohk], writes=[ohk])
                kb.op("dve", lambda e: e.tensor_tensor(out=ohs[sl][:], in0=ohs[sl][:], in1=base[:], op=ALU.mult),
                      reads=[ohk, "base"], writes=[ohk])
                kb.op("dve", lambda e: e.reduce_sum(out=dstf[sl][:], in_=ohs[sl][:].rearrange("p (s e) -> p s e", s=2),
                                                    axis=AX.X), reads=[ohk, dk], writes=[dk])
                kb.op("dve", lambda e: e.tensor_tensor(out=dstf[sl][:], in0=dstf[sl][:], in1=RI[:, 2:4], op=ALU.add),
                      reads=[dk, rik], writes=[dk])
                kb.op("dve", lambda e: e.tensor_copy(out=dsti[sl][:], in_=dstf[sl][:]), reads=[dk, ("dsti", sl)],
                      writes=[("dsti", sl)])
                kb.op("dve", lambda e: e.memset(pay[sl][:], 0.0), reads=[pk_], writes=[pk_])
                for sidx in range(2):
                    kb.op("dve", lambda e: e.tensor_copy(out=pay[sl][:, sidx, 0:1], in_=RI[:, 6:7]), reads=[rik, pk_],
                          writes=[pk_])
                    kb.op("dve", lambda e: e.tensor_copy(out=pay[sl][:, sidx, 1:2], in_=RI[:, 4 + sidx:5 + sidx]),
                          reads=[rik, pk_], writes=[pk_])
                    kb.op("dve", lambda e: e.tensor_scalar_add(out=pay[sl][:, sidx, 2:3], in0=RI[:, 6:7],
                                                               scalar1=float(sidx * TT_)),
                          reads=[rik, pk_], writes=[pk_])
                for sidx in range(2):
                    kb.idma(rowtab_s[:, :], bass.IndirectOffsetOnAxis(ap=dsti[sl][:, sidx:sidx + 1], axis=0),
                            pay[sl][:, sidx, :], None, pk_, reads=[pk_, ("dsti", sl), "rowtab_init"])
            kb.barrier()

        with contextlib.ExitStack() as p5:
            I32 = mybir.dt.int32
            w_ein_f = w_ein.rearrange("e k n -> (e k) n")
            w_eout_f = w_eout.rearrange("e k n -> (e k) n")
            wei = [SB("wei%d" % i, [128, 8, 1024], BF16, p5) for i in range(3)]
            weo = [SB("weo%d" % i, [128, 4, 1024], BF16, p5) for i in range(3)]
            rtab = SB("rtab", [128, 3, 4, 4], F32, p5)
            rti32 = SB("rti32", [128, 3, 4, 2], I32, p5)
            xg = [SB("xg%d" % i, [128, D], F32, p5) for i in range(8)]
            xT = [SB("xT%d" % i, [128, 8, 512], BF16, p5) for i in range(2)]
            sa = [SB("sa%d" % i, [128, 512], F32, p5) for i in range(2)]
            hid = [SB("hid%d" % i, [128, 4, 512], BF16, p5) for i in range(2)]
            yb_ = [SB("yb%d" % i, [128, D], F32, p5) for i in range(8)]
            ocnt_ = [0]

            reg_in = nc.gpsimd.to_reg(32 * 1024 - 1)
            reg_out = nc.gpsimd.to_reg(32 * 512 - 1)

            def LOADW(blk):
                ws = blk % 3
                for k in range(8):
                    kb.idma(wei[ws][:, k, :], None, w_ein_f[:, :],
                            bass.IndirectOffsetOnAxis(ap=widx_in[:, blk, k:k + 1], axis=0), ("wei", ws, k),
                            reads=["widx"], writes=[("wei", ws, k)], bounds_check=reg_in, oob_is_err=False)
                for k in range(4):
                    kb.idma(weo[ws][:, k, :], None, w_eout_f[:, :],
                            bass.IndirectOffsetOnAxis(ap=widx_out[:, blk, k:k + 1], axis=0), ("weo", ws, k),
                            reads=["widx"], writes=[("weo", ws, k)], bounds_check=reg_out, oob_is_err=False)

            def LOADX(blk):
                rs = blk % 3
                for sub in range(4):
                    r0 = blk * BLK + sub * 128
                    rk_ = ("rtab", rs, sub)
                    gi_ = (blk % 2) * 4 + sub
                    kb.dma("sp", rtab[:, rs, sub, :], rowtab_s[r0:r0 + 128, :], rk_, writes=[rk_])
                    kb.op("dve", lambda e: e.tensor_copy(out=rti32[:, rs, sub, 0:1], in_=rtab[:, rs, sub, 0:1]),
                          reads=[rk_, ("rti32", rs, sub)], writes=[("rti32", rs, sub)])
                    kb.op("dve", lambda e: e.tensor_copy(out=rti32[:, rs, sub, 1:2], in_=rtab[:, rs, sub, 2:3]),
                          reads=[rk_, ("rti32", rs, sub)], writes=[("rti32", rs, sub)])
                    kb.idma(xg[gi_][:, :], None, h2tok_s[:, :],
                            bass.IndirectOffsetOnAxis(ap=rti32[:, rs, sub, 0:1], axis=0), ("xg", gi_),
                            reads=[("rti32", rs, sub)], writes=[("xg", gi_)])

            def COMP(blk):
                ws = blk % 3
                rs = blk % 3
                xs = blk % 2
                for sub in range(4):
                    gi_ = (blk % 2) * 4 + sub
                    for k in range(8):
                        pi = 6 + k // 4
                        kb.op("pe", lambda e: e.transpose(ps[pi][:, (k % 4) * 128:(k % 4 + 1) * 128],
                                                          xg[gi_][:, k * 128:(k + 1) * 128], ident[:]),
                              reads=[("xg", gi_)], writes=[("ps", pi)])
                    kb.op("act", lambda e: e.copy(out=xT[xs][:, 0:4, sub * 128:(sub + 1) * 128],
                                                  in_=ps[6][:].rearrange("p (k t) -> p k t", k=4)),
                          reads=[("ps", 6)], writes=[("xT", xs, sub, 0)])
                    kb.op("dve", lambda e: e.tensor_copy(out=xT[xs][:, 4:8, sub * 128:(sub + 1) * 128],
                                                         in_=ps[7][:].rearrange("p (k t) -> p k t", k=4)),
                          reads=[("ps", 7)], writes=[("xT", xs, sub, 1)])
                xkeys = [("xT", xs, sub, hh) for sub in range(4) for hh in range(2)]
                for j in range(4):
                    pa_i, pg_i = (j % 2) * 2, (j % 2) * 2 + 1
                    for k in range(8):
                        kb.op("pe", lambda e: e.matmul(ps[pa_i][:], lhsT=wei[ws][:, k, j * 128:(j + 1) * 128],
                                                       rhs=xT[xs][:, k, :], start=(k == 0), stop=(k == 7)),
                              reads=[("wei", ws, k)] + xkeys, writes=[("ps", pa_i)])
                    for k in range(8):
                        kb.op("pe", lambda e: e.matmul(ps[pg_i][:],
                                                       lhsT=wei[ws][:, k, 512 + j * 128:512 + (j + 1) * 128],
                                                       rhs=xT[xs][:, k, :], start=(k == 0), stop=(k == 7)),
                              reads=[("wei", ws, k)] + xkeys, writes=[("ps", pg_i)])
                    kb.op("act", lambda e: e.activation(out=sa[j % 2][:], in_=ps[pa_i][:], func=AF.Silu),
                          reads=[("ps", pa_i)], writes=[("sa", j % 2)])
                    kb.op("dve", lambda e: e.tensor_tensor(out=hid[xs][:, j, :], in0=ps[pg_i][:],
                                                           in1=sa[j % 2][:], op=ALU.mult),
                          reads=[("ps", pg_i), ("sa", j % 2)], writes=[("hid", xs, j)])
                for sub in range(4):
                    yi = (blk % 2) * 4 + sub
                    for half in range(2):
                        po = 4 + ocnt_[0] % 2
                        ocnt_[0] += 1
                        for j in range(4):
                            kb.op("pe", lambda e: e.matmul(ps[po][:], lhsT=hid[xs][:, j, sub * 128:(sub + 1) * 128],
                                                           rhs=weo[ws][:, j, half * 512:(half + 1) * 512],
                                                           start=(j == 0), stop=(j == 3)),
                                  reads=[("hid", xs, j), ("weo", ws, j)], writes=[("ps", po)])
                        kb.op("dve" if half == 0 else "act",
                              (lambda e: e.tensor_scalar_mul(out=yb_[yi][:, 0:512], in0=ps[po][:],
                                                             scalar1=rtab[:, rs, sub, 1:2])) if half == 0 else
                              (lambda e: e.activation(out=yb_[yi][:, 512:1024], in_=ps[po][:], func=AF.Copy,
                                                      scale=rtab[:, rs, sub, 1:2])),
                              reads=[("ps", po), ("rtab", rs, sub)], writes=[("yb", yi, half)])

            def SCAT(blk):
                rs = blk % 3
                for sub in range(4):
                    yi = (blk % 2) * 4 + sub
                    kb.idma(ys_s[:, :], bass.IndirectOffsetOnAxis(ap=rti32[:, rs, sub, 1:2], axis=0),
                            yb_[yi][:, :], None, ("yb", yi),
                            reads=[("yb", yi, 0), ("yb", yi, 1), ("rti32", rs, sub)])

            LOADW(0)
            LOADX(0)
            if NB > 1:
                LOADW(1)
            for blk in range(NB):
                if blk + 1 < NB:
                    LOADX(blk + 1)
                if blk + 2 < NB:
                    LOADW(blk + 2)
                COMP(blk)
                if blk >= 1:
                    SCAT(blk - 1)
            SCAT(NB - 1)
            kb.barrier()

        with contextlib.ExitStack() as p6:
            gate2 = SB("gate2", [128, NS, D], F32, p6)
            gfbc = SB("gfbc", [128, D], F32, p6)
            x5 = [SB("x5_%d" % i, [128, D], F32, p6) for i in range(3)]
            y0 = [SB("y0_%d" % i, [128, D], F32, p6) for i in range(3)]
            y1 = [SB("y1_%d" % i, [128, D], F32, p6) for i in range(3)]
            xo = [SB("xo%d" % i, [128, D], F32, p6) for i in range(3)]
            junk5 = SB("junk5", [128, D], F32, p6)
            st5 = [SB("st5_%d" % i, [128, 8], F32, p6) for i in range(3)]
            kb.dma("sp", gfbc[:], final_g.partition_broadcast(128), "gfbc", writes=["gfbc"])
            for b in range(NS):
                kb.dma("sp", gate2[:, b, :], gate_s[:, (b * 4 + 1) * D:(b * 4 + 2) * D], "gate2", writes=["gate2"])
            tiles6 = [(b, ti) for b in range(NS) for ti in range(NT)]

            def p6_load(idx):
                b, ti = tiles6[idx]
                sl = idx % 3
                tok0 = ti * 128
                tg = b * S + tok0
                kb.dma("sp", x5[sl][:], xmid_s[b, tok0:tok0 + 128, :], ("x5", sl), writes=[("x5", sl)])
                kb.dma("sp", y0[sl][:], ys_s[tg:tg + 128, :], ("y0", sl), writes=[("y0", sl)])
                kb.dma("sp", y1[sl][:], ys_s[TT_ + tg:TT_ + tg + 128, :], ("y1", sl), writes=[("y1", sl)])

            p6_load(0)
            if len(tiles6) > 1:
                p6_load(1)
            for idx in range(len(tiles6)):
                if True:
                    b, ti = tiles6[idx]
                    sl = idx % 3
                    tok0 = ti * 128
                    st = st5[sl]
                    if idx + 2 < len(tiles6):
                        p6_load(idx + 2)
                    kb.op("dve", lambda e: e.tensor_tensor(out=y0[sl][:], in0=y0[sl][:], in1=y1[sl][:], op=ALU.add),
                          reads=[("y0", sl), ("y1", sl)], writes=[("y0", sl)])
                    kb.op("dve", lambda e: e.tensor_tensor(out=xo[sl][:], in0=y0[sl][:], in1=gate2[:, b, :], op=ALU.mult),
                          reads=[("y0", sl), "gate2"], writes=[("xo", sl)])
                    kb.op("dve", lambda e: e.tensor_tensor(out=xo[sl][:], in0=xo[sl][:], in1=x5[sl][:], op=ALU.add),
                          reads=[("xo", sl), ("x5", sl)], writes=[("xo", sl)])
                    kb.op("act", lambda e: e.activation(out=junk5[:], in_=xo[sl][:], func=AF.Square,
                                                        accum_out=st[:, 0:1]),
                          reads=[("xo", sl)], writes=["junk5", ("st5", sl)])
                    kb.op("dve", lambda e: e.tensor_scalar(out=st[:, 1:2], in0=st[:, 0:1], scalar1=1.0 / D,
                                                           scalar2=1e-6, op0=ALU.mult, op1=ALU.add),
                          reads=[("st5", sl)], writes=[("st5", sl)])
                    kb.op("act", lambda e: e.activation(out=st[:, 2:3], in_=st[:, 1:2], func=AF.Ln),
                          reads=[("st5", sl)], writes=[("st5", sl)])
                    kb.op("act", lambda e: e.activation(out=st[:, 3:4], in_=st[:, 2:3], func=AF.Exp, scale=-0.5),
                          reads=[("st5", sl)], writes=[("st5", sl)])
                    kb.op("dve", lambda e: e.scalar_tensor_tensor(out=xo[sl][:], in0=xo[sl][:], scalar=st[:, 3:4],
                                                                  in1=gfbc[:], op0=ALU.mult, op1=ALU.mult),
                          reads=[("xo", sl), ("st5", sl), "gfbc"], writes=[("xo", sl)])
                    kb.dma("sp", y[b, tok0:tok0 + 128, :], xo[sl][:], ("xo", sl), reads=[("xo", sl)])
            kb.barrier()

        kb.barrier()
    return nc


def _prep_inputs(inputs):
    f = lambda a: np.ascontiguousarray(np.asarray(a, dtype=np.float32))
    d = {}
    d["w_ada"] = f(inputs["w_ada"][0])
    d["b_ada"] = f(inputs["b_ada"][0]).reshape(1, -1)
    d["norm1_g"] = f(inputs["norm1_g"][0]).reshape(1, -1)
    d["w_in"] = f(inputs["w_in"][0])
    d["rel_bias"] = f(inputs["rel_bias"])
    for n in ("lambda_q1", "lambda_k1", "lambda_q2", "lambda_k2", "subln_g"):
        d[n] = f(inputs[n][0]).reshape(1, -1)
    d["ssm_lambda_re"] = f(inputs["ssm_lambda_re"][0])
    d["ssm_lambda_im"] = f(inputs["ssm_lambda_im"][0])
    d["ssm_log_step"] = f(inputs["ssm_log_step"][0]).reshape(32, 1)
    d["ssm_b_re"] = f(inputs["ssm_b_re"][0])
    d["ssm_b_im"] = f(inputs["ssm_b_im"][0])
    d["ssm_c_re"] = f(inputs["ssm_c_re"][0])
    d["ssm_c_im"] = f(inputs["ssm_c_im"][0])
    d["ssm_d"] = f(inputs["ssm_d"][0]).reshape(512, 1)
    d["w_glu"] = f(inputs["w_glu"][0])
    d["w_proj_attn"] = f(inputs["w_proj_attn"][0])
    d["w_proj_ssm"] = f(inputs["w_proj_ssm"][0])
    d["w_out"] = f(inputs["w_out"][0])
    d["norm2_g"] = f(inputs["norm2_g"][0]).reshape(1, -1)
    d["w_router_group"] = f(inputs["w_router_group"][0])
    d["b_router_group"] = f(inputs["b_router_group"][0]).reshape(1, 4)
    d["w_router_expert"] = f(inputs["w_router_expert"][0])
    d["b_router_expert"] = f(inputs["b_router_expert"][0]).reshape(1, 32)
    d["w_expert_in"] = f(inputs["w_expert_in"][0])
    d["w_expert_out"] = f(inputs["w_expert_out"][0])
    d["final_g"] = f(inputs["final_g"]).reshape(1, -1)
    d["relmask"] = _relmask()
    return d


def _relmask():
    kk = np.arange(128)[:, None]
    qq = np.arange(128)[None, :]
    out = np.zeros((33, 2, 128, 128), np.float32)
    for blk in range(2):
        dist = qq - kk + 128 * blk
        n = np.maximum(dist, 0)
        lr = np.log(np.maximum(n, 1).astype(np.float32) / 16) / math.log(128 / 16)
        large = np.minimum(16 + (lr * 16).astype(np.int32), 31)
        bucket = np.where(n < 16, n, large)
        for bkt in range(32):
            out[bkt, blk] = ((bucket == bkt) & (dist >= 0))
        out[32, blk] = (dist < 0)
    return out.reshape(33, -1)


def kernel(**inputs):
    S = inputs["x"].shape[1]
    B = inputs["x"].shape[0]
    NS = B // NCORES
    nc = build(S, NS)
    shared = _prep_inputs(inputs)
    xs = np.ascontiguousarray(np.asarray(inputs["x"], dtype=np.float32))
    cs = np.ascontiguousarray(np.asarray(inputs["c"], dtype=np.float32))
    in_maps = []
    for i in range(NCORES):
        m = dict(shared)
        m["x"] = xs[i * NS:(i + 1) * NS]
        m["c"] = cs[i * NS:(i + 1) * NS]
        in_maps.append(m)
    res = run_bass_kernel_spmd(nc, in_maps, core_ids=list(range(NCORES)))
    return np.concatenate([r["y"] for r in res.results], axis=0)
```
